# Optimizing a Trainium2 kernel written in Bass

```python
import math
import jax, jax.numpy as jnp
from jax import lax
import numpy as np

D_MODEL = 2048
BATCH = 4
SEQ = 2048
DEPTH = 4

N_MIXERS = 4
MIX_WIDTH = D_MODEL
BRANCH_W = MIX_WIDTH // N_MIXERS
HEAD_DIM = 128
A_HEADS = BRANCH_W // HEAD_DIM
A_PATTERNS = ((128, 1), (512, 4), (2048, 16))
B_HEADS = BRANCH_W // HEAD_DIM
B_Q_RANK = 384
B_KV_RANK = 256
B_NOPE = 128
B_ROPE = 64
B_V = 128
C_HEADS = BRANCH_W // HEAD_DIM
MOBA_BLOCK = 256
MOBA_TOPK = 3
MOBA_QCHUNK = 32
D_HEAD_DIM = 64
D_HEADS = BRANCH_W // D_HEAD_DIM
D_KV_HEADS = 2
D_WINDOW = 128
BAND_BLOCK = 128
Q_BLOCK = 128
ROPE_THETA = 10000.0
RMS_EPS = 1e-6
LN_EPS = 1e-5
NEG = -1e30
DN_ALPHA = (2 * DEPTH) ** 0.25
DN_BETA = (8 * DEPTH) ** -0.25
SPLIT_SIZES = (BRANCH_W, BRANCH_W, BRANCH_W,
               B_Q_RANK, B_KV_RANK, B_ROPE,
               BRANCH_W, BRANCH_W, BRANCH_W,
               BRANCH_W, D_KV_HEADS * D_HEAD_DIM, D_KV_HEADS * D_HEAD_DIM,
               MIX_WIDTH)
N_IN = sum(SPLIT_SIZES)

kernel_name = "hybrid_dilated_mla_moba_sinkswa_deepnorm"


def rope(x, pos):
    dim = x.shape[-1]
    inv = ROPE_THETA ** (-jnp.arange(0, dim, 2, dtype=jnp.float32) / dim)
    ang = pos.astype(jnp.float32)[:, None] * inv[None, :]
    cos = jnp.concatenate([jnp.cos(ang), jnp.cos(ang)], -1)
    sin = jnp.concatenate([jnp.sin(ang), jnp.sin(ang)], -1)
    xf = x.astype(jnp.float32)
    x1, x2 = xf[..., : dim // 2], xf[..., dim // 2:]
    rot = jnp.concatenate([-x2, x1], -1)
    return (xf * cos + rot * sin).astype(x.dtype)


def rmsnorm(x, g):
    xf = x.astype(jnp.float32)
    y = xf * lax.rsqrt(jnp.mean(xf * xf, -1, keepdims=True) + RMS_EPS)
    return (y * g.astype(jnp.float32)).astype(x.dtype)


def layernorm(x, g, b):
    xf = x.astype(jnp.float32)
    mu = jnp.mean(xf, -1, keepdims=True)
    var = jnp.mean(jnp.square(xf - mu), -1, keepdims=True)
    y = (xf - mu) * lax.rsqrt(var + LN_EPS) * g.astype(jnp.float32) + b.astype(jnp.float32)
    return y.astype(x.dtype)


def heads(t, n):
    B, S, _ = t.shape
    return t.reshape(B, S, n, -1).transpose(0, 2, 1, 3)


def merge_heads(t):
    B, H, S, dh = t.shape
    return t.transpose(0, 2, 1, 3).reshape(B, S, H * dh)


def banded_window_stats(q, k, v, max_dist, scale):
    B, H, L, _ = q.shape
    dv = v.shape[-1]
    blk = BAND_BLOCK
    nb = -(-L // blk)
    Lp = nb * blk
    padw = ((0, 0), (0, 0), (0, Lp - L), (0, 0))
    qb = jnp.pad(q, padw).reshape(B, H, nb, blk, -1)
    kb = jnp.pad(k, padw).reshape(B, H, nb, blk, -1)
    vb = jnp.pad(v, padw).reshape(B, H, nb, blk, dv)

    def with_prev(t):
        prev = jnp.pad(t[:, :, :-1], ((0, 0), (0, 0), (1, 0), (0, 0), (0, 0)))
        return jnp.concatenate([prev, t], axis=3)

    kk, vv = with_prev(kb), with_prev(vb)
    s = jnp.einsum('bhnqd,bhnkd->bhnqk', qb, kk).astype(jnp.float32) * scale
    qi = jnp.arange(blk)[:, None] + blk
    kj = jnp.arange(2 * blk)[None, :]
    dist = qi - kj
    band = (dist >= 0) & (dist <= max_dist)
    has_prev = (jnp.arange(nb)[:, None, None] > 0) | (kj[None] >= blk)
    s = jnp.where(band[None] & has_prev, s, NEG)
    m = jnp.max(s, -1)
    p = jnp.exp(s - m[..., None])
    l = jnp.sum(p, -1)
    acc = jnp.einsum('bhnqk,bhnkd->bhnqd', p, vv.astype(jnp.float32))
    return (m.reshape(B, H, Lp)[:, :, :L], l.reshape(B, H, Lp)[:, :, :L],
            acc.reshape(B, H, Lp, dv)[:, :, :L])


def dilated_mixture_attention(q, k, v):
    B, H, S, dh = q.shape
    scale = dh ** -0.5
    ms, ls, accs = [], [], []
    for window, dil in A_PATTERNS:
        L = S // dil

        def gather(t):
            return t.reshape(B, H, L, dil, dh).transpose(0, 1, 3, 2, 4).reshape(B, H * dil, L, dh)

        m, l, acc = banded_window_stats(gather(q), gather(k), gather(v), window // dil, scale)
        ms.append(m.reshape(B, H, dil, L).transpose(0, 1, 3, 2).reshape(B, H, S))
        ls.append(l.reshape(B, H, dil, L).transpose(0, 1, 3, 2).reshape(B, H, S))
        accs.append(acc.reshape(B, H, dil, L, dh).transpose(0, 1, 3, 2, 4).reshape(B, H, S, dh))
    m_all = jnp.stack(ms)
    w = jnp.exp(m_all - jnp.max(m_all, 0, keepdims=True))
    num = jnp.sum(w[..., None] * jnp.stack(accs), 0)
    den = jnp.sum(w * jnp.stack(ls), 0)
    return num / den[..., None]


def causal_block_attention(q, k, v, scale):
    B, H, S, _ = q.shape
    kpos = jnp.arange(S)
    vf = v.astype(jnp.float32)

    def one_block(i):
        start = i * Q_BLOCK
        qb = lax.dynamic_slice_in_dim(q, start, Q_BLOCK, axis=2)
        s = jnp.einsum('bhqd,bhkd->bhqk', qb, k).astype(jnp.float32) * scale
        qpos = start + jnp.arange(Q_BLOCK)
        s = jnp.where(kpos[None, :] <= qpos[:, None], s, NEG)
        p = jax.nn.softmax(s, axis=-1)
        return jnp.einsum('bhqk,bhkd->bhqd', p, vf)

    o = lax.map(one_block, jnp.arange(S // Q_BLOCK))
    return o.transpose(1, 2, 0, 3, 4).reshape(B, H, S, -1)


def mla_attention(c_q, c_kv, k_rope, q_norm, w_uq, kv_norm, w_ukv, pos):
    B, S, _ = c_q.shape
    q = (rmsnorm(c_q, q_norm) @ w_uq).reshape(B, S, B_HEADS, B_NOPE + B_ROPE).transpose(0, 2, 1, 3)
    q = jnp.concatenate([q[..., :B_NOPE], rope(q[..., B_NOPE:], pos)], -1)
    kv = (rmsnorm(c_kv, kv_norm) @ w_ukv).reshape(B, S, B_HEADS, B_NOPE + B_V).transpose(0, 2, 1, 3)
    kr = jnp.broadcast_to(rope(k_rope, pos)[:, None], (B, B_HEADS, S, B_ROPE))
    k = jnp.concatenate([kv[..., :B_NOPE], kr], -1)
    v = kv[..., B_NOPE:]
    return causal_block_attention(q, k, v, (B_NOPE + B_ROPE) ** -0.5)


def moba_attention(q, k, v):
    B, H, S, dh = q.shape
    scale = dh ** -0.5
    nb = -(-S // MOBA_BLOCK)
    padw = ((0, 0), (0, 0), (0, nb * MOBA_BLOCK - S), (0, 0))
    kb = jnp.pad(k, padw).reshape(B, H, nb, MOBA_BLOCK, dh)
    vb = jnp.pad(v, padw).reshape(B, H, nb, MOBA_BLOCK, dh)
    kmean = jnp.mean(kb.astype(jnp.float32), axis=3)
    k_sel = min(MOBA_TOPK, nb)
    n_sel = k_sel * MOBA_BLOCK
    b_ix = jnp.arange(B)[:, None, None, None]
    h_ix = jnp.arange(H)[None, :, None, None]
    block_ids = jnp.arange(nb)

    def one_chunk(c):
        start = c * MOBA_QCHUNK
        n = start // MOBA_BLOCK
        qc = lax.dynamic_slice_in_dim(q, start, MOBA_QCHUNK, axis=2)
        gate = jnp.einsum('bhqd,bhnd->bhqn', qc.astype(jnp.float32), kmean)
        gate = jnp.where(block_ids < n, gate, NEG)
        _, idx = lax.top_k(gate, k_sel)
        sel_ok = jnp.repeat(idx < n, MOBA_BLOCK, axis=-1)
        kg = kb[b_ix, h_ix, idx].reshape(B, H, MOBA_QCHUNK, n_sel, dh)
        vg = vb[b_ix, h_ix, idx].reshape(B, H, MOBA_QCHUNK, n_sel, dh)
        s_sel = jnp.einsum('bhqd,bhqkd->bhqk', qc, kg).astype(jnp.float32) * scale
        s_sel = jnp.where(sel_ok, s_sel, NEG)
        k_own = lax.dynamic_index_in_dim(kb, n, axis=2, keepdims=False)
        v_own = lax.dynamic_index_in_dim(vb, n, axis=2, keepdims=False)
        s_own = jnp.einsum('bhqd,bhkd->bhqk', qc, k_own).astype(jnp.float32) * scale
        qpos = start + jnp.arange(MOBA_QCHUNK)
        kpos = n * MOBA_BLOCK + jnp.arange(MOBA_BLOCK)
        s_own = jnp.where(kpos[None, :] <= qpos[:, None], s_own, NEG)
        p = jax.nn.softmax(jnp.concatenate([s_sel, s_own], -1), axis=-1)
        return (jnp.einsum('bhqk,bhqkd->bhqd', p[..., :n_sel], vg.astype(jnp.float32))
                + jnp.einsum('bhqk,bhkd->bhqd', p[..., n_sel:], v_own.astype(jnp.float32)))

    o = lax.map(one_chunk, jnp.arange(S // MOBA_QCHUNK))
    return o.transpose(1, 2, 0, 3, 4).reshape(B, H, S, dh)


def sink_window_attention(q, k, v, sinks):
    rep = D_HEADS // D_KV_HEADS
    k = jnp.repeat(k, rep, axis=1)
    v = jnp.repeat(v, rep, axis=1)
    m, l, acc = banded_window_stats(q, k, v, D_WINDOW - 1, D_HEAD_DIM ** -0.5)
    sink = sinks.astype(jnp.float32)[None, :, None]
    m2 = jnp.maximum(m, sink)
    corr = jnp.exp(m - m2)
    den = l * corr + jnp.exp(sink - m2)
    return acc * (corr / den)[..., None]


def split_columns(proj):
    offs, acc = [], 0
    for s in SPLIT_SIZES[:-1]:
        acc += s
        offs.append(acc)
    return jnp.split(proj, offs, axis=-1)


def hybrid_layer(x, w_in, q_norm, w_uq, kv_norm, w_ukv, sinks, branch_norm, w_out, ln_g, ln_b, pos):
    B, S, _ = x.shape
    proj = x @ w_in
    (a_q, a_k, a_v, b_cq, b_ckv, b_kr, c_q, c_k, c_v, d_q, d_k, d_v, gate) = split_columns(proj)
    y_a = dilated_mixture_attention(rope(heads(a_q, A_HEADS), pos), rope(heads(a_k, A_HEADS), pos),
                                    heads(a_v, A_HEADS))
    y_b = mla_attention(b_cq, b_ckv, b_kr, q_norm, w_uq, kv_norm, w_ukv, pos)
    y_c = moba_attention(rope(heads(c_q, C_HEADS), pos), rope(heads(c_k, C_HEADS), pos),
                         heads(c_v, C_HEADS))
    y_d = sink_window_attention(rope(heads(d_q, D_HEADS), pos), rope(heads(d_k, D_KV_HEADS), pos),
                                heads(d_v, D_KV_HEADS), sinks)
    y = jnp.stack([merge_heads(y_a), merge_heads(y_b), merge_heads(y_c), merge_heads(y_d)], axis=2)
    y = y.astype(jnp.float32)
    y = y * lax.rsqrt(jnp.mean(y * y, -1, keepdims=True) + RMS_EPS)
    y = y.reshape(B, S, MIX_WIDTH) * branch_norm.astype(jnp.float32)
    y = (y * jax.nn.silu(gate.astype(jnp.float32))).astype(x.dtype)
    out = y @ w_out
    return layernorm(DN_ALPHA * x + out, ln_g, ln_b)


def setup_inputs(seed: int = 0) -> dict:
    key = jax.random.key(seed)
    ks = jax.random.split(key, 12)
    f32 = jnp.float32
    return {
        "x": jax.random.normal(ks[0], (BATCH, SEQ, D_MODEL), f32),
        "w_in": jax.random.normal(ks[1], (DEPTH, D_MODEL, N_IN), f32) * D_MODEL ** -0.5,
        "q_norm": 1.0 + 0.02 * jax.random.normal(ks[2], (DEPTH, B_Q_RANK), f32),
        "w_uq": jax.random.normal(ks[3], (DEPTH, B_Q_RANK, B_HEADS * (B_NOPE + B_ROPE)), f32) * B_Q_RANK ** -0.5,
        "kv_norm": 1.0 + 0.02 * jax.random.normal(ks[4], (DEPTH, B_KV_RANK), f32),
        "w_ukv": jax.random.normal(ks[5], (DEPTH, B_KV_RANK, B_HEADS * (B_NOPE + B_V)), f32) * B_KV_RANK ** -0.5,
        "sinks": 0.5 * jax.random.normal(ks[6], (DEPTH, D_HEADS), f32),
        "branch_norm": 1.0 + 0.02 * jax.random.normal(ks[7], (DEPTH, MIX_WIDTH), f32),
        "w_out": jax.random.normal(ks[8], (DEPTH, MIX_WIDTH, D_MODEL), f32) * (MIX_WIDTH ** -0.5 * DN_BETA),
        "ln_gamma": 1.0 + 0.02 * jax.random.normal(ks[9], (DEPTH, D_MODEL), f32),
        "ln_beta": 0.02 * jax.random.normal(ks[10], (DEPTH, D_MODEL), f32),
    }


def reference(x, w_in, q_norm, w_uq, kv_norm, w_ukv, sinks, branch_norm, w_out, ln_gamma, ln_beta):
    pos = jnp.arange(x.shape[1], dtype=jnp.int32)
    for l in range(DEPTH):
        x = hybrid_layer(x, w_in[l], q_norm[l], w_uq[l], kv_norm[l], w_ukv[l], sinks[l],
                         branch_norm[l], w_out[l], ln_gamma[l], ln_beta[l], pos)
    return x
```

```python
import contextlib
import numpy as np
import ml_dtypes
import concourse.bass as bass
import concourse.mybir as mybir
from concourse.bass_utils import run_bass_kernel_spmd

F32 = mybir.dt.float32
BF16 = mybir.dt.bfloat16
AF = mybir.ActivationFunctionType
ALU = mybir.AluOpType
AX = mybir.AxisListType

D = 2048
NIN = 6592
NOWN = 1024
NB = 8
DEPTH = 4
ALPHA = (2 * DEPTH) ** 0.25
O_AQ, O_AK, O_AV = 0, 512, 1024
O_BCQ, O_BCKV, O_BKR = 1536, 1920, 2176
O_CQ, O_CK, O_CV = 2240, 2752, 3264
O_DQ, O_DK, O_DV = 3776, 4288, 4416
O_G = 4544
NEG = -1e30
PAIRS = [[0, 1], [2, 3], [4, 5], [6, 7]]


def gblk(r, j):
    return 2 * j + (j + r) % 2


def kap(G):
    jj = G // 2
    rr = (G % 2 - jj) % 2
    return 8 * rr + jj


class Sched:
    ENGS = ("sync", "scalar", "vector", "gpsimd", "tensor")

    def __init__(self, nc):
        self.nc = nc
        self.ops = {e: [] for e in self.ENGS}
        self.sems = {}
        self.cnt = {}
        self.mult = {}
        self.waited = {e: {} for e in self.ENGS}
        self._cms = []

    def sem(self, key, mult=1):
        if key not in self.sems:
            cm = self.nc.semaphore(key)
            self.sems[key] = cm.__enter__()
            self._cms.append(cm)
            self.cnt[key] = 0
            self.mult[key] = mult
        return self.sems[key]

    def wait(self, eng, toks):
        for t in toks:
            if t is None:
                continue
            key, n = t
            w = self.waited[eng]
            if w.get(key, 0) >= n:
                continue
            w[key] = n
            h = self.sems[key]
            self.ops[eng].append(lambda e, h=h, v=n * self.mult[key]: e.wait_ge(h, v))

    def op(self, eng, fn, deps=(), sig=True, key=None):
        self.wait(eng, deps)
        if sig:
            key = key or ("c_" + eng)
            h = self.sem(key)
            self.cnt[key] += 1
            self.ops[eng].append(lambda e, fn=fn, h=h: fn(e).then_inc(h, 1))
            return (key, self.cnt[key])
        self.ops[eng].append(lambda e, fn=fn: fn(e))
        return None

    def dma(self, eng, chan, items, deps=()):
        key = "d_" + chan
        h = self.sem(key, 16)
        if self.cnt[key]:
            self.wait(eng, [(key, self.cnt[key])])
        self.wait(eng, deps)
        for (o, i, kw) in items:
            self.cnt[key] += 1
            self.ops[eng].append(lambda e, o=o, i=i, kw=kw, h=h: e.dma_start(out=o, in_=i, **kw).then_inc(h, 16))
        return (key, self.cnt[key])

    def barrier(self):
        toks = [(k, c) for k, c in self.cnt.items() if c > 0]
        for e in self.ENGS:
            self.wait(e, toks)

    def emit(self):
        self.barrier()
        with self.nc.Block() as block:
            @block.sync
            def _(e):
                for f in self.ops["sync"]:
                    f(e)

            @block.scalar
            def _(e):
                for f in self.ops["scalar"]:
                    f(e)

            @block.vector
            def _(e):
                for f in self.ops["vector"]:
                    f(e)

            @block.gpsimd
            def _(e):
                for f in self.ops["gpsimd"]:
                    f(e)

            @block.tensor
            def _(e):
                for f in self.ops["tensor"]:
                    f(e)
        for cm in reversed(self._cms):
            cm.__exit__(None, None, None)


class Reg:
    def __init__(self):
        self.w = None
        self.r = {}

    def rdeps(self):
        return [self.w] if self.w else []

    def wdeps(self):
        return ([self.w] if self.w else []) + list(self.r.values())

    def read(self, tok):
        if tok is None:
            return
        k = tok[0]
        if k not in self.r or self.r[k][1] < tok[1]:
            self.r[k] = tok

    def write(self, tok):
        self.w = tok
        self.r = {}


def build(depth=DEPTH, debug=False):
    nc = bass.Bass("TRN2", target_bir_lowering=False)
    S = Sched(nc)
    dt_in = lambda name, shape, dt=F32: nc.dram_tensor(name, shape, dt, kind="ExternalInput")
    x_in = dt_in("x", [NOWN, D])
    w_in = dt_in("w_in", [DEPTH, D, NIN])
    w_uq = dt_in("w_uq", [DEPTH, 384, 768])
    w_ukv = dt_in("w_ukv", [DEPTH, 256, 1024])
    w_out = dt_in("w_out", [DEPTH, D, D])
    ln_g = dt_in("ln_gamma", [DEPTH, D])
    ln_b = dt_in("ln_beta", [DEPTH, D])
    cols_d = dt_in("cols", [128, 84])
    sinks_d = dt_in("sinks_bc", [128, 32])
    negm_d = dt_in("negm", [128, 64])
    tabs_d = dt_in("tabs", [128, 4, 1024])
    masks_d = dt_in("masks", [128, 44, 128], BF16)
    ident_d = dt_in("ident", [128, 128], BF16)
    out_d = nc.dram_tensor("out", [NOWN, D], F32, kind="ExternalOutput")
    if debug:
        ydbg = nc.dram_tensor("ydbg", [NOWN, D], F32, kind="ExternalOutput")
    xs = [nc.dram_tensor("xs0", [NOWN, D], F32), nc.dram_tensor("xs1", [NOWN, D], F32)]
    ibs = [[nc.dram_tensor(f"ib{l}_{i}", [320 if i == 3 else 1024, 1024], BF16) for i in range(4)] for l in range(depth)]
    obs = [[nc.dram_tensor(f"ob{l}_{i}", [640 if i == 3 else 2048, 1024], BF16) for i in range(4)] for l in range(depth)]

    es = contextlib.ExitStack()
    with es:
        sb = lambda name, shape, dt: es.enter_context(nc.sbuf_tensor("sb_" + name, shape, dt))
        xT = sb("xT", [128, 16, NOWN], BF16)
        yT = sb("yT", [128, 16, NOWN], BF16)
        arena = sb("arena", [128, 32768], BF16)
        wbuf = sb("wbuf", [128, 3, 4096], BF16)
        stage = sb("stage", [128, 4, 512], BF16)
        tabs = sb("tabs", [128, 4, 1024], F32)
        masks = sb("masks", [128, 44, 128], BF16)
        ident = sb("ident", [128, 128], BF16)
        pt = sb("pt", [128, 3, 512], BF16)
        ytile = sb("ytile", [128, 4, 512], F32)
        rt1 = sb("rt1", [128, 512], F32)
        rt2 = sb("rt2", [128, 512], F32)
        cols = sb("cols", [128, 84], F32)
        esink = sb("esink", [128, 32], F32)
        negm = sb("negm", [128, 8, 8], F32)
        sm = sb("sm", [128, 256], F32)
        accC = sb("accC", [128, 4, 132], F32)
        sg = sb("sg", [128, 2, 256], F32)
        ygb = sb("ygb", [128, 2, 256], BF16)
        cqb = sb("cqb", [128, 384], BF16)
        P = es.enter_context(nc.psum_tensor("P", [128, 8, 512], F32))

        R = {}
        def reg(name):
            if name not in R:
                R[name] = Reg()
            return R[name]
        pb = [reg(f"pb{i}") for i in range(4)]
        por = [reg(f"po{i}") for i in range(8)]
        wreg = [reg(f"w{i}") for i in range(3)]
        streg = [reg(f"st{i}") for i in range(4)]
        ptreg = [reg(f"pt{i}") for i in range(3)]

        po8 = P[:, 4:8, :].rearrange("p k (b c) -> p (k b) c", c=256)

        def av(off, n):
            return arena[:, off:off + n]
        kT = av(0, 8192).rearrange("p (h t) -> p h t", h=4)
        Vaug = av(8192, 8256).rearrange("p (k h c) -> p k h c", k=16, h=4)
        VD = av(8192, 2080).rearrange("p (k h c) -> p k h c", k=16, h=2)
        qT = av(16448, 4096).rearrange("p (h t) -> p h t", h=4)
        qrT = av(20544, 4096).rearrange("p (h t) -> p h t", h=4)
        krT = av(24640, 2048)
        cqnT = av(26688, 3072).rearrange("p (k t) -> p k t", k=3)
        kmean = av(29760, 32).rearrange("p (h n) -> p h n", h=4)
        ckvnT = av(29792, 2048).rearrange("p (k t) -> p k t", k=2)
        wo = arena[:, :].rearrange("p (k c) -> p k c", k=16)
        gam = wbuf[:, 0, :].bitcast(F32)
        bet = wbuf[:, 1, :].bitcast(F32)
        xr = wbuf[:, 2, :].bitcast(F32)
        zt = ytile[:, :, :].rearrange("p a b -> p (a b)")
        xb = stage[:, :, :].rearrange("p a b -> p (a b)")

        def OP(eng, fn, rd=(), wr=(), deps=()):
            d = list(deps)
            for t in rd:
                d.extend(t.rdeps())
            for t in wr:
                d.extend(t.wdeps())
            tok = S.op(eng, fn, d)
            for t in rd:
                t.read(tok)
            for t in wr:
                t.write(tok)
            return tok

        def DMA(eng, chan, items, rd=(), wr=(), deps=()):
            d = list(deps)
            for t in rd:
                d.extend(t.rdeps())
            for t in wr:
                d.extend(t.wdeps())
            tok = S.dma(eng, chan, [(o, i, {}) for (o, i) in items], d)
            for t in rd:
                t.read(tok)
            for t in wr:
                t.write(tok)
            return tok

        def MM(calls, rd=(), wr=()):
            d = []
            for t in rd:
                d.extend(t.rdeps())
            for t in wr:
                d.extend(t.wdeps())
            S.wait("tensor", d)
            for f in calls[:-1]:
                S.op("tensor", f, sig=False)
            tok = S.op("tensor", calls[-1])
            for t in rd:
                t.read(tok)
            for t in wr:
                t.write(tok)
            return tok

        def mm(out, lhsT, rhs, start, stop):
            return lambda e: e.matmul(out, lhsT=lhsT, rhs=rhs, start=start, stop=stop)

        creg = reg("const")
        DMA("sync", "const", [(tabs[:], tabs_d[:, :, :]), (masks[:], masks_d[:, :, :]), (ident[:], ident_d[:, :]),
                              (cols[:], cols_d[:, :]), (esink[:], sinks_d[:, :]),
                              (negm[:].rearrange("p a b -> p (a b)"), negm_d[:, :])], wr=[creg])
        OP("scalar", lambda e: e.activation(out=esink[:], in_=esink[:], func=AF.Exp), rd=[creg], wr=[reg("esink")])
        S.barrier()
        cos128, ssin128, cos64, ssin64 = (tabs[:, i, :] for i in range(4))
        TA0 = 0
        MB0 = 34
        MD0 = 38

        wstate = {"i": 0}

        def wload(items, name):
            s = wstate["i"] % 3
            wstate["i"] += 1
            DMA("gpsimd", f"w{s}", items(s), wr=[wreg[s]])
            return s

        def wv(s):
            return wbuf[:, s, :].rearrange("p (k c) -> p k c", c=256)

        def std_items(l, off, n):
            return lambda s: [(wv(s)[:, :, 0:n], w_in[l, :, off:off + n].rearrange("(k p) c -> p k c", p=128))]

        class WQ:
            def __init__(self, specs):
                self.specs = specs
                self.issued = 0
                self.slots = []
                self.pos = 0

            def _issue(self):
                name, items = self.specs[self.issued]
                self.slots.append(wload(items, name))
                self.issued += 1

            def next(self, name):
                assert self.specs[self.pos][0] == name, (self.specs[self.pos][0], name)
                while self.issued < min(len(self.specs), self.pos + 2):
                    self._issue()
                s = self.slots[self.pos]
                self.pos += 1
                return s

            def prefetch(self):
                while self.issued < min(len(self.specs), self.pos + 2):
                    self._issue()

        def layer_specs(l):
            sp = []
            for nm, off in (("Ak", O_AK), ("Av", O_AV), ("Ck", O_CK), ("Cv", O_CV)):
                for g in range(2):
                    sp.append((f"{nm}{g}", std_items(l, off + 256 * g, 256)))
            sp.append(("Bkv", std_items(l, O_BCKV, 256)))
            sp.append(("Bkr", std_items(l, O_BKR, 64)))
            sp.append(("WUKV", lambda s: [(wbuf[:, s, 0:2048].rearrange("p (k c) -> p k c", k=2),
                                           w_ukv[l, :, :].rearrange("(k p) c -> p k c", p=128))]))
            sp.append(("Dkv", std_items(l, O_DK, 256)))

            def gates(m):
                r = []
                for T in range(2):
                    for g in range(2):
                        r.append((f"G{m}{g}", std_items(l, O_G + 512 * m + 256 * g, 256)))
                return r
            sp += [("Aq0", std_items(l, O_AQ, 256)), ("Aq1", std_items(l, O_AQ + 256, 256))] + gates(0)
            sp += [("Cq0", std_items(l, O_CQ, 256)), ("Cq1", std_items(l, O_CQ + 256, 256))] + gates(2)
            sp += [("Bcq0", std_items(l, O_BCQ, 256)), ("Bcq1", std_items(l, O_BCQ + 256, 128)),
                   ("WUQ", lambda s: [(wbuf[:, s, 0:2304].rearrange("p (k c) -> p k c", k=3),
                                       w_uq[l, :, :].rearrange("(k p) c -> p k c", p=128))])] + gates(1)

            def dq_items(cp):
                def f(s):
                    it = []
                    for i in range(2):
                        c = 2 * cp + i
                        it.append((wv(s)[:, :, 128 * i:128 * i + 64],
                                   w_in[l, :, O_DQ + 64 * c:O_DQ + 64 * c + 64].rearrange("(k p) c -> p k c", p=128)))
                        it.append((wv(s)[:, :, 128 * i + 64:128 * i + 128],
                                   w_in[l, :, O_DQ + 64 * (4 + c):O_DQ + 64 * (4 + c) + 64].rearrange("(k p) c -> p k c", p=128)))
                    return it
                return f
            sp += [("Dq0", dq_items(0)), ("Dq1", dq_items(1))] + gates(3)
            return sp

        ctr = {"pp": 0, "ps": 0, "pt": 0, "st": 0, "po": 0, "sgq": 0, "po8": 0}

        def nxt(k, n):
            v = ctr[k] % n
            ctr[k] += 1
            return v

        xTreg = reg("xT")

        def proj_fm(s, c0, ncol, T):
            b = nxt("pp", 2)
            calls = [mm(P[0:ncol, b, :], wv(s)[:, k, c0:c0 + ncol], xT[:, k, T * 512:(T + 1) * 512], k == 0, k == 15)
                     for k in range(16)]
            MM(calls, rd=[wreg[s], xTreg], wr=[pb[b]])
            return b

        def proj_tm(s, j, ncol):
            b = nxt("pp", 2)
            calls = [mm(P[:, b, 0:ncol], xT[:, k, j * 128:(j + 1) * 128], wv(s)[:, k, 0:ncol], k == 0, k == 15)
                     for k in range(16)]
            MM(calls, rd=[wreg[s], xTreg], wr=[pb[b]])
            return b

        rtreg = reg("rt")

        def rope(src_bank, np_, dst, dstregs, half, T, n=512):
            ct, stb = (cos128, ssin128) if half == 64 else (cos64, ssin64)
            src = P[:, src_bank, 0:n]
            tsl = slice(T * 512, T * 512 + n)
            OP("vector", lambda e: e.tensor_tensor(out=rt1[0:np_, 0:n], in0=src[0:np_, :], in1=ct[0:np_, tsl], op=ALU.mult),
               rd=[pb[src_bank]], wr=[rtreg])
            for base in range(0, np_, 2 * half):
                lo, mid, hi = base, base + half, base + 2 * half
                OP("vector", lambda e, lo=lo, mid=mid, hi=hi: e.tensor_tensor(
                    out=rt2[lo:mid, 0:n], in0=src[mid:hi, :], in1=stb[lo:mid, tsl], op=ALU.mult), rd=[pb[src_bank]], wr=[rtreg])
                OP("vector", lambda e, lo=lo, mid=mid, hi=hi: e.tensor_tensor(
                    out=rt2[mid:hi, 0:n], in0=src[lo:mid, :], in1=stb[mid:hi, tsl], op=ALU.mult), rd=[pb[src_bank]], wr=[rtreg])
            OP("vector", lambda e: e.tensor_tensor(out=dst, in0=rt1[0:np_, 0:n], in1=rt2[0:np_, 0:n], op=ALU.add),
               rd=[rtreg], wr=dstregs)

        def transposes_to(dst_fn, src_fn, nchunk, srcregs, dstregs, scale_fn=None, np_out=128):
            c = 0
            while c < nchunk:
                g = min(8, nchunk - c)
                b = nxt("pp", 2)
                pbv = P[:, b, :].bitcast(BF16)
                calls = []
                for i in range(g):
                    calls.append((lambda e, o=pbv[:, i * 128:(i + 1) * 128], s_=src_fn(c + i): e.transpose(o, s_, ident[:])))
                MM(calls, rd=srcregs + [reg("const")], wr=[pb[b]])
                for i in range(g):
                    d_ = dst_fn(c + i)
                    i_ = pbv[0:np_out, i * 128:(i + 1) * 128]
                    if scale_fn is None:
                        OP("scalar", lambda e, d_=d_, i_=i_: e.activation(out=d_, in_=i_, func=AF.Copy), rd=[pb[b]], wr=dstregs)
                    else:
                        OP("scalar", lambda e, d_=d_, i_=i_, sc=scale_fn(c + i): e.activation(out=d_, in_=i_, func=AF.Copy, scale=sc),
                           rd=[pb[b]], wr=dstregs)
                c += g

        def block_to_xT(src_f32, srcreg, j):
            OP("scalar", lambda e: e.activation(out=xb, in_=src_f32, func=AF.Copy), rd=[srcreg], wr=streg)
            transposes_to(lambda c: xT[:, c, j * 128:(j + 1) * 128], lambda c: xb[:, c * 128:(c + 1) * 128], 16,
                          list(streg), [xTreg])

        def stage_out(src_ap_fn, srcregs, dst_dram, eng="scalar"):
            s = nxt("st", 4)
            src, shape_p, shape_n = src_ap_fn
            OP(eng, lambda e: e.activation(out=stage[0:shape_p, s, 0:shape_n], in_=src, func=AF.Copy) if eng == "scalar"
               else e.tensor_copy(out=stage[0:shape_p, s, 0:shape_n], in_=src), rd=srcregs, wr=[streg[s]])
            DMA("sync", f"st{s}", [(dst_dram, stage[0:shape_p, s, 0:shape_n])], rd=[streg[s]])

        def rmsnorm_rows(bank, ncol, dst_bf, dstregs, eps=1e-6):
            r = reg("sm")
            OP("vector", lambda e: e.memset(sm[:, 0:1], 0.0), wr=[r])
            OP("scalar", lambda e: e.activation(out=rt1[:, 0:ncol], in_=P[:, bank, 0:ncol], func=AF.Square, accum_out=sm[:, 0:1]),
               rd=[pb[bank]], wr=[r, rtreg])
            OP("vector", lambda e: e.tensor_scalar(out=sm[:, 1:2], in0=sm[:, 0:1], scalar1=1.0 / ncol, scalar2=eps, op0=ALU.mult, op1=ALU.add),
               rd=[r], wr=[r])
            OP("scalar", lambda e: e.activation(out=sm[:, 2:3], in_=sm[:, 1:2], func=AF.Ln), rd=[r], wr=[r])
            OP("scalar", lambda e: e.activation(out=sm[:, 3:4], in_=sm[:, 2:3], func=AF.Exp, scale=-0.5), rd=[r], wr=[r])
            OP("scalar", lambda e: e.activation(out=dst_bf, in_=P[:, bank, 0:ncol], func=AF.Copy, scale=sm[:, 3:4]),
               rd=[r, pb[bank]], wr=dstregs)

        xrreg = wreg[2]
        for j in range(NB):
            DMA("sync", "xr", [(xr, x_in[j * 128:(j + 1) * 128, :])], wr=[xrreg])
            block_to_xT(xr, xrreg, j)
        S.barrier()

        for l in range(depth):
            WS = WQ(layer_specs(l))
            ib, ob = ibs[l], obs[l]
            ccoll = [None] * 4
            areg = {n: reg(f"a_{n}") for n in ("kT", "V", "qT", "qrT", "krT", "cqnT", "kmean", "ckvnT")}

            def collective(i, deps):
                S.wait("gpsimd", deps)
                key = f"cc{i}"
                S.sem(key)
                tok = S.op("gpsimd", lambda e, ii=ib[i], oo=ob[i]: e.collective_compute(
                    "AllGather", ALU.bypass, replica_groups=PAIRS, ins=[ii.ap().opt()], outs=[oo.ap().opt()]), key=key)
                ccoll[i] = tok

            def kside_AC(nm, ci):
                toks = []
                for g in range(2):
                    s = WS.next(f"{nm}k{g}")
                    for hh in range(2):
                        h = 2 * g + hh
                        for T in range(2):
                            b = proj_fm(s, hh * 128, 128, T)
                            st = nxt("st", 4)
                            rope(b, 128, stage[:, st, :], [streg[st]], 64, T)
                            toks.append(DMA("sync", f"st{st}", [(ib[ci][h * 128:(h + 1) * 128, T * 512:(T + 1) * 512], stage[:, st, :])],
                                            rd=[streg[st]]))
                vview = ib[ci][512:1024, :].rearrange("r (two c) -> (r two) c", two=2)
                for g in range(2):
                    s = WS.next(f"{nm}v{g}")
                    for j in range(NB):
                        b = proj_tm(s, j, 256)
                        st = nxt("st", 4)
                        OP("scalar", lambda e, b=b, st=st: e.activation(out=stage[:, st, 0:256], in_=P[:, b, 0:256], func=AF.Copy),
                           rd=[pb[b]], wr=[streg[st]])
                        toks.append(DMA("sync", f"st{st}", [(vview[j * 128:(j + 1) * 128, g * 256:(g + 1) * 256], stage[:, st, 0:256])],
                                        rd=[streg[st]]))
                collective(ci, toks)

            kside_AC("A", 0)
            kside_AC("C", 1)

            toksB, toksD = [], []
            s = WS.next("Bkv")
            for j in range(NB):
                b = proj_tm(s, j, 256)
                rmsnorm_rows(b, 256, cqb[:, 0:256], [reg("cqb")])
                transposes_to(lambda c, j=j: ckvnT[:, c, j * 128:(j + 1) * 128], lambda c: cqb[:, c * 128:(c + 1) * 128], 2,
                              [reg("cqb")], [areg["ckvnT"]], scale_fn=lambda c: cols[:, 21 * l + 3 + c:21 * l + 4 + c])
            s = WS.next("Bkr")
            for T in range(2):
                b = proj_fm(s, 0, 64, T)
                st = nxt("st", 4)
                rope(b, 64, stage[0:64, st, :], [streg[st]], 32, T)
                toksD.append(DMA("sync", f"st{st}", [(ib[3][0:64, T * 512:(T + 1) * 512], stage[0:64, st, :])], rd=[streg[st]]))
            s = WS.next("WUKV")
            wkv = wbuf[:, s, 0:2048].rearrange("p (k h c) -> p k h c", k=2, h=4)
            for h in range(4):
                for T in range(2):
                    b = nxt("pp", 2)
                    MM([mm(P[:, b, :], wkv[:, k, h, 0:128], ckvnT[:, k, T * 512:(T + 1) * 512], k == 0, k == 1) for k in range(2)],
                       rd=[wreg[s], areg["ckvnT"]], wr=[pb[b]])
                    st = nxt("st", 4)
                    OP("scalar", lambda e, b=b, st=st: e.activation(out=stage[:, st, :], in_=P[:, b, :], func=AF.Copy),
                       rd=[pb[b]], wr=[streg[st]])
                    toksB.append(DMA("sync", f"st{st}", [(ib[2][h * 128:(h + 1) * 128, T * 512:(T + 1) * 512], stage[:, st, :])],
                                     rd=[streg[st]]))
            vviewB = ib[2][512:1024, :].rearrange("r (two c) -> (r two) c", two=2)
            for j in range(NB):
                b = nxt("pp", 2)
                MM([mm(P[:, b, :].rearrange("p (h c) -> p h c", h=4), ckvnT[:, k, j * 128:(j + 1) * 128], wkv[:, k, :, 128:256], k == 0, k == 1)
                    for k in range(2)], rd=[wreg[s], areg["ckvnT"]], wr=[pb[b]])
                st = nxt("st", 4)
                OP("scalar", lambda e, b=b, st=st: e.activation(out=stage[:, st, :], in_=P[:, b, :], func=AF.Copy),
                   rd=[pb[b]], wr=[streg[st]])
                toksB.append(DMA("sync", f"st{st}", [(vviewB[j * 128:(j + 1) * 128, :], stage[:, st, :])], rd=[streg[st]]))
            collective(2, toksB)

            s = WS.next("Dkv")
            for T in range(2):
                b = proj_fm(s, 0, 128, T)
                st = nxt("st", 4)
                rope(b, 128, stage[:, st, :], [streg[st]], 32, T)
                toksD.append(DMA("sync", f"st{st}", [(ib[3][64:192, T * 512:(T + 1) * 512], stage[:, st, :])], rd=[streg[st]]))
            vviewD = ib[3][192:320, :].rearrange("r (e c) -> (r e) c", e=8)
            for j in range(NB):
                b = nxt("pp", 2)
                MM([mm(P[:, b, 0:128], xT[:, k, j * 128:(j + 1) * 128], wv(s)[:, k, 128:256], k == 0, k == 15) for k in range(16)],
                   rd=[wreg[s], xTreg], wr=[pb[b]])
                st = nxt("st", 4)
                OP("scalar", lambda e, b=b, st=st: e.activation(out=stage[:, st, 0:128], in_=P[:, b, 0:128], func=AF.Copy),
                   rd=[pb[b]], wr=[streg[st]])
                toksD.append(DMA("sync", f"st{st}", [(vviewD[j * 128:(j + 1) * 128, :], stage[:, st, 0:128])], rd=[streg[st]]))
            collective(3, toksD)

            def load_ctx_AC(ci, wait_ones=True):
                items = []
                for rr in range(2):
                    items.append((kT[:, :, rr * 1024:(rr + 1) * 1024],
                                  ob[ci][rr * 1024:rr * 1024 + 512, :].rearrange("(h d) t -> d h t", d=128)))
                DMA("sync", "ctxk", items, wr=[areg["kT"]], deps=[ccoll[ci]])
                items = []
                for rr in range(2):
                    src = ob[ci][rr * 1024 + 512:rr * 1024 + 1024, :].rearrange("r (two c) -> (r two) c", two=2)
                    for h in range(4):
                        items.append((Vaug[:, rr * 8:(rr + 1) * 8, h, 0:128],
                                      src[:, h * 128:(h + 1) * 128].rearrange("(jj p) c -> p jj c", p=128)))
                DMA("sync", "ctxv", items, wr=[areg["V"]], deps=[ccoll[ci]])
                OP("vector", lambda e: e.memset(Vaug[:, :, :, 128:129], 1.0), wr=[areg["V"]])

            def qproj_AC(nm):
                for g in range(2):
                    s = WS.next(f"{nm}q{g}")
                    for hh in range(2):
                        h = 2 * g + hh
                        for T in range(2):
                            b = proj_fm(s, hh * 128, 128, T)
                            rope(b, 128, qT[:, h, T * 512:(T + 1) * 512], [areg["qT"]], 64, T)

            def normalize_po(slots, T, h, src4=None, srcregs=None, width=128, ycol=None):
                r = reg("sm")
                for b in range(4):
                    src = po8[:, slots[b], :] if src4 is None else src4[:, b, :]
                    rg = [por[slots[b]]] if src4 is None else srcregs
                    OP("vector", lambda e, src=src, b=b: e.reciprocal(out=sm[:, 8 + b:9 + b], in_=src[:, width:width + 1]), rd=rg, wr=[r])
                    OP("scalar", lambda e, src=src, b=b: e.activation(out=ytile[:, b, h * width:(h + 1) * width], in_=src[:, 0:width],
                                                                     func=AF.Copy, scale=sm[:, 8 + b:9 + b]),
                       rd=rg + [r], wr=[reg("ytile")])

            def scores(T, h, G, isB, scale):
                kp = kap(G)
                sbk = 2 + nxt("ps", 2)
                calls = [mm(P[:, sbk, :], kT[:, h, kp * 128:(kp + 1) * 128], qT[:, h, T * 512:(T + 1) * 512], True, not isB)]
                rd = [areg["kT"], areg["qT"]]
                if isB:
                    calls.append(mm(P[:, sbk, :], krT[0:64, kp * 128:(kp + 1) * 128], qrT[0:64, h, T * 512:(T + 1) * 512], False, True))
                    rd += [areg["krT"], areg["qrT"]]
                MM(calls, rd=rd, wr=[pb[sbk]])
                ps_ = nxt("pt", 3)
                OP("scalar", lambda e: e.activation(out=pt[:, ps_, :], in_=P[:, sbk, :], func=AF.Exp, scale=scale),
                   rd=[pb[sbk]], wr=[ptreg[ps_]])
                return ps_, kp

            def mask128(ps_, b, slot):
                OP("vector", lambda e: e.tensor_tensor(out=pt[:, ps_, b * 128:(b + 1) * 128], in0=pt[:, ps_, b * 128:(b + 1) * 128],
                                                        in1=masks[:, slot, :], op=ALU.mult), rd=[ptreg[ps_], creg], wr=[ptreg[ps_]])

            def attn_dense(T, h, isA, isB, scale):
                j0 = 4 * T
                ob_ = nxt("po", 2)
                slots = [ob_ * 4 + b for b in range(4)]
                for G in range(0, 8 * T + 8):
                    ps_, kp = scores(T, h, G, isB, scale)
                    for b in range(4):
                        j = j0 + b
                        if G > 2 * j + 1:
                            continue
                        if isA:
                            t = 2 * j + 1 - G
                            mask128(ps_, b, TA0 + t * 2 + (j % 2))
                        elif G >= 2 * j:
                            mask128(ps_, b, MB0 + (j % 2) * 2 + (G - 2 * j))
                    calls, wr = [], []
                    for b in range(4):
                        j = j0 + b
                        if G <= 2 * j + 1:
                            calls.append(mm(po8[:, slots[b], 0:129], pt[:, ps_, b * 128:(b + 1) * 128], Vaug[:, kp, h, :],
                                            G == 0 and b % 2 == 0, G == 2 * j + 1))
                            wr.append(por[slots[b]])
                    MM(calls, rd=[ptreg[ps_], areg["V"]], wr=wr)
                normalize_po(slots, T, h)

            def epilogue(m, T):
                r = reg("sm")
                yreg = reg("ytile")
                if debug:
                    for b in range(4):
                        DMA("sync", "dbg", [(ydbg[(4 * T + b) * 128:(4 * T + b + 1) * 128, m * 512:(m + 1) * 512], ytile[:, b, :])], rd=[yreg])
                OP("vector", lambda e: e.memset(sm[:, 16:20], 0.0), wr=[r])
                for b in range(4):
                    OP("scalar", lambda e, b=b: e.activation(out=rt1[:, :], in_=ytile[:, b, :], func=AF.Square, accum_out=sm[:, 16 + b:17 + b]),
                       rd=[yreg], wr=[r, rtreg])
                OP("vector", lambda e: e.tensor_scalar(out=sm[:, 20:24], in0=sm[:, 16:20], scalar1=1.0 / 512, scalar2=1e-6, op0=ALU.mult, op1=ALU.add),
                   rd=[r], wr=[r])
                OP("scalar", lambda e: e.activation(out=sm[:, 24:28], in_=sm[:, 20:24], func=AF.Ln), rd=[r], wr=[r])
                OP("scalar", lambda e: e.activation(out=sm[:, 28:32], in_=sm[:, 24:28], func=AF.Exp, scale=-0.5), rd=[r], wr=[r])
                for g in range(2):
                    s = WS.next(f"G{m}{g}")
                    for b in range(4):
                        j = 4 * T + b
                        bk = proj_tm(s, j, 256)
                        q = nxt("sgq", 2)
                        OP("scalar", lambda e, bk=bk, q=q: e.activation(out=sg[:, q, :], in_=P[:, bk, 0:256], func=AF.Silu),
                           rd=[pb[bk]], wr=[reg(f"sg{q}")])
                        OP("vector", lambda e, b=b, g=g, q=q: e.scalar_tensor_tensor(
                            out=ygb[:, q, :], in0=ytile[:, b, g * 256:(g + 1) * 256], scalar=sm[:, 28 + b:29 + b], in1=sg[:, q, :],
                            op0=ALU.mult, op1=ALU.mult), rd=[yreg, r, reg(f"sg{q}")], wr=[reg(f"yg{q}")])
                        c0 = m * 4 + g * 2
                        transposes_to(lambda c, j=j, c0=c0: yT[:, c0 + c, j * 128:(j + 1) * 128],
                                      lambda c, q=q: ygb[:, q, c * 128:(c + 1) * 128], 2, [reg(f"yg{q}")], [reg("yT")],
                                      scale_fn=lambda c, c0=c0: cols[:, 21 * l + 5 + c0 + c:21 * l + 6 + c0 + c])

            load_ctx_AC(0)
            WS.prefetch()
            qproj_AC("A")
            for T in range(2):
                for h in range(4):
                    attn_dense(T, h, True, False, 128 ** -0.5)
                epilogue(0, T)

            load_ctx_AC(1)
            qproj_AC("C")
            r = reg("sm")
            for h in range(4):
                OP("vector", lambda e, h=h: e.tensor_reduce(out=sm[:, 32:48], in_=kT[:, h, :].rearrange("p (k t) -> p k t", k=16),
                                                            op=ALU.add, axis=AX.X), rd=[areg["kT"]], wr=[r])
                OP("vector", lambda e: e.tensor_tensor(out=sm[:, 48:56], in0=sm[:, 32:40], in1=sm[:, 40:48], op=ALU.add), rd=[r], wr=[r])
                OP("vector", lambda e, h=h: e.tensor_scalar(out=kmean[:, h, :], in0=sm[:, 48:56], scalar1=1.0 / 256, scalar2=None, op0=ALU.mult),
                   rd=[r], wr=[areg["kmean"]])
            sel = sm[:, 64:96].rearrange("p (b n) -> p b n", b=4)
            gmv = sm[:, 96:128].rearrange("p (b n) -> p b n", b=4)
            top = sm[:, 128:160].rearrange("p (b n) -> p b n", b=4)
            selr = reg("sel")
            accr = reg("accC")
            for T in range(2):
                j0 = 4 * T
                for h in range(4):
                    bk = nxt("pp", 2)
                    MM([mm(P[:, bk, b * 8:(b + 1) * 8], qT[:, h, (j0 + b) * 128:(j0 + b + 1) * 128], kmean[:, h, :], True, True) for b in range(4)],
                       rd=[areg["qT"], areg["kmean"]], wr=[pb[bk]])
                    OP("vector", lambda e, bk=bk, j0=j0: e.tensor_tensor(out=gmv, in0=P[:, bk, 0:32].rearrange("p (b n) -> p b n", b=4),
                                                                  in1=negm[:, j0:j0 + 4, :], op=ALU.add), rd=[pb[bk], creg], wr=[selr])
                    for b in range(4):
                        OP("vector", lambda e, b=b: e.max(out=top[:, b, :], in_=gmv[:, b, :]), rd=[selr], wr=[selr])
                    for b in range(4):
                        OP("vector", lambda e, b=b: e.tensor_scalar(out=sel[:, b, :], in0=gmv[:, b, :], scalar1=top[:, b, 2:3], scalar2=None,
                                                                    op0=ALU.is_ge), rd=[selr], wr=[selr])
                    first = [True] * 4
                    for n in range(0, j0 + 4):
                        ob_ = nxt("po", 2)
                        slots = [ob_ * 4 + b for b in range(4)]
                        pss = []
                        for w in range(2):
                            ps_, kp = scores(T, h, 2 * n + w, False, 128 ** -0.5)
                            for b in range(4):
                                if j0 + b == n:
                                    mask128(ps_, b, MB0 + (n % 2) * 2 + w)
                            pss.append((ps_, kp))
                        calls, wr = [], []
                        for b in range(4):
                            if j0 + b >= n:
                                for w in range(2):
                                    ps_, kp = pss[w]
                                    calls.append(mm(po8[:, slots[b], 0:129], pt[:, ps_, b * 128:(b + 1) * 128], Vaug[:, kp, h, :], w == 0, w == 1))
                                wr.append(por[slots[b]])
                        MM(calls, rd=[ptreg[pss[0][0]], ptreg[pss[1][0]], areg["V"]], wr=wr)
                        for b in range(4):
                            j = j0 + b
                            if j < n:
                                continue
                            src = po8[:, slots[b], 0:129]
                            dst = accC[:, b, 0:129]
                            if j == n:
                                if first[b]:
                                    OP("vector", lambda e, src=src, dst=dst: e.tensor_copy(out=dst, in_=src), rd=[por[slots[b]]], wr=[accr])
                                else:
                                    OP("vector", lambda e, src=src, dst=dst: e.tensor_tensor(out=dst, in0=src, in1=dst, op=ALU.add),
                                       rd=[por[slots[b]]], wr=[accr])
                            else:
                                if first[b]:
                                    OP("vector", lambda e, src=src, dst=dst, b=b, n=n: e.tensor_scalar(
                                        out=dst, in0=src, scalar1=sel[:, b, n:n + 1], scalar2=None, op0=ALU.mult),
                                       rd=[por[slots[b]], selr], wr=[accr])
                                else:
                                    OP("vector", lambda e, src=src, dst=dst, b=b, n=n: e.scalar_tensor_tensor(
                                        out=dst, in0=src, scalar=sel[:, b, n:n + 1], in1=dst, op0=ALU.mult, op1=ALU.add),
                                       rd=[por[slots[b]], selr], wr=[accr])
                            first[b] = False
                    normalize_po(None, T, h, src4=accC, srcregs=[accr])
                epilogue(2, T)

            load_ctx_AC(2)
            DMA("sync", "ctxr", [(krT[0:64, rr * 1024:(rr + 1) * 1024], ob[3][rr * 320:rr * 320 + 64, :]) for rr in range(2)],
                wr=[areg["krT"]], deps=[ccoll[3]])
            s0 = WS.next("Bcq0")
            s1 = WS.next("Bcq1")
            for j in range(NB):
                bk = nxt("pp", 2)
                calls = [mm(P[:, bk, 0:256], xT[:, k, j * 128:(j + 1) * 128], wv(s0)[:, k, 0:256], k == 0, k == 15) for k in range(16)]
                calls += [mm(P[:, bk, 256:384], xT[:, k, j * 128:(j + 1) * 128], wv(s1)[:, k, 0:128], k == 0, k == 15) for k in range(16)]
                MM(calls, rd=[wreg[s0], wreg[s1], xTreg], wr=[pb[bk]])
                rmsnorm_rows(bk, 384, cqb[:, 0:384], [reg("cqb")])
                transposes_to(lambda c, j=j: cqnT[:, c, j * 128:(j + 1) * 128], lambda c: cqb[:, c * 128:(c + 1) * 128], 3,
                              [reg("cqb")], [areg["cqnT"]], scale_fn=lambda c: cols[:, 21 * l + c:21 * l + 1 + c])
            s = WS.next("WUQ")
            wq = wbuf[:, s, 0:2304].rearrange("p (k h c) -> p k h c", k=3, h=4)
            for h in range(4):
                for T in range(2):
                    bk = nxt("pp", 2)
                    MM([mm(P[:, bk, :], wq[:, k, h, 0:128], cqnT[:, k, T * 512:(T + 1) * 512], k == 0, k == 2) for k in range(3)],
                       rd=[wreg[s], areg["cqnT"]], wr=[pb[bk]])
                    OP("scalar", lambda e, bk=bk, h=h, T=T: e.activation(out=qT[:, h, T * 512:(T + 1) * 512], in_=P[:, bk, :], func=AF.Copy),
                       rd=[pb[bk]], wr=[areg["qT"]])
                    bk = nxt("pp", 2)
                    MM([mm(P[0:64, bk, :], wq[:, k, h, 128:192], cqnT[:, k, T * 512:(T + 1) * 512], k == 0, k == 2) for k in range(3)],
                       rd=[wreg[s], areg["cqnT"]], wr=[pb[bk]])
                    rope(bk, 64, qrT[0:64, h, T * 512:(T + 1) * 512], [areg["qrT"]], 32, T)
            for T in range(2):
                for h in range(4):
                    attn_dense(T, h, False, True, 192 ** -0.5)
                epilogue(1, T)

            kTD = av(0, 2048)
            items = [(kTD[:, rr * 1024:(rr + 1) * 1024], ob[3][rr * 320 + 64:rr * 320 + 192, :]) for rr in range(2)]
            DMA("sync", "ctxk", items, wr=[areg["kT"]], deps=[ccoll[3]])
            items = []
            for rr in range(2):
                src = ob[3][rr * 320 + 192:rr * 320 + 320, :].rearrange("r (e c) -> (r e) c", e=8)
                for hh in range(2):
                    items.append((VD[:, rr * 8:(rr + 1) * 8, hh, 0:64], src[:, hh * 64:(hh + 1) * 64].rearrange("(jj p) c -> p jj c", p=128)))
            DMA("sync", "ctxv", items, wr=[areg["V"]], deps=[ccoll[3]])
            OP("vector", lambda e: e.memset(VD[:, :, :, 64:65], 1.0), wr=[areg["V"]])
            for g in range(2):
                s = WS.next(f"Dq{g}")
                for cc in range(2):
                    c = 2 * g + cc
                    for T in range(2):
                        bk = proj_fm(s, cc * 128, 128, T)
                        rope(bk, 128, qT[:, c, T * 512:(T + 1) * 512], [areg["qT"]], 32, T)
            r = reg("sm")
            for T in range(2):
                for b in range(4):
                    j = 4 * T + b
                    Gs = [G for G in (2 * j - 1, 2 * j, 2 * j + 1) if G >= 0]
                    w0 = 3 - len(Gs)
                    for c in range(4):
                        for sl in range(2):
                            hd = c + 4 * sl
                            prt = slice(64 * sl, 64 * sl + 64)
                            sbk = 2 + nxt("ps", 2)
                            MM([mm(P[:, sbk, (w0 + i) * 128:(w0 + i + 1) * 128], kTD[prt, kap(G) * 128:(kap(G) + 1) * 128],
                                   qT[prt, c, j * 128:(j + 1) * 128], True, True) for i, G in enumerate(Gs)],
                               rd=[areg["kT"], areg["qT"]], wr=[pb[sbk]])
                            ps_ = nxt("pt", 3)
                            OP("scalar", lambda e, sbk=sbk, ps_=ps_, w0=w0: e.activation(out=pt[:, ps_, w0 * 128:384], in_=P[:, sbk, w0 * 128:384],
                                                                                 func=AF.Exp, scale=0.125), rd=[pb[sbk]], wr=[ptreg[ps_]])
                            OP("vector", lambda e, ps_=ps_, w0=w0, j=j: e.tensor_tensor(
                                out=pt[:, ps_, w0 * 128:384], in0=pt[:, ps_, w0 * 128:384],
                                in1=masks[:, MD0 + (j % 2) * 3 + w0:MD0 + (j % 2) * 3 + 3, :].rearrange("p a b -> p (a b)"), op=ALU.mult),
                               rd=[ptreg[ps_], creg], wr=[ptreg[ps_]])
                            slot = nxt("po8", 8)
                            MM([mm(po8[:, slot, 0:65], pt[:, ps_, (w0 + i) * 128:(w0 + i + 1) * 128], VD[:, kap(G), sl, :], i == 0, i == len(Gs) - 1)
                                for i, G in enumerate(Gs)], rd=[ptreg[ps_], areg["V"]], wr=[por[slot]])
                            u = 160 + (hd % 8)
                            OP("vector", lambda e, slot=slot, u=u, hd=hd, l=l: e.tensor_tensor(out=sm[:, u:u + 1], in0=po8[:, slot, 64:65],
                                                                                       in1=esink[:, 8 * l + hd:8 * l + hd + 1], op=ALU.add),
                               rd=[por[slot], reg("esink")], wr=[r])
                            OP("vector", lambda e, u=u: e.reciprocal(out=sm[:, u + 8:u + 9], in_=sm[:, u:u + 1]), rd=[r], wr=[r])
                            OP("scalar", lambda e, slot=slot, u=u, hd=hd, b=b: e.activation(out=ytile[:, b, hd * 64:(hd + 1) * 64], in_=po8[:, slot, 0:64],
                                                                                         func=AF.Copy, scale=sm[:, u + 8:u + 9]),
                               rd=[por[slot], r], wr=[reg("ytile")])
                epilogue(3, T)

            S.barrier()
            DMA("gpsimd", "wo", [(wo[:, :, q * 512:(q + 1) * 512], w_out[l, :, q * 512:(q + 1) * 512].rearrange("(k p) c -> p k c", p=128))
                                 for q in range(4)], wr=[reg("wo")])
            DMA("sync", "gb", [(gam, ln_g[l:l + 1, :].partition_broadcast(128)[:, 0, :]),
                               (bet, ln_b[l:l + 1, :].partition_broadcast(128)[:, 0, :])], wr=[wreg[0], wreg[1]])
            xsrc = x_in if l == 0 else xs[(l - 1) % 2]
            xdst = out_d if l == depth - 1 else xs[l % 2]
            zreg = reg("ytile")
            r = reg("sm")
            for j in range(NB):
                DMA("sync", "xr", [(xr, xsrc[j * 128:(j + 1) * 128, :])], wr=[wreg[2]])
                for q in range(4):
                    MM([mm(P[:, 4 + q, :], yT[:, k, j * 128:(j + 1) * 128], wo[:, k, q * 512:(q + 1) * 512], k == 0, k == 15) for k in range(16)],
                       rd=[reg("wo"), reg("yT")], wr=[por[2 * q], por[2 * q + 1]])
                for q in range(4):
                    OP("vector", lambda e, q=q: e.scalar_tensor_tensor(out=zt[:, q * 512:(q + 1) * 512], in0=xr[:, q * 512:(q + 1) * 512], scalar=ALPHA,
                                                                      in1=P[:, 4 + q, :], op0=ALU.mult, op1=ALU.add),
                       rd=[wreg[2], por[2 * q], por[2 * q + 1]], wr=[zreg])
                    OP("vector", lambda e, q=q: e.bn_stats(out=sm[:, 176 + 6 * q:182 + 6 * q], in_=zt[:, q * 512:(q + 1) * 512]), rd=[zreg], wr=[r])
                OP("vector", lambda e: e.bn_aggr(out=sm[:, 200:202], in_=sm[:, 176:200]), rd=[r], wr=[r])
                OP("vector", lambda e: e.tensor_scalar(out=sm[:, 202:203], in0=sm[:, 201:202], scalar1=1e-5, scalar2=None, op0=ALU.add), rd=[r], wr=[r])
                OP("scalar", lambda e: e.activation(out=sm[:, 203:204], in_=sm[:, 202:203], func=AF.Ln), rd=[r], wr=[r])
                OP("scalar", lambda e: e.activation(out=sm[:, 204:205], in_=sm[:, 203:204], func=AF.Exp, scale=-0.5), rd=[r], wr=[r])
                OP("vector", lambda e: e.tensor_scalar(out=zt, in0=zt, scalar1=sm[:, 200:201], scalar2=sm[:, 204:205], op0=ALU.subtract, op1=ALU.mult),
                   rd=[r, zreg], wr=[zreg])
                OP("vector", lambda e: e.tensor_tensor(out=zt, in0=zt, in1=gam, op=ALU.mult), rd=[zreg, wreg[0]], wr=[zreg])
                OP("vector", lambda e: e.tensor_tensor(out=zt, in0=zt, in1=bet, op=ALU.add), rd=[zreg, wreg[1]], wr=[zreg])
                DMA("sync", "xo", [(xdst[j * 128:(j + 1) * 128, :], zt)], rd=[zreg])
                if l < depth - 1:
                    block_to_xT(zt, zreg, j)
            S.barrier()

        S.emit()
    return nc


def _mult(delta):
    m = np.zeros_like(delta, dtype=np.float32)
    ok = delta >= 0
    m += (ok & (delta <= 128))
    m += (ok & (delta % 4 == 0) & (delta <= 512))
    m += (ok & (delta % 16 == 0) & (delta <= 2048))
    return m


def host_tables(r):
    i = np.arange(128)[None, :]
    k = np.arange(128)[:, None]
    masks = np.zeros((128, 44, 128), np.float32)
    for jpar in range(2):
        e = (jpar + r) % 2
        for t in range(17):
            masks[:, t * 2 + jpar, :] = _mult((t - 1 + e) * 128 + i - k)
        for w in range(2):
            masks[:, 34 + jpar * 2 + w, :] = (((e - w) * 128 + i - k) >= 0)
        for w in range(3):
            dd = (e + 1 - w) * 128 + i - k
            masks[:, 38 + jpar * 3 + w, :] = ((dd >= 0) & (dd <= 127))
    pos = np.concatenate([gblk(r, j) * 128 + np.arange(128) for j in range(NB)]).astype(np.float32)
    tabs = np.zeros((128, 4, 1024), np.float32)
    p = np.arange(128)
    inv128 = (np.float32(10000.0) ** (-np.arange(0, 128, 2, dtype=np.float32) / np.float32(128))).astype(np.float32)
    inv64 = (np.float32(10000.0) ** (-np.arange(0, 64, 2, dtype=np.float32) / np.float32(64))).astype(np.float32)
    a128 = (pos[None, :] * inv128[p % 64][:, None]).astype(np.float32)
    a64 = (pos[None, :] * inv64[(p % 64) % 32][:, None]).astype(np.float32)
    tabs[:, 0] = np.cos(a128)
    tabs[:, 1] = np.sin(a128) * np.where(p < 64, -1.0, 1.0)[:, None]
    tabs[:, 2] = np.cos(a64)
    tabs[:, 3] = np.sin(a64) * np.where((p % 64) < 32, -1.0, 1.0)[:, None]
    return masks.astype(ml_dtypes.bfloat16), tabs


def host_inputs(x, w_in, q_norm, w_uq, kv_norm, w_ukv, sinks, branch_norm, w_out, ln_gamma, ln_beta):
    f = lambda a: np.ascontiguousarray(np.asarray(a, dtype=np.float32))
    x, w_in, w_uq, w_ukv, w_out = f(x), f(w_in), f(w_uq), f(w_ukv), f(w_out)
    q_norm, kv_norm, sinks, branch_norm, ln_gamma, ln_beta = f(q_norm), f(kv_norm), f(sinks), f(branch_norm), f(ln_gamma), f(ln_beta)
    cols = np.zeros((128, 84), np.float32)
    for l in range(DEPTH):
        cols[:, 21 * l:21 * l + 3] = q_norm[l].reshape(3, 128).T
        cols[:, 21 * l + 3:21 * l + 5] = kv_norm[l].reshape(2, 128).T
        cols[:, 21 * l + 5:21 * l + 21] = branch_norm[l].reshape(16, 128).T
    sinks_bc = np.ascontiguousarray(np.broadcast_to(sinks.reshape(1, 32), (128, 32)))
    negm = np.zeros((128, 8, 8), np.float32)
    for j in range(8):
        negm[:, j, j:] = NEG
    negm = negm.reshape(128, 64)
    ident = np.eye(128, dtype=np.float32).astype(ml_dtypes.bfloat16)
    tb = [host_tables(r) for r in range(2)]
    in_maps = []
    for c in range(8):
        b, r = c // 2, c % 2
        xo = np.concatenate([x[b, gblk(r, j) * 128:(gblk(r, j) + 1) * 128, :] for j in range(NB)], 0)
        in_maps.append({"x": np.ascontiguousarray(xo), "w_in": w_in, "w_uq": w_uq, "w_ukv": w_ukv, "w_out": w_out,
                        "ln_gamma": ln_gamma, "ln_beta": ln_beta, "cols": cols, "sinks_bc": sinks_bc, "negm": negm,
                        "tabs": tb[r][1], "masks": tb[r][0], "ident": ident})
    return in_maps


def assemble(results, key="out"):
    out = np.zeros((4, 2048, D), np.float32)
    for c in range(8):
        b, r = c // 2, c % 2
        o = np.asarray(results[c][key])
        for j in range(NB):
            g = gblk(r, j)
            out[b, g * 128:(g + 1) * 128, :] = o[j * 128:(j + 1) * 128, :]
    return out


_NC = {}


def kernel(x, w_in, q_norm, w_uq, kv_norm, w_ukv, sinks, branch_norm, w_out, ln_gamma, ln_beta):
    in_maps = host_inputs(x, w_in, q_norm, w_uq, kv_norm, w_ukv, sinks, branch_norm, w_out, ln_gamma, ln_beta)
    if "nc" not in _NC:
        _NC["nc"] = build(DEPTH, False)
    res = run_bass_kernel_spmd(_NC["nc"], in_maps, core_ids=list(range(8)))
    return assemble(res.results)
```

```python
import contextlib
import numpy as np
import ml_dtypes
import concourse.bass as bass
import concourse.mybir as mybir
from concourse.bass_utils import run_bass_kernel_spmd

F32 = mybir.dt.float32
BF16 = mybir.dt.bfloat16
AF = mybir.ActivationFunctionType
ALU = mybir.AluOpType
AX = mybir.AxisListType

D = 2048
NIN = 6592
NOWN = 1024
NB = 8
DEPTH = 4
ALPHA = (2 * DEPTH) ** 0.25
O_AQ, O_AK, O_AV = 0, 512, 1024
O_BCQ, O_BCKV, O_BKR = 1536, 1920, 2176
O_CQ, O_CK, O_CV = 2240, 2752, 3264
O_DQ, O_DK, O_DV = 3776, 4288, 4416
O_G = 4544
NEG = -1e30
PAIRS = [[0, 1], [2, 3], [4, 5], [6, 7]]


def gblk(r, j):
    return 2 * j + (j + r) % 2


def kap(G):
    jj = G // 2
    rr = (G % 2 - jj) % 2
    return 8 * rr + jj


class Sched:
    ENGS = ("sync", "scalar", "vector", "gpsimd", "tensor")

    def __init__(self, nc):
        self.nc = nc
        self.ops = {e: [] for e in self.ENGS}
        self.sems = {}
        self.cnt = {}
        self.mult = {}
        self.waited = {e: {} for e in self.ENGS}
        self._cms = []

    def sem(self, key, mult=1):
        if key not in self.sems:
            cm = self.nc.semaphore(key)
            self.sems[key] = cm.__enter__()
            self._cms.append(cm)
            self.cnt[key] = 0
            self.mult[key] = mult
        return self.sems[key]

    def wait(self, eng, toks):
        for t in toks:
            if t is None:
                continue
            key, n = t
            w = self.waited[eng]
            if w.get(key, 0) >= n:
                continue
            w[key] = n
            h = self.sems[key]
            self.ops[eng].append(lambda e, h=h, v=n * self.mult[key]: e.wait_ge(h, v))

    def op(self, eng, fn, deps=(), sig=True, key=None):
        self.wait(eng, deps)
        if sig:
            key = key or ("c_" + eng)
            h = self.sem(key)
            self.cnt[key] += 1
            self.ops[eng].append(lambda e, fn=fn, h=h: fn(e).then_inc(h, 1))
            return (key, self.cnt[key])
        self.ops[eng].append(lambda e, fn=fn: fn(e))
        return None

    def dma(self, eng, chan, items, deps=()):
        key = "d_" + chan
        h = self.sem(key, 16)
        if self.cnt[key]:
            self.wait(eng, [(key, self.cnt[key])])
        self.wait(eng, deps)
        for (o, i, kw) in items:
            self.cnt[key] += 1
            self.ops[eng].append(lambda e, o=o, i=i, kw=kw, h=h: e.dma_start(out=o, in_=i, **kw).then_inc(h, 16))
        return (key, self.cnt[key])

    def barrier(self):
        toks = [(k, c) for k, c in self.cnt.items() if c > 0]
        for e in self.ENGS:
            self.wait(e, toks)

    def emit(self):
        self.barrier()
        with self.nc.Block() as block:
            @block.sync
            def _(e):
                for f in self.ops["sync"]:
                    f(e)

            @block.scalar
            def _(e):
                for f in self.ops["scalar"]:
                    f(e)

            @block.vector
            def _(e):
                for f in self.ops["vector"]:
                    f(e)

            @block.gpsimd
            def _(e):
                for f in self.ops["gpsimd"]:
                    f(e)

            @block.tensor
            def _(e):
                for f in self.ops["tensor"]:
                    f(e)
        for cm in reversed(self._cms):
            cm.__exit__(None, None, None)


class Reg:
    def __init__(self):
        self.w = None
        self.r = {}

    def rdeps(self):
        return [self.w] if self.w else []

    def wdeps(self):
        return ([self.w] if self.w else []) + list(self.r.values())

    def read(self, tok):
        if tok is None:
            return
        k = tok[0]
        if k not in self.r or self.r[k][1] < tok[1]:
            self.r[k] = tok

    def write(self, tok):
        self.w = tok
        self.r = {}


def build(depth=DEPTH, debug=False):
    nc = bass.Bass("TRN2", target_bir_lowering=False)
    S = Sched(nc)
    dt_in = lambda name, shape, dt=F32: nc.dram_tensor(name, shape, dt, kind="ExternalInput")
    x_in = dt_in("x", [NOWN, D])
    w_in = dt_in("w_in", [DEPTH, D, NIN])
    w_uq = dt_in("w_uq", [DEPTH, 384, 768])
    w_ukv = dt_in("w_ukv", [DEPTH, 256, 1024])
    w_out = dt_in("w_out", [DEPTH, D, D])
    ln_g = dt_in("ln_gamma", [DEPTH, D])
    ln_b = dt_in("ln_beta", [DEPTH, D])
    cols_d = dt_in("cols", [128, 84])
    sinks_d = dt_in("sinks_bc", [128, 32])
    negm_d = dt_in("negm", [128, 64])
    tabs_d = dt_in("tabs", [128, 4, 1024])
    masks_d = dt_in("masks", [128, 44, 128], BF16)
    ident_d = dt_in("ident", [128, 128], BF16)
    out_d = nc.dram_tensor("out", [NOWN, D], F32, kind="ExternalOutput")
    if debug:
        ydbg = nc.dram_tensor("ydbg", [NOWN, D], F32, kind="ExternalOutput")
    xs = [nc.dram_tensor("xs0", [NOWN, D], F32), nc.dram_tensor("xs1", [NOWN, D], F32)]
    ibs = [[nc.dram_tensor(f"ib{l}_{i}", [320 if i == 3 else 1024, 1024], BF16) for i in range(4)] for l in range(depth)]
    obs = [[nc.dram_tensor(f"ob{l}_{i}", [640 if i == 3 else 2048, 1024], BF16) for i in range(4)] for l in range(depth)]

    es = contextlib.ExitStack()
    with es:
        sb = lambda name, shape, dt: es.enter_context(nc.sbuf_tensor("sb_" + name, shape, dt))
        xT = sb("xT", [128, 16, NOWN], BF16)
        yT = sb("yT", [128, 16, NOWN], BF16)
        arena = sb("arena", [128, 32768], BF16)
        wbuf = sb("wbuf", [128, 3, 4096], BF16)
        stage = sb("stage", [128, 4, 512], BF16)
        tabs = sb("tabs", [128, 4, 1024], F32)
        masks = sb("masks", [128, 44, 128], BF16)
        ident = sb("ident", [128, 128], BF16)
        pt = sb("pt", [128, 4, 512], BF16)
        ytile = sb("ytile", [128, 4, 512], F32)
        rt1 = sb("rt1", [128, 512], F32)
        rt2 = sb("rt2", [128, 512], F32)
        cols = sb("cols", [128, 84], F32)
        esink = sb("esink", [128, 32], F32)
        negm = sb("negm", [128, 8, 8], F32)
        sm = sb("sm", [128, 256], F32)
        accC = sb("accC", [128, 4, 132], F32)
        sg = sb("sg", [128, 2, 256], F32)
        ygb = sb("ygb", [128, 2, 256], BF16)
        cqb = sb("cqb", [128, 384], BF16)
        P = es.enter_context(nc.psum_tensor("P", [128, 8, 512], F32))

        R = {}
        def reg(name):
            if name not in R:
                R[name] = Reg()
            return R[name]
        pb = [reg(f"pb{i}") for i in range(4)]
        por = [reg(f"po{i}") for i in range(8)]
        wreg = [reg(f"w{i}") for i in range(3)]
        streg = [reg(f"st{i}") for i in range(4)]
        ptreg = [reg(f"pt{i}") for i in range(4)]

        po8 = P[:, 4:8, :].rearrange("p k (b c) -> p (k b) c", c=256)

        def av(off, n):
            return arena[:, off:off + n]
        kT = av(0, 8192).rearrange("p (h t) -> p h t", h=4)
        Vaug = av(8192, 8256).rearrange("p (k h c) -> p k h c", k=16, h=4)
        VD = av(8192, 2080).rearrange("p (k h c) -> p k h c", k=16, h=2)
        qT = av(16448, 4096).rearrange("p (h t) -> p h t", h=4)
        qrT = av(20544, 4096).rearrange("p (h t) -> p h t", h=4)
        krT = av(24640, 2048)
        cqnT = av(26688, 3072).rearrange("p (k t) -> p k t", k=3)
        kmean = av(29760, 32).rearrange("p (h n) -> p h n", h=4)
        ckvnT = av(29792, 2048).rearrange("p (k t) -> p k t", k=2)
        wo = arena[:, :].rearrange("p (k c) -> p k c", k=16)
        gam = wbuf[:, 0, :].bitcast(F32)
        bet = wbuf[:, 1, :].bitcast(F32)
        xr = wbuf[:, 2, :].bitcast(F32)
        zt = ytile[:, :, :].rearrange("p a b -> p (a b)")
        xb = stage[:, :, :].rearrange("p a b -> p (a b)")

        def OP(eng, fn, rd=(), wr=(), deps=()):
            d = list(deps)
            for t in rd:
                d.extend(t.rdeps())
            for t in wr:
                d.extend(t.wdeps())
            tok = S.op(eng, fn, d)
            for t in rd:
                t.read(tok)
            for t in wr:
                t.write(tok)
            return tok

        def DMA(eng, chan, items, rd=(), wr=(), deps=()):
            d = list(deps)
            for t in rd:
                d.extend(t.rdeps())
            for t in wr:
                d.extend(t.wdeps())
            tok = S.dma(eng, chan, [(o, i, {}) for (o, i) in items], d)
            for t in rd:
                t.read(tok)
            for t in wr:
                t.write(tok)
            return tok

        def MM(calls, rd=(), wr=()):
            d = []
            for t in rd:
                d.extend(t.rdeps())
            for t in wr:
                d.extend(t.wdeps())
            S.wait("tensor", d)
            for f in calls[:-1]:
                S.op("tensor", f, sig=False)
            tok = S.op("tensor", calls[-1])
            for t in rd:
                t.read(tok)
            for t in wr:
                t.write(tok)
            return tok

        def mm(out, lhsT, rhs, start, stop):
            return lambda e: e.matmul(out, lhsT=lhsT, rhs=rhs, start=start, stop=stop)

        creg = reg("const")
        DMA("sync", "const", [(tabs[:], tabs_d[:, :, :]), (masks[:], masks_d[:, :, :]), (ident[:], ident_d[:, :]),
                              (cols[:], cols_d[:, :]), (esink[:], sinks_d[:, :]),
                              (negm[:].rearrange("p a b -> p (a b)"), negm_d[:, :])], wr=[creg])
        OP("scalar", lambda e: e.activation(out=esink[:], in_=esink[:], func=AF.Exp), rd=[creg], wr=[reg("esink")])
        S.barrier()
        cos128, ssin128, cos64, ssin64 = (tabs[:, i, :] for i in range(4))
        TA0 = 0
        MB0 = 34
        MD0 = 38

        wstate = {"i": 0}

        def wload(items, name):
            s = wstate["i"] % 3
            wstate["i"] += 1
            DMA("gpsimd", f"w{s}", items(s), wr=[wreg[s]])
            return s

        def wv(s):
            return wbuf[:, s, :].rearrange("p (k c) -> p k c", c=256)

        def std_items(l, off, n):
            return lambda s: [(wv(s)[:, :, 0:n], w_in[l, :, off:off + n].rearrange("(k p) c -> p k c", p=128))]

        class WQ:
            def __init__(self, specs):
                self.specs = specs
                self.issued = 0
                self.slots = []
                self.pos = 0

            def _issue(self):
                name, items = self.specs[self.issued]
                self.slots.append(wload(items, name))
                self.issued += 1

            def next(self, name):
                assert self.specs[self.pos][0] == name, (self.specs[self.pos][0], name)
                while self.issued < min(len(self.specs), self.pos + 2):
                    self._issue()
                s = self.slots[self.pos]
                self.pos += 1
                return s

            def prefetch(self):
                while self.issued < min(len(self.specs), self.pos + 2):
                    self._issue()

        def layer_specs(l):
            sp = []
            for nm, off in (("Ak", O_AK), ("Av", O_AV), ("Ck", O_CK), ("Cv", O_CV)):
                for g in range(2):
                    sp.append((f"{nm}{g}", std_items(l, off + 256 * g, 256)))
            sp.append(("Bkv", std_items(l, O_BCKV, 256)))
            sp.append(("Bkr", std_items(l, O_BKR, 64)))
            sp.append(("WUKV", lambda s: [(wbuf[:, s, 0:2048].rearrange("p (k c) -> p k c", k=2),
                                           w_ukv[l, :, :].rearrange("(k p) c -> p k c", p=128))]))
            sp.append(("Dkv", std_items(l, O_DK, 256)))

            def gates(m):
                r = []
                for T in range(2):
                    for g in range(2):
                        r.append((f"G{m}{g}", std_items(l, O_G + 512 * m + 256 * g, 256)))
                return r
            sp += [("Aq0", std_items(l, O_AQ, 256)), ("Aq1", std_items(l, O_AQ + 256, 256))] + gates(0)
            sp += [("Cq0", std_items(l, O_CQ, 256)), ("Cq1", std_items(l, O_CQ + 256, 256))] + gates(2)
            sp += [("Bcq0", std_items(l, O_BCQ, 256)), ("Bcq1", std_items(l, O_BCQ + 256, 128)),
                   ("WUQ", lambda s: [(wbuf[:, s, 0:2304].rearrange("p (k c) -> p k c", k=3),
                                       w_uq[l, :, :].rearrange("(k p) c -> p k c", p=128))])] + gates(1)

            def dq_items(cp):
                def f(s):
                    it = []
                    for i in range(2):
                        c = 2 * cp + i
                        it.append((wv(s)[:, :, 128 * i:128 * i + 64],
                                   w_in[l, :, O_DQ + 64 * c:O_DQ + 64 * c + 64].rearrange("(k p) c -> p k c", p=128)))
                        it.append((wv(s)[:, :, 128 * i + 64:128 * i + 128],
                                   w_in[l, :, O_DQ + 64 * (4 + c):O_DQ + 64 * (4 + c) + 64].rearrange("(k p) c -> p k c", p=128)))
                    return it
                return f
            sp += [("Dq0", dq_items(0)), ("Dq1", dq_items(1))] + gates(3)
            return sp

        ctr = {"pp": 0, "ps": 0, "pt": 0, "st": 0, "po": 0, "sgq": 0, "po8": 0}

        def nxt(k, n):
            v = ctr[k] % n
            ctr[k] += 1
            return v

        xTreg = reg("xT")

        def proj_fm(s, c0, ncol, T):
            b = nxt("pp", 2)
            calls = [mm(P[0:ncol, b, :], wv(s)[:, k, c0:c0 + ncol], xT[:, k, T * 512:(T + 1) * 512], k == 0, k == 15)
                     for k in range(16)]
            MM(calls, rd=[wreg[s], xTreg], wr=[pb[b]])
            return b

        def proj_tm(s, j, ncol):
            b = nxt("pp", 2)
            calls = [mm(P[:, b, 0:ncol], xT[:, k, j * 128:(j + 1) * 128], wv(s)[:, k, 0:ncol], k == 0, k == 15)
                     for k in range(16)]
            MM(calls, rd=[wreg[s], xTreg], wr=[pb[b]])
            return b

        rtreg = reg("rt")

        def rope(src_bank, np_, dst, dstregs, half, T, n=512):
            ct, stb = (cos128, ssin128) if half == 64 else (cos64, ssin64)
            src = P[:, src_bank, 0:n]
            tsl = slice(T * 512, T * 512 + n)
            OP("vector", lambda e: e.tensor_tensor(out=rt1[0:np_, 0:n], in0=src[0:np_, :], in1=ct[0:np_, tsl], op=ALU.mult),
               rd=[pb[src_bank]], wr=[rtreg])
            for base in range(0, np_, 2 * half):
                lo, mid, hi = base, base + half, base + 2 * half
                OP("vector", lambda e, lo=lo, mid=mid, hi=hi: e.tensor_tensor(
                    out=rt2[lo:mid, 0:n], in0=src[mid:hi, :], in1=stb[lo:mid, tsl], op=ALU.mult), rd=[pb[src_bank]], wr=[rtreg])
                OP("vector", lambda e, lo=lo, mid=mid, hi=hi: e.tensor_tensor(
                    out=rt2[mid:hi, 0:n], in0=src[lo:mid, :], in1=stb[mid:hi, tsl], op=ALU.mult), rd=[pb[src_bank]], wr=[rtreg])
            OP("vector", lambda e: e.tensor_tensor(out=dst, in0=rt1[0:np_, 0:n], in1=rt2[0:np_, 0:n], op=ALU.add),
               rd=[rtreg], wr=dstregs)

        def transposes_to(dst_fn, src_fn, nchunk, srcregs, dstregs, scale_fn=None, np_out=128):
            c = 0
            while c < nchunk:
                g = min(8, nchunk - c)
                b = nxt("pp", 2)
                pbv = P[:, b, :].bitcast(BF16)
                calls = []
                for i in range(g):
                    calls.append((lambda e, o=pbv[:, i * 128:(i + 1) * 128], s_=src_fn(c + i): e.transpose(o, s_, ident[:])))
                MM(calls, rd=srcregs + [reg("const")], wr=[pb[b]])
                for i in range(g):
                    d_ = dst_fn(c + i)
                    i_ = pbv[0:np_out, i * 128:(i + 1) * 128]
                    if scale_fn is None:
                        OP("scalar", lambda e, d_=d_, i_=i_: e.activation(out=d_, in_=i_, func=AF.Copy), rd=[pb[b]], wr=dstregs)
                    else:
                        OP("scalar", lambda e, d_=d_, i_=i_, sc=scale_fn(c + i): e.activation(out=d_, in_=i_, func=AF.Copy, scale=sc),
                           rd=[pb[b]], wr=dstregs)
                c += g

        def block_to_xT(src_f32, srcreg, j):
            OP("scalar", lambda e: e.activation(out=xb, in_=src_f32, func=AF.Copy), rd=[srcreg], wr=streg)
            transposes_to(lambda c: xT[:, c, j * 128:(j + 1) * 128], lambda c: xb[:, c * 128:(c + 1) * 128], 16,
                          list(streg), [xTreg])

        def stage_out(src_ap_fn, srcregs, dst_dram, eng="scalar"):
            s = nxt("st", 4)
            src, shape_p, shape_n = src_ap_fn
            OP(eng, lambda e: e.activation(out=stage[0:shape_p, s, 0:shape_n], in_=src, func=AF.Copy) if eng == "scalar"
               else e.tensor_copy(out=stage[0:shape_p, s, 0:shape_n], in_=src), rd=srcregs, wr=[streg[s]])
            DMA("sync", f"st{s}", [(dst_dram, stage[0:shape_p, s, 0:shape_n])], rd=[streg[s]])

        def rmsnorm_rows(bank, ncol, dst_bf, dstregs, eps=1e-6):
            r = reg("sm")
            OP("vector", lambda e: e.memset(sm[:, 0:1], 0.0), wr=[r])
            OP("scalar", lambda e: e.activation(out=rt1[:, 0:ncol], in_=P[:, bank, 0:ncol], func=AF.Square, accum_out=sm[:, 0:1]),
               rd=[pb[bank]], wr=[r, rtreg])
            OP("vector", lambda e: e.tensor_scalar(out=sm[:, 1:2], in0=sm[:, 0:1], scalar1=1.0 / ncol, scalar2=eps, op0=ALU.mult, op1=ALU.add),
               rd=[r], wr=[r])
            OP("scalar", lambda e: e.activation(out=sm[:, 2:3], in_=sm[:, 1:2], func=AF.Ln), rd=[r], wr=[r])
            OP("scalar", lambda e: e.activation(out=sm[:, 3:4], in_=sm[:, 2:3], func=AF.Exp, scale=-0.5), rd=[r], wr=[r])
            OP("scalar", lambda e: e.activation(out=dst_bf, in_=P[:, bank, 0:ncol], func=AF.Copy, scale=sm[:, 3:4]),
               rd=[r, pb[bank]], wr=dstregs)

        xrreg = wreg[2]
        for j in range(NB):
            DMA("sync", "xr", [(xr, x_in[j * 128:(j + 1) * 128, :])], wr=[xrreg])
            block_to_xT(xr, xrreg, j)
        S.barrier()

        for l in range(depth):
            WS = WQ(layer_specs(l))
            ib, ob = ibs[l], obs[l]
            ccoll = [None] * 4
            areg = {n: reg(f"a_{n}") for n in ("kT", "V", "qT", "qrT", "krT", "cqnT", "kmean", "ckvnT")}

            def collective(i, deps):
                S.wait("gpsimd", deps)
                key = f"cc{i}"
                S.sem(key)
                tok = S.op("gpsimd", lambda e, ii=ib[i], oo=ob[i]: e.collective_compute(
                    "AllGather", ALU.bypass, replica_groups=PAIRS, ins=[ii.ap().opt()], outs=[oo.ap().opt()]), key=key)
                ccoll[i] = tok

            def kside_AC(nm, ci):
                toks = []
                for g in range(2):
                    s = WS.next(f"{nm}k{g}")
                    for hh in range(2):
                        h = 2 * g + hh
                        for T in range(2):
                            b = proj_fm(s, hh * 128, 128, T)
                            st = nxt("st", 4)
                            rope(b, 128, stage[:, st, :], [streg[st]], 64, T)
                            toks.append(DMA("sync", f"st{st}", [(ib[ci][h * 128:(h + 1) * 128, T * 512:(T + 1) * 512], stage[:, st, :])],
                                            rd=[streg[st]]))
                vview = ib[ci][512:1024, :].rearrange("r (two c) -> (r two) c", two=2)
                for g in range(2):
                    s = WS.next(f"{nm}v{g}")
                    for j in range(NB):
                        b = proj_tm(s, j, 256)
                        st = nxt("st", 4)
                        OP("scalar", lambda e, b=b, st=st: e.activation(out=stage[:, st, 0:256], in_=P[:, b, 0:256], func=AF.Copy),
                           rd=[pb[b]], wr=[streg[st]])
                        toks.append(DMA("sync", f"st{st}", [(vview[j * 128:(j + 1) * 128, g * 256:(g + 1) * 256], stage[:, st, 0:256])],
                                        rd=[streg[st]]))
                collective(ci, toks)

            kside_AC("A", 0)
            kside_AC("C", 1)

            toksB, toksD = [], []
            s = WS.next("Bkv")
            for j in range(NB):
                b = proj_tm(s, j, 256)
                rmsnorm_rows(b, 256, cqb[:, 0:256], [reg("cqb")])
                transposes_to(lambda c, j=j: ckvnT[:, c, j * 128:(j + 1) * 128], lambda c: cqb[:, c * 128:(c + 1) * 128], 2,
                              [reg("cqb")], [areg["ckvnT"]], scale_fn=lambda c: cols[:, 21 * l + 3 + c:21 * l + 4 + c])
            s = WS.next("Bkr")
            for T in range(2):
                b = proj_fm(s, 0, 64, T)
                st = nxt("st", 4)
                rope(b, 64, stage[0:64, st, :], [streg[st]], 32, T)
                toksD.append(DMA("sync", f"st{st}", [(ib[3][0:64, T * 512:(T + 1) * 512], stage[0:64, st, :])], rd=[streg[st]]))
            s = WS.next("WUKV")
            wkv = wbuf[:, s, 0:2048].rearrange("p (k h c) -> p k h c", k=2, h=4)
            for h in range(4):
                for T in range(2):
                    b = nxt("pp", 2)
                    MM([mm(P[:, b, :], wkv[:, k, h, 0:128], ckvnT[:, k, T * 512:(T + 1) * 512], k == 0, k == 1) for k in range(2)],
                       rd=[wreg[s], areg["ckvnT"]], wr=[pb[b]])
                    st = nxt("st", 4)
                    OP("scalar", lambda e, b=b, st=st: e.activation(out=stage[:, st, :], in_=P[:, b, :], func=AF.Copy),
                       rd=[pb[b]], wr=[streg[st]])
                    toksB.append(DMA("sync", f"st{st}", [(ib[2][h * 128:(h + 1) * 128, T * 512:(T + 1) * 512], stage[:, st, :])],
                                     rd=[streg[st]]))
            vviewB = ib[2][512:1024, :].rearrange("r (two c) -> (r two) c", two=2)
            for j in range(NB):
                b = nxt("pp", 2)
                MM([mm(P[:, b, :].rearrange("p (h c) -> p h c", h=4), ckvnT[:, k, j * 128:(j + 1) * 128], wkv[:, k, :, 128:256], k == 0, k == 1)
                    for k in range(2)], rd=[wreg[s], areg["ckvnT"]], wr=[pb[b]])
                st = nxt("st", 4)
                OP("scalar", lambda e, b=b, st=st: e.activation(out=stage[:, st, :], in_=P[:, b, :], func=AF.Copy),
                   rd=[pb[b]], wr=[streg[st]])
                toksB.append(DMA("sync", f"st{st}", [(vviewB[j * 128:(j + 1) * 128, :], stage[:, st, :])], rd=[streg[st]]))
            collective(2, toksB)

            s = WS.next("Dkv")
            for T in range(2):
                b = proj_fm(s, 0, 128, T)
                st = nxt("st", 4)
                rope(b, 128, stage[:, st, :], [streg[st]], 32, T)
                toksD.append(DMA("sync", f"st{st}", [(ib[3][64:192, T * 512:(T + 1) * 512], stage[:, st, :])], rd=[streg[st]]))
            vviewD = ib[3][192:320, :].rearrange("r (e c) -> (r e) c", e=8)
            for j in range(NB):
                b = nxt("pp", 2)
                MM([mm(P[:, b, 0:128], xT[:, k, j * 128:(j + 1) * 128], wv(s)[:, k, 128:256], k == 0, k == 15) for k in range(16)],
                   rd=[wreg[s], xTreg], wr=[pb[b]])
                st = nxt("st", 4)
                OP("scalar", lambda e, b=b, st=st: e.activation(out=stage[:, st, 0:128], in_=P[:, b, 0:128], func=AF.Copy),
                   rd=[pb[b]], wr=[streg[st]])
                toksD.append(DMA("sync", f"st{st}", [(vviewD[j * 128:(j + 1) * 128, :], stage[:, st, 0:128])], rd=[streg[st]]))
            collective(3, toksD)

            def load_ctx_AC(ci, wait_ones=True):
                items = []
                for rr in range(2):
                    items.append((kT[:, :, rr * 1024:(rr + 1) * 1024],
                                  ob[ci][rr * 1024:rr * 1024 + 512, :].rearrange("(h d) t -> d h t", d=128)))
                DMA("sync", "ctxk", items, wr=[areg["kT"]], deps=[ccoll[ci]])
                items = []
                for rr in range(2):
                    src = ob[ci][rr * 1024 + 512:rr * 1024 + 1024, :].rearrange("r (two c) -> (r two) c", two=2)
                    for h in range(4):
                        items.append((Vaug[:, rr * 8:(rr + 1) * 8, h, 0:128],
                                      src[:, h * 128:(h + 1) * 128].rearrange("(jj p) c -> p jj c", p=128)))
                DMA("sync", "ctxv", items, wr=[areg["V"]], deps=[ccoll[ci]])
                OP("vector", lambda e: e.memset(Vaug[:, :, :, 128:129], 1.0), wr=[areg["V"]])

            def qproj_AC(nm):
                for g in range(2):
                    s = WS.next(f"{nm}q{g}")
                    for hh in range(2):
                        h = 2 * g + hh
                        for T in range(2):
                            b = proj_fm(s, hh * 128, 128, T)
                            rope(b, 128, qT[:, h, T * 512:(T + 1) * 512], [areg["qT"]], 64, T)

            smr = {k: reg("sm_" + k) for k in ("norm", "epi", "sel0", "sel1", "D", "km", "ssq")}
            ssq = sm[:, 208:240].rearrange("p (b h) -> p b h", b=4)
            yreg = reg("ytile")
            SB = (2, 3, 0, 1)

            def pipeline(items, LA):
                n = len(items)
                for i in range(n + LA):
                    if i < n:
                        items[i][0]()
                    if i >= LA:
                        items[i - LA][1]()

            def sumsq(b, col, lo, hi):
                OP("vector", lambda e: e.scalar_tensor_tensor(out=rt1[:, 0:hi - lo], in0=ytile[:, b, lo:hi], scalar=1.0, in1=ytile[:, b, lo:hi],
                                                              op0=ALU.mult, op1=ALU.mult, accum_out=ssq[:, b, col:col + 1]),
                   rd=[yreg], wr=[rtreg, smr["ssq"]])

            def normalize_po(slots, h, src4=None, srcregs=None):
                r = smr["norm"]
                for b in range(4):
                    src = po8[:, slots[b], :] if src4 is None else src4[:, b, :]
                    rg = [por[slots[b]]] if src4 is None else srcregs
                    OP("vector", lambda e, src=src, b=b: e.reciprocal(out=sm[:, 8 + b:9 + b], in_=src[:, 128:129]), rd=rg, wr=[r])
                    OP("scalar", lambda e, src=src, b=b: e.activation(out=ytile[:, b, h * 128:(h + 1) * 128], in_=src[:, 0:128],
                                                                     func=AF.Copy, scale=sm[:, 8 + b:9 + b]),
                       rd=rg + [r], wr=[yreg])
                    sumsq(b, h, h * 128, (h + 1) * 128)

            def scores(T, h, G, isB, scale):
                kp = kap(G)
                sbk = SB[nxt("ps", 4)]
                calls = [mm(P[:, sbk, :], kT[:, h, kp * 128:(kp + 1) * 128], qT[:, h, T * 512:(T + 1) * 512], True, not isB)]
                rd = [areg["kT"], areg["qT"]]
                if isB:
                    calls.append(mm(P[:, sbk, :], krT[0:64, kp * 128:(kp + 1) * 128], qrT[0:64, h, T * 512:(T + 1) * 512], False, True))
                    rd += [areg["krT"], areg["qrT"]]
                MM(calls, rd=rd, wr=[pb[sbk]])
                ps_ = nxt("pt", 4)
                OP("scalar", lambda e: e.activation(out=pt[:, ps_, :], in_=P[:, sbk, :], func=AF.Exp, scale=scale),
                   rd=[pb[sbk]], wr=[ptreg[ps_]])
                return ps_, kp

            def mask128(ps_, b, slot):
                OP("vector", lambda e: e.tensor_tensor(out=pt[:, ps_, b * 128:(b + 1) * 128], in0=pt[:, ps_, b * 128:(b + 1) * 128],
                                                        in1=masks[:, slot, :], op=ALU.mult), rd=[ptreg[ps_], creg], wr=[ptreg[ps_]])

            def maskA(ps_, T, G):
                j0 = 4 * T
                t0 = 2 * j0 + 1 - G
                bmin = 0
                while t0 + 2 * bmin < 0:
                    bmin += 1
                slot = lambda b: TA0 + (t0 + 2 * b) * 2 + (b % 2)
                if bmin == 0:
                    mk = bass.AP(masks, slot(0) * 128, [[44 * 128, 128], [8 * 128, 2], [5 * 128, 2], [1, 128]])
                    ptv = pt[:, ps_, :].rearrange("p (u v i) -> p u v i", u=2, v=2)
                    OP("vector", lambda e: e.tensor_tensor(out=ptv, in0=ptv, in1=mk, op=ALU.mult), rd=[ptreg[ps_], creg], wr=[ptreg[ps_]])
                    return
                if bmin == 1:
                    mask128(ps_, 1, slot(1))
                if bmin <= 2:
                    mk = bass.AP(masks, slot(2) * 128, [[44 * 128, 128], [5 * 128, 2], [1, 128]])
                    ptv = pt[:, ps_, 256:512].rearrange("p (v i) -> p v i", v=2)
                    OP("vector", lambda e: e.tensor_tensor(out=ptv, in0=ptv, in1=mk, op=ALU.mult), rd=[ptreg[ps_], creg], wr=[ptreg[ps_]])
                else:
                    mask128(ps_, 3, slot(3))

            def epilogue(m, T):
                r = smr["epi"]
                if debug:
                    for b in range(4):
                        DMA("sync", "dbg", [(ydbg[(4 * T + b) * 128:(4 * T + b + 1) * 128, m * 512:(m + 1) * 512], ytile[:, b, :])], rd=[yreg])
                nh = 8 if m == 3 else 4
                OP("vector", lambda e: e.tensor_reduce(out=sm[:, 16:20], in_=ssq[:, :, 0:nh], op=ALU.add, axis=AX.X), rd=[smr["ssq"]], wr=[r])
                OP("vector", lambda e: e.tensor_scalar(out=sm[:, 20:24], in0=sm[:, 16:20], scalar1=1.0 / 512, scalar2=1e-6, op0=ALU.mult, op1=ALU.add),
                   rd=[r], wr=[r])
                OP("scalar", lambda e: e.activation(out=sm[:, 24:28], in_=sm[:, 20:24], func=AF.Ln), rd=[r], wr=[r])
                OP("scalar", lambda e: e.activation(out=sm[:, 28:32], in_=sm[:, 24:28], func=AF.Exp, scale=-0.5), rd=[r], wr=[r])
                its = []
                slots_g = [WS.next(f"G{m}{g}") for g in range(2)]
                for g in range(2):
                    s = slots_g[g]
                    for b in range(4):
                        q = nxt("sgq", 2)
                        j = 4 * T + b

                        def sA(s=s, b=b, g=g, q=q, j=j):
                            bk = proj_tm(s, j, 256)
                            OP("scalar", lambda e: e.activation(out=sg[:, q, :], in_=P[:, bk, 0:256], func=AF.Silu),
                               rd=[pb[bk]], wr=[reg(f"sg{q}")])
                            OP("vector", lambda e: e.scalar_tensor_tensor(
                                out=ygb[:, q, :], in0=ytile[:, b, g * 256:(g + 1) * 256], scalar=sm[:, 28 + b:29 + b], in1=sg[:, q, :],
                                op0=ALU.mult, op1=ALU.mult), rd=[yreg, r, reg(f"sg{q}")], wr=[reg(f"yg{q}")])

                        def sB(g=g, q=q, j=j):
                            c0 = m * 4 + g * 2
                            transposes_to(lambda c: yT[:, c0 + c, j * 128:(j + 1) * 128],
                                          lambda c: ygb[:, q, c * 128:(c + 1) * 128], 2, [reg(f"yg{q}")], [reg("yT")],
                                          scale_fn=lambda c: cols[:, 21 * l + 5 + c0 + c:21 * l + 6 + c0 + c])
                        its.append((sA, sB))
                pipeline(its, 1)

            def dense_stream(m, isA, isB, scale):
                items = []
                for T in range(2):
                    j0 = 4 * T
                    nG = 8 * T + 8
                    for h in range(4):
                        st = {}
                        for G in range(nG):
                            d = {}

                            def s1(T=T, h=h, G=G, d=d, st=st, j0=j0):
                                if G == 0:
                                    ob_ = nxt("po", 2)
                                    st["slots"] = [ob_ * 4 + b for b in range(4)]
                                d["ps"], d["kp"] = scores(T, h, G, isB, scale)
                                if isA:
                                    maskA(d["ps"], T, G)
                                else:
                                    for b in range(4):
                                        j = j0 + b
                                        if 2 * j <= G <= 2 * j + 1:
                                            mask128(d["ps"], b, MB0 + (j % 2) * 2 + (G - 2 * j))

                            def s2(T=T, h=h, G=G, d=d, st=st, j0=j0, nG=nG):
                                slots = st["slots"]
                                calls, wr = [], []
                                for b in range(4):
                                    j = j0 + b
                                    if G <= 2 * j + 1:
                                        calls.append(lambda e, b=b, j=j: e.matmul(
                                            po8[:, slots[b], 0:129], lhsT=pt[:, d["ps"], b * 128:(b + 1) * 128], rhs=Vaug[:, d["kp"], h, :],
                                            start=(G == 0 and b % 2 == 0), stop=(G == 2 * j + 1), skip_group_check=True))
                                        wr.append(por[slots[b]])
                                MM(calls, rd=[ptreg[d["ps"]], areg["V"]], wr=wr)
                                if G == nG - 1:
                                    normalize_po(slots, h)
                                    if h == 3:
                                        epilogue(m, T)
                            items.append((s1, s2))
                pipeline(items, 2)

            def zero_ssq():
                OP("vector", lambda e: e.memset(sm[:, 208:240], 0.0), wr=[smr["ssq"]])

            load_ctx_AC(0)
            qproj_AC("A")
            zero_ssq()
            dense_stream(0, True, False, 128 ** -0.5)

            load_ctx_AC(1)
            qproj_AC("C")
            zero_ssq()
            r = smr["km"]
            for h in range(4):
                OP("vector", lambda e, h=h: e.tensor_reduce(out=sm[:, 32:48], in_=kT[:, h, :].rearrange("p (k t) -> p k t", k=16),
                                                            op=ALU.add, axis=AX.X), rd=[areg["kT"]], wr=[r])
                OP("vector", lambda e: e.tensor_tensor(out=sm[:, 48:56], in0=sm[:, 32:40], in1=sm[:, 40:48], op=ALU.add), rd=[r], wr=[r])
                OP("vector", lambda e, h=h: e.tensor_scalar(out=kmean[:, h, :], in0=sm[:, 48:56], scalar1=1.0 / 256, scalar2=None, op0=ALU.mult),
                   rd=[r], wr=[areg["kmean"]])
            selbuf = [sm[:, 64:96].rearrange("p (b n) -> p b n", b=4), sm[:, 144:176].rearrange("p (b n) -> p b n", b=4)]
            gmv = sm[:, 96:128].rearrange("p (b n) -> p b n", b=4)
            top = sm[:, 176:208].rearrange("p (b n) -> p b n", b=4)
            gmr = reg("sm_gm")
            accr = reg("accC")
            items = []
            grp = 0
            for T in range(2):
                j0 = 4 * T
                for h in range(4):
                    st = {"first": [True] * 4}
                    sel = selbuf[grp % 2]
                    selr = smr[f"sel{grp % 2}"]
                    grp += 1
                    nN = j0 + 4
                    for n in range(nN):
                        d = {}

                        def s1(T=T, h=h, n=n, d=d, j0=j0, sel=sel, selr=selr):
                            if n == 0:
                                bk = nxt("pp", 2)
                                MM([mm(P[:, bk, b * 8:(b + 1) * 8], qT[:, h, (j0 + b) * 128:(j0 + b + 1) * 128], kmean[:, h, :], True, True)
                                    for b in range(4)], rd=[areg["qT"], areg["kmean"]], wr=[pb[bk]])
                                OP("vector", lambda e: e.tensor_tensor(out=gmv, in0=P[:, bk, 0:32].rearrange("p (b n) -> p b n", b=4),
                                                                       in1=negm[:, j0:j0 + 4, :], op=ALU.add), rd=[pb[bk], creg], wr=[gmr])
                                for b in range(4):
                                    OP("vector", lambda e, b=b: e.max(out=top[:, b, :], in_=gmv[:, b, :]), rd=[gmr], wr=[gmr])
                                for b in range(4):
                                    OP("vector", lambda e, b=b: e.tensor_scalar(out=sel[:, b, :], in0=gmv[:, b, :], scalar1=top[:, b, 2:3], scalar2=None,
                                                                                op0=ALU.is_ge), rd=[gmr], wr=[selr])
                            d["pss"] = []
                            for w in range(2):
                                ps_, kp = scores(T, h, 2 * n + w, False, 128 ** -0.5)
                                for b in range(4):
                                    if j0 + b == n:
                                        mask128(ps_, b, MB0 + (n % 2) * 2 + w)
                                d["pss"].append((ps_, kp))

                        def s2(T=T, h=h, n=n, d=d, j0=j0, sel=sel, selr=selr, st=st, nN=nN):
                            ob_ = nxt("po", 2)
                            slots = [ob_ * 4 + b for b in range(4)]
                            pss = d["pss"]
                            calls, wr = [], []
                            for b in range(4):
                                if j0 + b >= n:
                                    for w in range(2):
                                        ps_, kp = pss[w]
                                        calls.append(mm(po8[:, slots[b], 0:129], pt[:, ps_, b * 128:(b + 1) * 128], Vaug[:, kp, h, :], w == 0, w == 1))
                                    wr.append(por[slots[b]])
                            MM(calls, rd=[ptreg[pss[0][0]], ptreg[pss[1][0]], areg["V"]], wr=wr)
                            first = st["first"]
                            for b in range(4):
                                j = j0 + b
                                if j < n:
                                    continue
                                src = po8[:, slots[b], 0:129]
                                dst = accC[:, b, 0:129]
                                if j == n:
                                    if first[b]:
                                        OP("vector", lambda e, src=src, dst=dst: e.tensor_copy(out=dst, in_=src), rd=[por[slots[b]]], wr=[accr])
                                    else:
                                        OP("vector", lambda e, src=src, dst=dst: e.tensor_tensor(out=dst, in0=src, in1=dst, op=ALU.add),
                                           rd=[por[slots[b]]], wr=[accr])
                                else:
                                    if first[b]:
                                        OP("vector", lambda e, src=src, dst=dst, b=b: e.tensor_scalar(
                                            out=dst, in0=src, scalar1=sel[:, b, n:n + 1], scalar2=None, op0=ALU.mult),
                                           rd=[por[slots[b]], selr], wr=[accr])
                                    else:
                                        OP("vector", lambda e, src=src, dst=dst, b=b: e.scalar_tensor_tensor(
                                            out=dst, in0=src, scalar=sel[:, b, n:n + 1], in1=dst, op0=ALU.mult, op1=ALU.add),
                                           rd=[por[slots[b]], selr], wr=[accr])
                                first[b] = False
                            if n == nN - 1:
                                normalize_po(None, h, src4=accC, srcregs=[accr])
                                if h == 3:
                                    epilogue(2, T)
                        items.append((s1, s2))
            pipeline(items, 1)

            load_ctx_AC(2)
            DMA("sync", "ctxr", [(krT[0:64, rr * 1024:(rr + 1) * 1024], ob[3][rr * 320:rr * 320 + 64, :]) for rr in range(2)],
                wr=[areg["krT"]], deps=[ccoll[3]])
            s0 = WS.next("Bcq0")
            s1_ = WS.next("Bcq1")
            for j in range(NB):
                bk = nxt("pp", 2)
                calls = [mm(P[:, bk, 0:256], xT[:, k, j * 128:(j + 1) * 128], wv(s0)[:, k, 0:256], k == 0, k == 15) for k in range(16)]
                calls += [mm(P[:, bk, 256:384], xT[:, k, j * 128:(j + 1) * 128], wv(s1_)[:, k, 0:128], k == 0, k == 15) for k in range(16)]
                MM(calls, rd=[wreg[s0], wreg[s1_], xTreg], wr=[pb[bk]])
                rmsnorm_rows(bk, 384, cqb[:, 0:384], [reg("cqb")])
                transposes_to(lambda c, j=j: cqnT[:, c, j * 128:(j + 1) * 128], lambda c: cqb[:, c * 128:(c + 1) * 128], 3,
                              [reg("cqb")], [areg["cqnT"]], scale_fn=lambda c: cols[:, 21 * l + c:21 * l + 1 + c])
            s = WS.next("WUQ")
            wq = wbuf[:, s, 0:2304].rearrange("p (k h c) -> p k h c", k=3, h=4)
            for h in range(4):
                for T in range(2):
                    bk = nxt("pp", 2)
                    MM([mm(P[:, bk, :], wq[:, k, h, 0:128], cqnT[:, k, T * 512:(T + 1) * 512], k == 0, k == 2) for k in range(3)],
                       rd=[wreg[s], areg["cqnT"]], wr=[pb[bk]])
                    OP("scalar", lambda e, bk=bk, h=h, T=T: e.activation(out=qT[:, h, T * 512:(T + 1) * 512], in_=P[:, bk, :], func=AF.Copy),
                       rd=[pb[bk]], wr=[areg["qT"]])
                    bk = nxt("pp", 2)
                    MM([mm(P[0:64, bk, :], wq[:, k, h, 128:192], cqnT[:, k, T * 512:(T + 1) * 512], k == 0, k == 2) for k in range(3)],
                       rd=[wreg[s], areg["cqnT"]], wr=[pb[bk]])
                    rope(bk, 64, qrT[0:64, h, T * 512:(T + 1) * 512], [areg["qrT"]], 32, T)
            zero_ssq()
            dense_stream(1, False, True, 192 ** -0.5)

            kTD = av(0, 2048)
            items = [(kTD[:, rr * 1024:(rr + 1) * 1024], ob[3][rr * 320 + 64:rr * 320 + 192, :]) for rr in range(2)]
            DMA("sync", "ctxk", items, wr=[areg["kT"]], deps=[ccoll[3]])
            items = []
            for rr in range(2):
                src = ob[3][rr * 320 + 192:rr * 320 + 320, :].rearrange("r (e c) -> (r e) c", e=8)
                for hh in range(2):
                    items.append((VD[:, rr * 8:(rr + 1) * 8, hh, 0:64], src[:, hh * 64:(hh + 1) * 64].rearrange("(jj p) c -> p jj c", p=128)))
            DMA("sync", "ctxv", items, wr=[areg["V"]], deps=[ccoll[3]])
            OP("vector", lambda e: e.memset(VD[:, :, :, 64:65], 1.0), wr=[areg["V"]])
            for g in range(2):
                s = WS.next(f"Dq{g}")
                for cc in range(2):
                    c = 2 * g + cc
                    for T in range(2):
                        bk = proj_fm(s, cc * 128, 128, T)
                        rope(bk, 128, qT[:, c, T * 512:(T + 1) * 512], [areg["qT"]], 32, T)
            zero_ssq()
            r = smr["D"]
            items = []
            for T in range(2):
                for b in range(4):
                    for c in range(4):
                        for sl in range(2):
                            d = {}
                            j = 4 * T + b
                            Gs = [G for G in (2 * j - 1, 2 * j, 2 * j + 1) if G >= 0]

                            def s1(j=j, c=c, sl=sl, d=d, Gs=Gs):
                                w0 = 3 - len(Gs)
                                prt = slice(64 * sl, 64 * sl + 64)
                                sbk = SB[nxt("ps", 4)]
                                MM([mm(P[:, sbk, (w0 + i) * 128:(w0 + i + 1) * 128], kTD[prt, kap(G) * 128:(kap(G) + 1) * 128],
                                       qT[prt, c, j * 128:(j + 1) * 128], True, True) for i, G in enumerate(Gs)],
                                   rd=[areg["kT"], areg["qT"]], wr=[pb[sbk]])
                                ps_ = nxt("pt", 4)
                                d["ps"] = ps_
                                OP("scalar", lambda e: e.activation(out=pt[:, ps_, w0 * 128:384], in_=P[:, sbk, w0 * 128:384],
                                                                    func=AF.Exp, scale=0.125), rd=[pb[sbk]], wr=[ptreg[ps_]])
                                OP("vector", lambda e: e.tensor_tensor(
                                    out=pt[:, ps_, w0 * 128:384], in0=pt[:, ps_, w0 * 128:384],
                                    in1=masks[:, MD0 + (j % 2) * 3 + w0:MD0 + (j % 2) * 3 + 3, :].rearrange("p a b -> p (a b)"), op=ALU.mult),
                                   rd=[ptreg[ps_], creg], wr=[ptreg[ps_]])

                            def s2(T=T, b=b, c=c, sl=sl, d=d, Gs=Gs, l=l):
                                w0 = 3 - len(Gs)
                                ps_ = d["ps"]
                                hd = c + 4 * sl
                                slot = nxt("po8", 8)
                                MM([mm(po8[:, slot, 0:65], pt[:, ps_, (w0 + i) * 128:(w0 + i + 1) * 128], VD[:, kap(G), sl, :], i == 0, i == len(Gs) - 1)
                                    for i, G in enumerate(Gs)], rd=[ptreg[ps_], areg["V"]], wr=[por[slot]])
                                u = 240 + (hd % 8)
                                OP("vector", lambda e: e.tensor_tensor(out=sm[:, u:u + 1], in0=po8[:, slot, 64:65],
                                                                       in1=esink[:, 8 * l + hd:8 * l + hd + 1], op=ALU.add),
                                   rd=[por[slot], reg("esink")], wr=[r])
                                OP("vector", lambda e: e.reciprocal(out=sm[:, u + 8:u + 9], in_=sm[:, u:u + 1]), rd=[r], wr=[r])
                                OP("scalar", lambda e: e.activation(out=ytile[:, b, hd * 64:(hd + 1) * 64], in_=po8[:, slot, 0:64],
                                                                    func=AF.Copy, scale=sm[:, u + 8:u + 9]),
                                   rd=[por[slot], r], wr=[yreg])
                                sumsq(b, hd, hd * 64, (hd + 1) * 64)
                                if b == 3 and c == 3 and sl == 1:
                                    epilogue(3, T)
                            items.append((s1, s2))
            pipeline(items, 2)

            S.barrier()
            DMA("gpsimd", "wo", [(wo[:, :, q * 512:(q + 1) * 512], w_out[l, :, q * 512:(q + 1) * 512].rearrange("(k p) c -> p k c", p=128))
                                 for q in range(4)], wr=[reg("wo")])
            DMA("sync", "gb", [(gam, ln_g[l:l + 1, :].partition_broadcast(128)[:, 0, :]),
                               (bet, ln_b[l:l + 1, :].partition_broadcast(128)[:, 0, :])], wr=[wreg[0], wreg[1]])
            xsrc = x_in if l == 0 else xs[(l - 1) % 2]
            xdst = out_d if l == depth - 1 else xs[l % 2]
            zreg = reg("ytile")
            r = reg("sm")
            for j in range(NB):
                DMA("sync", "xr", [(xr, xsrc[j * 128:(j + 1) * 128, :])], wr=[wreg[2]])
                for q in range(4):
                    MM([mm(P[:, 4 + q, :], yT[:, k, j * 128:(j + 1) * 128], wo[:, k, q * 512:(q + 1) * 512], k == 0, k == 15) for k in range(16)],
                       rd=[reg("wo"), reg("yT")], wr=[por[2 * q], por[2 * q + 1]])
                for q in range(4):
                    OP("vector", lambda e, q=q: e.scalar_tensor_tensor(out=zt[:, q * 512:(q + 1) * 512], in0=xr[:, q * 512:(q + 1) * 512], scalar=ALPHA,
                                                                      in1=P[:, 4 + q, :], op0=ALU.mult, op1=ALU.add),
                       rd=[wreg[2], por[2 * q], por[2 * q + 1]], wr=[zreg])
                    OP("vector", lambda e, q=q: e.bn_stats(out=sm[:, 176 + 6 * q:182 + 6 * q], in_=zt[:, q * 512:(q + 1) * 512]), rd=[zreg], wr=[r])
                OP("vector", lambda e: e.bn_aggr(out=sm[:, 200:202], in_=sm[:, 176:200]), rd=[r], wr=[r])
                OP("vector", lambda e: e.tensor_scalar(out=sm[:, 202:203], in0=sm[:, 201:202], scalar1=1e-5, scalar2=None, op0=ALU.add), rd=[r], wr=[r])
                OP("scalar", lambda e: e.activation(out=sm[:, 203:204], in_=sm[:, 202:203], func=AF.Ln), rd=[r], wr=[r])
                OP("scalar", lambda e: e.activation(out=sm[:, 204:205], in_=sm[:, 203:204], func=AF.Exp, scale=-0.5), rd=[r], wr=[r])
                OP("vector", lambda e: e.tensor_scalar(out=zt, in0=zt, scalar1=sm[:, 200:201], scalar2=sm[:, 204:205], op0=ALU.subtract, op1=ALU.mult),
                   rd=[r, zreg], wr=[zreg])
                OP("vector", lambda e: e.tensor_tensor(out=zt, in0=zt, in1=gam, op=ALU.mult), rd=[zreg, wreg[0]], wr=[zreg])
                OP("vector", lambda e: e.tensor_tensor(out=zt, in0=zt, in1=bet, op=ALU.add), rd=[zreg, wreg[1]], wr=[zreg])
                DMA("sync", "xo", [(xdst[j * 128:(j + 1) * 128, :], zt)], rd=[zreg])
                if l < depth - 1:
                    block_to_xT(zt, zreg, j)
            S.barrier()

        S.emit()
    return nc


def _mult(delta):
    m = np.zeros_like(delta, dtype=np.float32)
    ok = delta >= 0
    m += (ok & (delta <= 128))
    m += (ok & (delta % 4 == 0) & (delta <= 512))
    m += (ok & (delta % 16 == 0) & (delta <= 2048))
    return m


def host_tables(r):
    i = np.arange(128)[None, :]
    k = np.arange(128)[:, None]
    masks = np.zeros((128, 44, 128), np.float32)
    for jpar in range(2):
        e = (jpar + r) % 2
        for t in range(17):
            masks[:, t * 2 + jpar, :] = _mult((t - 1 + e) * 128 + i - k)
        for w in range(2):
            masks[:, 34 + jpar * 2 + w, :] = (((e - w) * 128 + i - k) >= 0)
        for w in range(3):
            dd = (e + 1 - w) * 128 + i - k
            masks[:, 38 + jpar * 3 + w, :] = ((dd >= 0) & (dd <= 127))
    pos = np.concatenate([gblk(r, j) * 128 + np.arange(128) for j in range(NB)]).astype(np.float32)
    tabs = np.zeros((128, 4, 1024), np.float32)
    p = np.arange(128)
    inv128 = (np.float32(10000.0) ** (-np.arange(0, 128, 2, dtype=np.float32) / np.float32(128))).astype(np.float32)
    inv64 = (np.float32(10000.0) ** (-np.arange(0, 64, 2, dtype=np.float32) / np.float32(64))).astype(np.float32)
    a128 = (pos[None, :] * inv128[p % 64][:, None]).astype(np.float32)
    a64 = (pos[None, :] * inv64[(p % 64) % 32][:, None]).astype(np.float32)
    tabs[:, 0] = np.cos(a128)
    tabs[:, 1] = np.sin(a128) * np.where(p < 64, -1.0, 1.0)[:, None]
    tabs[:, 2] = np.cos(a64)
    tabs[:, 3] = np.sin(a64) * np.where((p % 64) < 32, -1.0, 1.0)[:, None]
    return masks.astype(ml_dtypes.bfloat16), tabs


def host_inputs(x, w_in, q_norm, w_uq, kv_norm, w_ukv, sinks, branch_norm, w_out, ln_gamma, ln_beta):
    f = lambda a: np.ascontiguousarray(np.asarray(a, dtype=np.float32))
    x, w_in, w_uq, w_ukv, w_out = f(x), f(w_in), f(w_uq), f(w_ukv), f(w_out)
    q_norm, kv_norm, sinks, branch_norm, ln_gamma, ln_beta = f(q_norm), f(kv_norm), f(sinks), f(branch_norm), f(ln_gamma), f(ln_beta)
    cols = np.zeros((128, 84), np.float32)
    for l in range(DEPTH):
        cols[:, 21 * l:21 * l + 3] = q_norm[l].reshape(3, 128).T
        cols[:, 21 * l + 3:21 * l + 5] = kv_norm[l].reshape(2, 128).T
        cols[:, 21 * l + 5:21 * l + 21] = branch_norm[l].reshape(16, 128).T
    sinks_bc = np.ascontiguousarray(np.broadcast_to(sinks.reshape(1, 32), (128, 32)))
    negm = np.zeros((128, 8, 8), np.float32)
    for j in range(8):
        negm[:, j, j:] = NEG
    negm = negm.reshape(128, 64)
    ident = np.eye(128, dtype=np.float32).astype(ml_dtypes.bfloat16)
    tb = [host_tables(r) for r in range(2)]
    in_maps = []
    for c in range(8):
        b, r = c // 2, c % 2
        xo = np.concatenate([x[b, gblk(r, j) * 128:(gblk(r, j) + 1) * 128, :] for j in range(NB)], 0)
        in_maps.append({"x": np.ascontiguousarray(xo), "w_in": w_in, "w_uq": w_uq, "w_ukv": w_ukv, "w_out": w_out,
                        "ln_gamma": ln_gamma, "ln_beta": ln_beta, "cols": cols, "sinks_bc": sinks_bc, "negm": negm,
                        "tabs": tb[r][1], "masks": tb[r][0], "ident": ident})
    return in_maps


def assemble(results, key="out"):
    out = np.zeros((4, 2048, D), np.float32)
    for c in range(8):
        b, r = c // 2, c % 2
        o = np.asarray(results[c][key])
        for j in range(NB):
            g = gblk(r, j)
            out[b, g * 128:(g + 1) * 128, :] = o[j * 128:(j + 1) * 128, :]
    return out


_NC = {}


def kernel(x, w_in, q_norm, w_uq, kv_norm, w_ukv, sinks, branch_norm, w_out, ln_gamma, ln_beta):
    in_maps = host_inputs(x, w_in, q_norm, w_uq, kv_norm, w_ukv, sinks, branch_norm, w_out, ln_gamma, ln_beta)
    if "nc" not in _NC:
        _NC["nc"] = build(DEPTH, False)
    res = run_bass_kernel_spmd(_NC["nc"], in_maps, core_ids=list(range(8)))
    return assemble(res.results)
```

```python
import contextlib
import numpy as np
import ml_dtypes
import concourse.bass as bass
import concourse.mybir as mybir
from concourse.bass_utils import run_bass_kernel_spmd

F32 = mybir.dt.float32
BF16 = mybir.dt.bfloat16
AF = mybir.ActivationFunctionType
ALU = mybir.AluOpType
AX = mybir.AxisListType

D = 2048
NIN = 6592
NOWN = 1024
NB = 8
DEPTH = 4
ALPHA = (2 * DEPTH) ** 0.25
O_AQ, O_AK, O_AV = 0, 512, 1024
O_BCQ, O_BCKV, O_BKR = 1536, 1920, 2176
O_CQ, O_CK, O_CV = 2240, 2752, 3264
O_DQ, O_DK, O_DV = 3776, 4288, 4416
O_G = 4544
NEG = -1e30
PAIRS = [[0, 1], [2, 3], [4, 5], [6, 7]]


def gblk(r, j):
    return 2 * j + (j + r) % 2


def kap(G):
    jj = G // 2
    rr = (G % 2 - jj) % 2
    return 8 * rr + jj


class Sched:
    ENGS = ("sync", "scalar", "vector", "gpsimd", "tensor")

    def __init__(self, nc):
        self.nc = nc
        self.ops = {e: [] for e in self.ENGS}
        self.sems = {}
        self.cnt = {}
        self.mult = {}
        self.waited = {e: {} for e in self.ENGS}
        self._cms = []

    def sem(self, key, mult=1):
        if key not in self.sems:
            cm = self.nc.semaphore(key)
            self.sems[key] = cm.__enter__()
            self._cms.append(cm)
            self.cnt[key] = 0
            self.mult[key] = mult
        return self.sems[key]

    def wait(self, eng, toks):
        for t in toks:
            if t is None:
                continue
            key, n = t
            w = self.waited[eng]
            if w.get(key, 0) >= n:
                continue
            w[key] = n
            h = self.sems[key]
            self.ops[eng].append(lambda e, h=h, v=n * self.mult[key]: e.wait_ge(h, v))

    def op(self, eng, fn, deps=(), sig=True, key=None):
        self.wait(eng, deps)
        if sig:
            key = key or ("c_" + eng)
            h = self.sem(key)
            self.cnt[key] += 1
            self.ops[eng].append(lambda e, fn=fn, h=h: fn(e).then_inc(h, 1))
            return (key, self.cnt[key])
        self.ops[eng].append(lambda e, fn=fn: fn(e))
        return None

    def dma(self, eng, chan, items, deps=()):
        key = "d_" + chan
        h = self.sem(key, 16)
        if self.cnt[key]:
            self.wait(eng, [(key, self.cnt[key])])
        self.wait(eng, deps)
        for (o, i, kw) in items:
            self.cnt[key] += 1
            self.ops[eng].append(lambda e, o=o, i=i, kw=kw, h=h: e.dma_start(out=o, in_=i, **kw).then_inc(h, 16))
        return (key, self.cnt[key])

    def barrier(self):
        toks = [(k, c) for k, c in self.cnt.items() if c > 0]
        for e in self.ENGS:
            self.wait(e, toks)

    def emit(self):
        self.barrier()
        with self.nc.Block() as block:
            @block.sync
            def _(e):
                for f in self.ops["sync"]:
                    f(e)

            @block.scalar
            def _(e):
                for f in self.ops["scalar"]:
                    f(e)

            @block.vector
            def _(e):
                for f in self.ops["vector"]:
                    f(e)

            @block.gpsimd
            def _(e):
                for f in self.ops["gpsimd"]:
                    f(e)

            @block.tensor
            def _(e):
                for f in self.ops["tensor"]:
                    f(e)
        for cm in reversed(self._cms):
            cm.__exit__(None, None, None)


class Reg:
    def __init__(self):
        self.w = None
        self.r = {}

    def rdeps(self):
        return [self.w] if self.w else []

    def wdeps(self):
        return ([self.w] if self.w else []) + list(self.r.values())

    def read(self, tok):
        if tok is None:
            return
        k = tok[0]
        if k not in self.r or self.r[k][1] < tok[1]:
            self.r[k] = tok

    def write(self, tok):
        self.w = tok
        self.r = {}


def build(depth=DEPTH, debug=False):
    nc = bass.Bass("TRN2", target_bir_lowering=False)
    S = Sched(nc)
    dt_in = lambda name, shape, dt=F32: nc.dram_tensor(name, shape, dt, kind="ExternalInput")
    x_in = dt_in("x", [NOWN, D])
    w_in = dt_in("w_in", [DEPTH, D, NIN])
    w_uq = dt_in("w_uq", [DEPTH, 384, 768])
    w_ukv = dt_in("w_ukv", [DEPTH, 256, 1024])
    w_out = dt_in("w_out", [DEPTH, D, D])
    ln_g = dt_in("ln_gamma", [DEPTH, D])
    ln_b = dt_in("ln_beta", [DEPTH, D])
    cols_d = dt_in("cols", [128, 84])
    sinks_d = dt_in("sinks_bc", [128, 32])
    negm_d = dt_in("negm", [128, 64])
    tabs_d = dt_in("tabs", [128, 4, 1024])
    masks_d = dt_in("masks", [128, 44, 128], BF16)
    ident_d = dt_in("ident", [128, 128], BF16)
    out_d = nc.dram_tensor("out", [NOWN, D], F32, kind="ExternalOutput")
    if debug:
        ydbg = nc.dram_tensor("ydbg", [NOWN, D], F32, kind="ExternalOutput")
    xs = [nc.dram_tensor("xs0", [NOWN, D], F32), nc.dram_tensor("xs1", [NOWN, D], F32)]
    ibs = [[nc.dram_tensor(f"ib{l}_{i}", [320 if i == 3 else 1024, 1024], BF16) for i in range(4)] for l in range(depth)]
    obs = [[nc.dram_tensor(f"ob{l}_{i}", [640 if i == 3 else 2048, 1024], BF16) for i in range(4)] for l in range(depth)]

    es = contextlib.ExitStack()
    with es:
        sb = lambda name, shape, dt: es.enter_context(nc.sbuf_tensor("sb_" + name, shape, dt))
        xT = sb("xT", [128, 16, NOWN], BF16)
        yT = sb("yT", [128, 16, NOWN], BF16)
        arena = sb("arena", [128, 32768], BF16)
        wbuf = sb("wbuf", [128, 3, 4096], BF16)
        stage = sb("stage", [128, 4, 512], BF16)
        tabs = sb("tabs", [128, 4, 1024], F32)
        masks = sb("masks", [128, 44, 128], BF16)
        ident = sb("ident", [128, 128], BF16)
        pt = sb("pt", [128, 4, 512], BF16)
        ytile = sb("ytile", [128, 4, 512], F32)
        rt1 = sb("rt1", [128, 512], F32)
        rt2 = sb("rt2", [128, 512], F32)
        cols = sb("cols", [128, 84], F32)
        esink = sb("esink", [128, 32], F32)
        negm = sb("negm", [128, 8, 8], F32)
        sm = sb("sm", [128, 256], F32)
        accC = sb("accC", [128, 4, 132], F32)
        sg = sb("sg", [128, 2, 256], F32)
        ygb = sb("ygb", [128, 2, 256], BF16)
        cqb = sb("cqb", [128, 384], BF16)
        P = es.enter_context(nc.psum_tensor("P", [128, 8, 512], F32))

        R = {}
        def reg(name):
            if name not in R:
                R[name] = Reg()
            return R[name]
        pb = [reg(f"pb{i}") for i in range(4)]
        por = [reg(f"po{i}") for i in range(8)]
        wreg = [reg(f"w{i}") for i in range(3)]
        streg = [reg(f"st{i}") for i in range(4)]
        ptreg = [reg(f"pt{i}") for i in range(4)]

        po8 = P[:, 4:8, :].rearrange("p k (b c) -> p (k b) c", c=256)

        def av(off, n):
            return arena[:, off:off + n]
        kT = av(0, 8192).rearrange("p (h t) -> p h t", h=4)
        Vaug = av(8192, 8256).rearrange("p (k h c) -> p k h c", k=16, h=4)
        VD = av(8192, 2080).rearrange("p (k h c) -> p k h c", k=16, h=2)
        qT = av(16448, 4096).rearrange("p (h t) -> p h t", h=4)
        qrT = av(20544, 4096).rearrange("p (h t) -> p h t", h=4)
        krT = av(24640, 2048)
        cqnT = av(26688, 3072).rearrange("p (k t) -> p k t", k=3)
        kmean = av(29760, 32).rearrange("p (h n) -> p h n", h=4)
        ckvnT = av(29792, 2048).rearrange("p (k t) -> p k t", k=2)
        wo = arena[:, :].rearrange("p (k c) -> p k c", k=16)
        gam = wbuf[:, 0, :].bitcast(F32)
        bet = wbuf[:, 1, :].bitcast(F32)
        xr = wbuf[:, 2, :].bitcast(F32)
        zt = ytile[:, :, :].rearrange("p a b -> p (a b)")
        xb = stage[:, :, :].rearrange("p a b -> p (a b)")

        def OP(eng, fn, rd=(), wr=(), deps=()):
            d = list(deps)
            for t in rd:
                d.extend(t.rdeps())
            for t in wr:
                d.extend(t.wdeps())
            tok = S.op(eng, fn, d)
            for t in rd:
                t.read(tok)
            for t in wr:
                t.write(tok)
            return tok

        def DMA(eng, chan, items, rd=(), wr=(), deps=()):
            d = list(deps)
            for t in rd:
                d.extend(t.rdeps())
            for t in wr:
                d.extend(t.wdeps())
            tok = S.dma(eng, chan, [(o, i, {}) for (o, i) in items], d)
            for t in rd:
                t.read(tok)
            for t in wr:
                t.write(tok)
            return tok

        def MM(calls, rd=(), wr=()):
            d = []
            for t in rd:
                d.extend(t.rdeps())
            for t in wr:
                d.extend(t.wdeps())
            S.wait("tensor", d)
            for f in calls[:-1]:
                S.op("tensor", f, sig=False)
            tok = S.op("tensor", calls[-1])
            for t in rd:
                t.read(tok)
            for t in wr:
                t.write(tok)
            return tok

        def mm(out, lhsT, rhs, start, stop):
            return lambda e: e.matmul(out, lhsT=lhsT, rhs=rhs, start=start, stop=stop)

        creg = reg("const")
        DMA("sync", "const", [(tabs[:], tabs_d[:, :, :]), (masks[:], masks_d[:, :, :]), (ident[:], ident_d[:, :]),
                              (cols[:], cols_d[:, :]), (esink[:], sinks_d[:, :]),
                              (negm[:].rearrange("p a b -> p (a b)"), negm_d[:, :])], wr=[creg])
        OP("scalar", lambda e: e.activation(out=esink[:], in_=esink[:], func=AF.Exp), rd=[creg], wr=[reg("esink")])
        S.barrier()
        cos128, ssin128, cos64, ssin64 = (tabs[:, i, :] for i in range(4))
        TA0 = 0
        MB0 = 34
        MD0 = 38

        wstate = {"i": 0}

        def wload(items, name):
            s = wstate["i"] % 3
            wstate["i"] += 1
            DMA("gpsimd", f"w{s}", items(s), wr=[wreg[s]])
            return s

        def wv(s):
            return wbuf[:, s, :].rearrange("p (k c) -> p k c", c=256)

        def std_items(l, off, n):
            return lambda s: [(wv(s)[:, :, 0:n], w_in[l, :, off:off + n].rearrange("(k p) c -> p k c", p=128))]

        class WQ:
            def __init__(self, specs):
                self.specs = specs
                self.issued = 0
                self.slots = []
                self.pos = 0

            def _issue(self):
                name, items = self.specs[self.issued]
                self.slots.append(wload(items, name))
                self.issued += 1

            def next(self, name, la=3):
                assert self.specs[self.pos][0] == name, (self.specs[self.pos][0], name)
                while self.issued < min(len(self.specs), self.pos + la):
                    self._issue()
                s = self.slots[self.pos]
                self.pos += 1
                return s

            def prefetch(self):
                while self.issued < min(len(self.specs), self.pos + 3):
                    self._issue()

        def layer_specs(l):
            sp = []
            for nm, off in (("Ak", O_AK), ("Av", O_AV), ("Ck", O_CK), ("Cv", O_CV)):
                for g in range(2):
                    sp.append((f"{nm}{g}", std_items(l, off + 256 * g, 256)))
            sp.append(("Bkv", std_items(l, O_BCKV, 256)))
            sp.append(("Bkr", std_items(l, O_BKR, 64)))
            sp.append(("WUKV", lambda s: [(wbuf[:, s, 0:2048].rearrange("p (k c) -> p k c", k=2),
                                           w_ukv[l, :, :].rearrange("(k p) c -> p k c", p=128))]))
            sp.append(("Dkv", std_items(l, O_DK, 256)))

            def gates(m):
                r = []
                for T in range(2):
                    for g in range(2):
                        r.append((f"G{m}{g}", std_items(l, O_G + 512 * m + 256 * g, 256)))
                return r
            sp += [("Aq0", std_items(l, O_AQ, 256)), ("Aq1", std_items(l, O_AQ + 256, 256))] + gates(0)
            sp += [("Cq0", std_items(l, O_CQ, 256)), ("Cq1", std_items(l, O_CQ + 256, 256))] + gates(2)
            sp += [("Bcq0", std_items(l, O_BCQ, 256)), ("Bcq1", std_items(l, O_BCQ + 256, 128)),
                   ("WUQ", lambda s: [(wbuf[:, s, 0:2304].rearrange("p (k c) -> p k c", k=3),
                                       w_uq[l, :, :].rearrange("(k p) c -> p k c", p=128))])] + gates(1)

            def dq_items(cp):
                def f(s):
                    it = []
                    for i in range(2):
                        c = 2 * cp + i
                        it.append((wv(s)[:, :, 128 * i:128 * i + 64],
                                   w_in[l, :, O_DQ + 64 * c:O_DQ + 64 * c + 64].rearrange("(k p) c -> p k c", p=128)))
                        it.append((wv(s)[:, :, 128 * i + 64:128 * i + 128],
                                   w_in[l, :, O_DQ + 64 * (4 + c):O_DQ + 64 * (4 + c) + 64].rearrange("(k p) c -> p k c", p=128)))
                    return it
                return f
            sp += [("Dq0", dq_items(0)), ("Dq1", dq_items(1))] + gates(3)
            return sp

        ctr = {"pp": 0, "ps": 0, "pt": 0, "st": 0, "po": 0, "sgq": 0, "po8": 0}

        def nxt(k, n):
            v = ctr[k] % n
            ctr[k] += 1
            return v

        xTreg = reg("xT")

        def proj_fm(s, c0, ncol, T):
            b = nxt("pp", 2)
            calls = [mm(P[0:ncol, b, :], wv(s)[:, k, c0:c0 + ncol], xT[:, k, T * 512:(T + 1) * 512], k == 0, k == 15)
                     for k in range(16)]
            MM(calls, rd=[wreg[s], xTreg], wr=[pb[b]])
            return b

        def proj_tm(s, j, ncol):
            b = nxt("pp", 2)
            calls = [mm(P[:, b, 0:ncol], xT[:, k, j * 128:(j + 1) * 128], wv(s)[:, k, 0:ncol], k == 0, k == 15)
                     for k in range(16)]
            MM(calls, rd=[wreg[s], xTreg], wr=[pb[b]])
            return b

        rtreg = reg("rt")

        def rope(src_bank, np_, dst, dstregs, half, T, n=512):
            ct, stb = (cos128, ssin128) if half == 64 else (cos64, ssin64)
            src = P[:, src_bank, 0:n]
            tsl = slice(T * 512, T * 512 + n)
            OP("vector", lambda e: e.tensor_tensor(out=rt1[0:np_, 0:n], in0=src[0:np_, :], in1=ct[0:np_, tsl], op=ALU.mult),
               rd=[pb[src_bank]], wr=[rtreg])
            for base in range(0, np_, 2 * half):
                lo, mid, hi = base, base + half, base + 2 * half
                OP("vector", lambda e, lo=lo, mid=mid, hi=hi: e.tensor_tensor(
                    out=rt2[lo:mid, 0:n], in0=src[mid:hi, :], in1=stb[lo:mid, tsl], op=ALU.mult), rd=[pb[src_bank]], wr=[rtreg])
                OP("vector", lambda e, lo=lo, mid=mid, hi=hi: e.tensor_tensor(
                    out=rt2[mid:hi, 0:n], in0=src[lo:mid, :], in1=stb[mid:hi, tsl], op=ALU.mult), rd=[pb[src_bank]], wr=[rtreg])
            OP("vector", lambda e: e.tensor_tensor(out=dst, in0=rt1[0:np_, 0:n], in1=rt2[0:np_, 0:n], op=ALU.add),
               rd=[rtreg], wr=dstregs)

        def transposes_to(dst_fn, src_fn, nchunk, srcregs, dstregs, scale_fn=None, np_out=128):
            c = 0
            while c < nchunk:
                g = min(8, nchunk - c)
                b = nxt("pp", 2)
                pbv = P[:, b, :].bitcast(BF16)
                calls = []
                for i in range(g):
                    calls.append((lambda e, o=pbv[:, i * 128:(i + 1) * 128], s_=src_fn(c + i): e.transpose(o, s_, ident[:])))
                MM(calls, rd=srcregs + [reg("const")], wr=[pb[b]])
                for i in range(g):
                    d_ = dst_fn(c + i)
                    i_ = pbv[0:np_out, i * 128:(i + 1) * 128]
                    if scale_fn is None:
                        OP("scalar", lambda e, d_=d_, i_=i_: e.activation(out=d_, in_=i_, func=AF.Copy), rd=[pb[b]], wr=dstregs)
                    else:
                        OP("scalar", lambda e, d_=d_, i_=i_, sc=scale_fn(c + i): e.activation(out=d_, in_=i_, func=AF.Copy, scale=sc),
                           rd=[pb[b]], wr=dstregs)
                c += g

        def block_to_xT(src_f32, srcreg, j):
            OP("scalar", lambda e: e.activation(out=xb, in_=src_f32, func=AF.Copy), rd=[srcreg], wr=streg)
            for g in range(2):
                bk = nxt("pp", 2)
                pbv = P[:, bk, :].bitcast(BF16)
                MM([(lambda e, o=pbv[:, i * 128:(i + 1) * 128], s_=xb[:, (g * 8 + i) * 128:(g * 8 + i + 1) * 128]: e.transpose(o, s_, ident[:]))
                    for i in range(8)], rd=list(streg) + [reg("const")], wr=[pb[bk]])
                dst = xT[:, g * 8:(g + 1) * 8, j * 128:(j + 1) * 128]
                src = pbv[:, :].rearrange("p (c i) -> p c i", c=8)
                if g == 0:
                    OP("scalar", lambda e, dst=dst, src=src: e.activation(out=dst, in_=src, func=AF.Copy), rd=[pb[bk]], wr=[xTreg])
                else:
                    OP("vector", lambda e, dst=dst, src=src: e.tensor_copy(out=dst, in_=src), rd=[pb[bk]], wr=[xTreg])

        def stage_out(src_ap_fn, srcregs, dst_dram, eng="scalar"):
            s = nxt("st", 4)
            src, shape_p, shape_n = src_ap_fn
            OP(eng, lambda e: e.activation(out=stage[0:shape_p, s, 0:shape_n], in_=src, func=AF.Copy) if eng == "scalar"
               else e.tensor_copy(out=stage[0:shape_p, s, 0:shape_n], in_=src), rd=srcregs, wr=[streg[s]])
            DMA("sync", f"st{s}", [(dst_dram, stage[0:shape_p, s, 0:shape_n])], rd=[streg[s]])

        def rmsnorm_rows(bank, ncol, dst_bf, dstregs, eps=1e-6):
            r = reg("sm")
            OP("vector", lambda e: e.memset(sm[:, 0:1], 0.0), wr=[r])
            OP("scalar", lambda e: e.activation(out=rt1[:, 0:ncol], in_=P[:, bank, 0:ncol], func=AF.Square, accum_out=sm[:, 0:1]),
               rd=[pb[bank]], wr=[r, rtreg])
            OP("vector", lambda e: e.tensor_scalar(out=sm[:, 1:2], in0=sm[:, 0:1], scalar1=1.0 / ncol, scalar2=eps, op0=ALU.mult, op1=ALU.add),
               rd=[r], wr=[r])
            OP("scalar", lambda e: e.activation(out=sm[:, 2:3], in_=sm[:, 1:2], func=AF.Ln), rd=[r], wr=[r])
            OP("scalar", lambda e: e.activation(out=sm[:, 3:4], in_=sm[:, 2:3], func=AF.Exp, scale=-0.5), rd=[r], wr=[r])
            OP("scalar", lambda e: e.activation(out=dst_bf, in_=P[:, bank, 0:ncol], func=AF.Copy, scale=sm[:, 3:4]),
               rd=[r, pb[bank]], wr=dstregs)

        xrreg = wreg[2]
        for j in range(NB):
            DMA("sync", "xr", [(xr, x_in[j * 128:(j + 1) * 128, :])], wr=[xrreg])
            block_to_xT(xr, xrreg, j)
        S.barrier()

        for l in range(depth):
            WS = WQ(layer_specs(l))
            ib, ob = ibs[l], obs[l]
            ccoll = [None] * 4
            areg = {n: reg(f"a_{n}") for n in ("kT", "V", "qT", "qrT", "krT", "cqnT", "kmean", "ckvnT")}

            def collective(i, deps):
                S.wait("gpsimd", deps)
                key = f"cc{i}"
                S.sem(key)
                tok = S.op("gpsimd", lambda e, ii=ib[i], oo=ob[i]: e.collective_compute(
                    "AllGather", ALU.bypass, replica_groups=PAIRS, ins=[ii.ap().opt()], outs=[oo.ap().opt()]), key=key)
                ccoll[i] = tok

            def kside_AC(nm, ci):
                toks = []
                for g in range(2):
                    s = WS.next(f"{nm}k{g}")
                    for hh in range(2):
                        h = 2 * g + hh
                        for T in range(2):
                            b = proj_fm(s, hh * 128, 128, T)
                            st = nxt("st", 4)
                            rope(b, 128, stage[:, st, :], [streg[st]], 64, T)
                            toks.append(DMA("sync", f"st{st}", [(ib[ci][h * 128:(h + 1) * 128, T * 512:(T + 1) * 512], stage[:, st, :])],
                                            rd=[streg[st]]))
                vview = ib[ci][512:1024, :].rearrange("r (two c) -> (r two) c", two=2)
                for g in range(2):
                    s = WS.next(f"{nm}v{g}")
                    for j in range(NB):
                        b = proj_tm(s, j, 256)
                        st = nxt("st", 4)
                        OP("scalar", lambda e, b=b, st=st: e.activation(out=stage[:, st, 0:256], in_=P[:, b, 0:256], func=AF.Copy),
                           rd=[pb[b]], wr=[streg[st]])
                        toks.append(DMA("sync", f"st{st}", [(vview[j * 128:(j + 1) * 128, g * 256:(g + 1) * 256], stage[:, st, 0:256])],
                                        rd=[streg[st]]))
                collective(ci, toks)

            kside_AC("A", 0)
            kside_AC("C", 1)

            toksB, toksD = [], []
            s = WS.next("Bkv")
            for j in range(NB):
                b = proj_tm(s, j, 256)
                rmsnorm_rows(b, 256, cqb[:, 0:256], [reg("cqb")])
                transposes_to(lambda c, j=j: ckvnT[:, c, j * 128:(j + 1) * 128], lambda c: cqb[:, c * 128:(c + 1) * 128], 2,
                              [reg("cqb")], [areg["ckvnT"]], scale_fn=lambda c: cols[:, 21 * l + 3 + c:21 * l + 4 + c])
            s = WS.next("Bkr")
            for T in range(2):
                b = proj_fm(s, 0, 64, T)
                st = nxt("st", 4)
                rope(b, 64, stage[0:64, st, :], [streg[st]], 32, T)
                toksD.append(DMA("sync", f"st{st}", [(ib[3][0:64, T * 512:(T + 1) * 512], stage[0:64, st, :])], rd=[streg[st]]))
            s = WS.next("WUKV")
            wkv = wbuf[:, s, 0:2048].rearrange("p (k h c) -> p k h c", k=2, h=4)
            for h in range(4):
                for T in range(2):
                    b = nxt("pp", 2)
                    MM([mm(P[:, b, :], wkv[:, k, h, 0:128], ckvnT[:, k, T * 512:(T + 1) * 512], k == 0, k == 1) for k in range(2)],
                       rd=[wreg[s], areg["ckvnT"]], wr=[pb[b]])
                    st = nxt("st", 4)
                    OP("scalar", lambda e, b=b, st=st: e.activation(out=stage[:, st, :], in_=P[:, b, :], func=AF.Copy),
                       rd=[pb[b]], wr=[streg[st]])
                    toksB.append(DMA("sync", f"st{st}", [(ib[2][h * 128:(h + 1) * 128, T * 512:(T + 1) * 512], stage[:, st, :])],
                                     rd=[streg[st]]))
            vviewB = ib[2][512:1024, :].rearrange("r (two c) -> (r two) c", two=2)
            for j in range(NB):
                b = nxt("pp", 2)
                MM([mm(P[:, b, :].rearrange("p (h c) -> p h c", h=4), ckvnT[:, k, j * 128:(j + 1) * 128], wkv[:, k, :, 128:256], k == 0, k == 1)
                    for k in range(2)], rd=[wreg[s], areg["ckvnT"]], wr=[pb[b]])
                st = nxt("st", 4)
                OP("scalar", lambda e, b=b, st=st: e.activation(out=stage[:, st, :], in_=P[:, b, :], func=AF.Copy),
                   rd=[pb[b]], wr=[streg[st]])
                toksB.append(DMA("sync", f"st{st}", [(vviewB[j * 128:(j + 1) * 128, :], stage[:, st, :])], rd=[streg[st]]))
            collective(2, toksB)

            s = WS.next("Dkv")
            for T in range(2):
                b = proj_fm(s, 0, 128, T)
                st = nxt("st", 4)
                rope(b, 128, stage[:, st, :], [streg[st]], 32, T)
                toksD.append(DMA("sync", f"st{st}", [(ib[3][64:192, T * 512:(T + 1) * 512], stage[:, st, :])], rd=[streg[st]]))
            vviewD = ib[3][192:320, :].rearrange("r (e c) -> (r e) c", e=8)
            for j in range(NB):
                b = nxt("pp", 2)
                MM([mm(P[:, b, 0:128], xT[:, k, j * 128:(j + 1) * 128], wv(s)[:, k, 128:256], k == 0, k == 15) for k in range(16)],
                   rd=[wreg[s], xTreg], wr=[pb[b]])
                st = nxt("st", 4)
                OP("scalar", lambda e, b=b, st=st: e.activation(out=stage[:, st, 0:128], in_=P[:, b, 0:128], func=AF.Copy),
                   rd=[pb[b]], wr=[streg[st]])
                toksD.append(DMA("sync", f"st{st}", [(vviewD[j * 128:(j + 1) * 128, :], stage[:, st, 0:128])], rd=[streg[st]]))
            collective(3, toksD)

            def load_ctx_AC(ci, wait_ones=True):
                items = []
                for rr in range(2):
                    items.append((kT[:, :, rr * 1024:(rr + 1) * 1024],
                                  ob[ci][rr * 1024:rr * 1024 + 512, :].rearrange("(h d) t -> d h t", d=128)))
                DMA("sync", "ctxk", items, wr=[areg["kT"]], deps=[ccoll[ci]])
                items = []
                for rr in range(2):
                    src = ob[ci][rr * 1024 + 512:rr * 1024 + 1024, :].rearrange("r (two c) -> (r two) c", two=2)
                    for h in range(4):
                        items.append((Vaug[:, rr * 8:(rr + 1) * 8, h, 0:128],
                                      src[:, h * 128:(h + 1) * 128].rearrange("(jj p) c -> p jj c", p=128)))
                DMA("sync", "ctxv", items, wr=[areg["V"]], deps=[ccoll[ci]])
                OP("vector", lambda e: e.memset(Vaug[:, :, :, 128:129], 1.0), wr=[areg["V"]])

            def qproj_AC(nm):
                for g in range(2):
                    s = WS.next(f"{nm}q{g}")
                    for hh in range(2):
                        h = 2 * g + hh
                        for T in range(2):
                            b = proj_fm(s, hh * 128, 128, T)
                            rope(b, 128, qT[:, h, T * 512:(T + 1) * 512], [areg["qT"]], 64, T)

            smr = {k: reg("sm_" + k) for k in ("norm", "epi", "sel0", "sel1", "D", "km", "ssq")}
            ssq = sm[:, 208:240].rearrange("p (b h) -> p b h", b=4)
            yreg = reg("ytile")
            SB = (2, 3, 0, 1)

            def pipeline(items, LA):
                n = len(items)
                for i in range(n + LA):
                    if i < n:
                        items[i][0]()
                    if i >= LA:
                        items[i - LA][1]()

            def sumsq(b, col, lo, hi):
                OP("vector", lambda e: e.scalar_tensor_tensor(out=rt1[:, 0:hi - lo], in0=ytile[:, b, lo:hi], scalar=1.0, in1=ytile[:, b, lo:hi],
                                                              op0=ALU.mult, op1=ALU.mult, accum_out=ssq[:, b, col:col + 1]),
                   rd=[yreg], wr=[rtreg, smr["ssq"]])

            def normalize_po(slots, h, src4=None, srcregs=None):
                r = smr["norm"]
                if src4 is None:
                    den = po8[:, slots[0]:slots[0] + 4, 128:129]
                    rg = [por[s_] for s_ in slots]
                else:
                    den = src4[:, :, 128:129]
                    rg = srcregs
                OP("vector", lambda e: e.reciprocal(out=sm[:, 8:12].rearrange("p (b o) -> p b o", o=1), in_=den), rd=rg, wr=[r])
                for b in range(4):
                    src = po8[:, slots[b], :] if src4 is None else src4[:, b, :]
                    OP("vector", lambda e, src=src, b=b: e.tensor_scalar(out=ytile[:, b, h * 128:(h + 1) * 128], in0=src[:, 0:128],
                                                                       scalar1=sm[:, 8 + b:9 + b], scalar2=None, op0=ALU.mult),
                       rd=rg + [r], wr=[yreg])
                    sumsq(b, h, h * 128, (h + 1) * 128)

            def scores(T, h, G, isB, scale):
                kp = kap(G)
                sbk = SB[nxt("ps", 4)]
                calls = [mm(P[:, sbk, :], kT[:, h, kp * 128:(kp + 1) * 128], qT[:, h, T * 512:(T + 1) * 512], True, not isB)]
                rd = [areg["kT"], areg["qT"]]
                if isB:
                    calls.append(mm(P[:, sbk, :], krT[:, kp * 128:(kp + 1) * 128], qrT[:, h, T * 512:(T + 1) * 512], False, True))
                    rd += [areg["krT"], areg["qrT"]]
                MM(calls, rd=rd, wr=[pb[sbk]])
                ps_ = nxt("pt", 4)
                OP("scalar", lambda e: e.activation(out=pt[:, ps_, :], in_=P[:, sbk, :], func=AF.Exp, scale=scale),
                   rd=[pb[sbk]], wr=[ptreg[ps_]])
                return ps_, kp

            def mask128(ps_, b, slot):
                OP("vector", lambda e: e.tensor_tensor(out=pt[:, ps_, b * 128:(b + 1) * 128], in0=pt[:, ps_, b * 128:(b + 1) * 128],
                                                        in1=masks[:, slot, :], op=ALU.mult), rd=[ptreg[ps_], creg], wr=[ptreg[ps_]])

            def maskA(ps_, T, G):
                j0 = 4 * T
                t0 = 2 * j0 + 1 - G
                bmin = 0
                while t0 + 2 * bmin < 0:
                    bmin += 1
                slot = lambda b: TA0 + (t0 + 2 * b) * 2 + (b % 2)
                if bmin == 0:
                    mk = bass.AP(masks, slot(0) * 128, [[44 * 128, 128], [8 * 128, 2], [5 * 128, 2], [1, 128]])
                    ptv = pt[:, ps_, :].rearrange("p (u v i) -> p u v i", u=2, v=2)
                    OP("vector", lambda e: e.tensor_tensor(out=ptv, in0=ptv, in1=mk, op=ALU.mult), rd=[ptreg[ps_], creg], wr=[ptreg[ps_]])
                    return
                if bmin == 1:
                    mask128(ps_, 1, slot(1))
                if bmin <= 2:
                    mk = bass.AP(masks, slot(2) * 128, [[44 * 128, 128], [5 * 128, 2], [1, 128]])
                    ptv = pt[:, ps_, 256:512].rearrange("p (v i) -> p v i", v=2)
                    OP("vector", lambda e: e.tensor_tensor(out=ptv, in0=ptv, in1=mk, op=ALU.mult), rd=[ptreg[ps_], creg], wr=[ptreg[ps_]])
                else:
                    mask128(ps_, 3, slot(3))

            def epilogue(m, T):
                r = smr["epi"]
                if debug:
                    for b in range(4):
                        DMA("sync", "dbg", [(ydbg[(4 * T + b) * 128:(4 * T + b + 1) * 128, m * 512:(m + 1) * 512], ytile[:, b, :])], rd=[yreg])
                nh = 8 if m == 3 else 4
                OP("vector", lambda e: e.tensor_reduce(out=sm[:, 16:20], in_=ssq[:, :, 0:nh], op=ALU.add, axis=AX.X), rd=[smr["ssq"]], wr=[r])
                OP("vector", lambda e: e.tensor_scalar(out=sm[:, 20:24], in0=sm[:, 16:20], scalar1=1.0 / 512, scalar2=1e-6, op0=ALU.mult, op1=ALU.add),
                   rd=[r], wr=[r])
                OP("scalar", lambda e: e.activation(out=sm[:, 24:28], in_=sm[:, 20:24], func=AF.Ln), rd=[r], wr=[r])
                OP("scalar", lambda e: e.activation(out=sm[:, 28:32], in_=sm[:, 24:28], func=AF.Exp, scale=-0.5), rd=[r], wr=[r])
                its = []
                slots_g = [WS.next(f"G{m}{g}", la=3 - g) for g in range(2)]
                for g in range(2):
                    s = slots_g[g]
                    for b in range(4):
                        q = nxt("sgq", 2)
                        j = 4 * T + b

                        def sA(s=s, b=b, g=g, q=q, j=j):
                            bk = proj_tm(s, j, 256)
                            OP("scalar", lambda e: e.activation(out=sg[:, q, :], in_=P[:, bk, 0:256], func=AF.Silu),
                               rd=[pb[bk]], wr=[reg(f"sg{q}")])
                            OP("vector", lambda e: e.scalar_tensor_tensor(
                                out=ygb[:, q, :], in0=ytile[:, b, g * 256:(g + 1) * 256], scalar=sm[:, 28 + b:29 + b], in1=sg[:, q, :],
                                op0=ALU.mult, op1=ALU.mult), rd=[yreg, r, reg(f"sg{q}")], wr=[reg(f"yg{q}")])

                        def sB(g=g, q=q, j=j):
                            c0 = m * 4 + g * 2
                            transposes_to(lambda c: yT[:, c0 + c, j * 128:(j + 1) * 128],
                                          lambda c: ygb[:, q, c * 128:(c + 1) * 128], 2, [reg(f"yg{q}")], [reg("yT")],
                                          scale_fn=lambda c: cols[:, 21 * l + 5 + c0 + c:21 * l + 6 + c0 + c])
                        its.append((sA, sB))
                pipeline(its, 1)

            def dense_stream(m, isA, isB, scale):
                items = []
                for T in range(2):
                    j0 = 4 * T
                    nG = 8 * T + 8
                    for h in range(4):
                        st = {}
                        for G in range(nG):
                            d = {}

                            def s1(T=T, h=h, G=G, d=d, st=st, j0=j0):
                                if G == 0:
                                    ob_ = nxt("po", 2)
                                    st["slots"] = [ob_ * 4 + b for b in range(4)]
                                d["ps"], d["kp"] = scores(T, h, G, isB, scale)
                                if isA:
                                    maskA(d["ps"], T, G)
                                else:
                                    for b in range(4):
                                        j = j0 + b
                                        if 2 * j <= G <= 2 * j + 1:
                                            mask128(d["ps"], b, MB0 + (j % 2) * 2 + (G - 2 * j))

                            def s2(T=T, h=h, G=G, d=d, st=st, j0=j0, nG=nG):
                                slots = st["slots"]
                                calls, wr = [], []
                                for b in range(4):
                                    j = j0 + b
                                    if G <= 2 * j + 1:
                                        calls.append(lambda e, b=b, j=j: e.matmul(
                                            po8[:, slots[b], 0:129], lhsT=pt[:, d["ps"], b * 128:(b + 1) * 128], rhs=Vaug[:, d["kp"], h, :],
                                            start=(G == 0 and b % 2 == 0), stop=(G == 2 * j + 1), skip_group_check=True))
                                        wr.append(por[slots[b]])
                                MM(calls, rd=[ptreg[d["ps"]], areg["V"]], wr=wr)
                                if G == nG - 1:
                                    normalize_po(slots, h)
                                    if h == 3:
                                        epilogue(m, T)
                            items.append((s1, s2))
                pipeline(items, 2)

            def zero_ssq():
                OP("vector", lambda e: e.memset(sm[:, 208:240], 0.0), wr=[smr["ssq"]])

            load_ctx_AC(0)
            qproj_AC("A")
            zero_ssq()
            dense_stream(0, True, False, 128 ** -0.5)

            load_ctx_AC(1)
            zero_ssq()
            r = smr["km"]
            for h in range(4):
                OP("vector", lambda e, h=h: e.tensor_reduce(out=sm[:, 32:48], in_=kT[:, h, :].rearrange("p (k t) -> p k t", k=16),
                                                            op=ALU.add, axis=AX.X), rd=[areg["kT"]], wr=[r])
                OP("vector", lambda e: e.tensor_tensor(out=sm[:, 48:56], in0=sm[:, 32:40], in1=sm[:, 40:48], op=ALU.add), rd=[r], wr=[r])
                OP("vector", lambda e, h=h: e.tensor_scalar(out=kmean[:, h, :], in0=sm[:, 48:56], scalar1=1.0 / 256, scalar2=None, op0=ALU.mult),
                   rd=[r], wr=[areg["kmean"]])
            qproj_AC("C")
            selbuf = [sm[:, 64:96].rearrange("p (b n) -> p b n", b=4), sm[:, 144:176].rearrange("p (b n) -> p b n", b=4)]
            gmv = sm[:, 96:128].rearrange("p (b n) -> p b n", b=4)
            top = sm[:, 176:208].rearrange("p (b n) -> p b n", b=4)
            gmr = reg("sm_gm")
            accr = reg("accC")
            items = []
            grp = 0
            for T in range(2):
                j0 = 4 * T
                for h in range(4):
                    st = {"first": [True] * 4}
                    sel = selbuf[grp % 2]
                    selr = smr[f"sel{grp % 2}"]
                    grp += 1
                    nN = j0 + 4
                    for n in range(nN):
                        d = {}

                        def s1(T=T, h=h, n=n, d=d, j0=j0, sel=sel, selr=selr):
                            if n == 0:
                                bk = nxt("pp", 2)
                                MM([mm(P[:, bk, b * 8:(b + 1) * 8], qT[:, h, (j0 + b) * 128:(j0 + b + 1) * 128], kmean[:, h, :], True, True)
                                    for b in range(4)], rd=[areg["qT"], areg["kmean"]], wr=[pb[bk]])
                                OP("vector", lambda e: e.tensor_tensor(out=gmv, in0=P[:, bk, 0:32].rearrange("p (b n) -> p b n", b=4),
                                                                       in1=negm[:, j0:j0 + 4, :], op=ALU.add), rd=[pb[bk], creg], wr=[gmr])
                                for b in range(4):
                                    OP("vector", lambda e, b=b: e.max(out=top[:, b, :], in_=gmv[:, b, :]), rd=[gmr], wr=[gmr])
                                for b in range(4):
                                    OP("vector", lambda e, b=b: e.tensor_scalar(out=sel[:, b, :], in0=gmv[:, b, :], scalar1=top[:, b, 2:3], scalar2=None,
                                                                                op0=ALU.is_ge), rd=[gmr], wr=[selr])
                            d["pss"] = []
                            for w in range(2):
                                ps_, kp = scores(T, h, 2 * n + w, False, 128 ** -0.5)
                                for b in range(4):
                                    if j0 + b == n:
                                        mask128(ps_, b, MB0 + (n % 2) * 2 + w)
                                d["pss"].append((ps_, kp))

                        def s2(T=T, h=h, n=n, d=d, j0=j0, sel=sel, selr=selr, st=st, nN=nN):
                            ob_ = nxt("po", 2)
                            slots = [ob_ * 4 + b for b in range(4)]
                            pss = d["pss"]
                            calls, wr = [], []
                            for b in range(4):
                                if j0 + b >= n:
                                    for w in range(2):
                                        ps_, kp = pss[w]
                                        calls.append(mm(po8[:, slots[b], 0:129], pt[:, ps_, b * 128:(b + 1) * 128], Vaug[:, kp, h, :], w == 0, w == 1))
                                    wr.append(por[slots[b]])
                            MM(calls, rd=[ptreg[pss[0][0]], ptreg[pss[1][0]], areg["V"]], wr=wr)
                            first = st["first"]
                            for b in range(4):
                                j = j0 + b
                                if j < n:
                                    continue
                                src = po8[:, slots[b], 0:129]
                                dst = accC[:, b, 0:129]
                                if j == n:
                                    if first[b]:
                                        OP("vector", lambda e, src=src, dst=dst: e.tensor_copy(out=dst, in_=src), rd=[por[slots[b]]], wr=[accr])
                                    else:
                                        OP("vector", lambda e, src=src, dst=dst: e.tensor_tensor(out=dst, in0=src, in1=dst, op=ALU.add),
                                           rd=[por[slots[b]]], wr=[accr])
                                else:
                                    if first[b]:
                                        OP("vector", lambda e, src=src, dst=dst, b=b: e.tensor_scalar(
                                            out=dst, in0=src, scalar1=sel[:, b, n:n + 1], scalar2=None, op0=ALU.mult),
                                           rd=[por[slots[b]], selr], wr=[accr])
                                    else:
                                        OP("vector", lambda e, src=src, dst=dst, b=b: e.scalar_tensor_tensor(
                                            out=dst, in0=src, scalar=sel[:, b, n:n + 1], in1=dst, op0=ALU.mult, op1=ALU.add),
                                           rd=[por[slots[b]], selr], wr=[accr])
                                first[b] = False
                            if n == nN - 1:
                                normalize_po(None, h, src4=accC, srcregs=[accr])
                                if h == 3:
                                    epilogue(2, T)
                        items.append((s1, s2))
            pipeline(items, 1)

            load_ctx_AC(2)
            DMA("sync", "ctxr", [(krT[0:64, rr * 1024:(rr + 1) * 1024], ob[3][rr * 320:rr * 320 + 64, :]) for rr in range(2)],
                wr=[areg["krT"]], deps=[ccoll[3]])
            OP("gpsimd", lambda e: e.memset(krT[64:128, :], 0.0), wr=[areg["krT"]])
            OP("gpsimd", lambda e: e.memset(qrT[64:128, :, :], 0.0), wr=[areg["qrT"]])
            s0 = WS.next("Bcq0")
            s1_ = WS.next("Bcq1", la=2)
            for j in range(NB):
                bk = nxt("pp", 2)
                calls = [mm(P[:, bk, 0:256], xT[:, k, j * 128:(j + 1) * 128], wv(s0)[:, k, 0:256], k == 0, k == 15) for k in range(16)]
                calls += [mm(P[:, bk, 256:384], xT[:, k, j * 128:(j + 1) * 128], wv(s1_)[:, k, 0:128], k == 0, k == 15) for k in range(16)]
                MM(calls, rd=[wreg[s0], wreg[s1_], xTreg], wr=[pb[bk]])
                rmsnorm_rows(bk, 384, cqb[:, 0:384], [reg("cqb")])
                transposes_to(lambda c, j=j: cqnT[:, c, j * 128:(j + 1) * 128], lambda c: cqb[:, c * 128:(c + 1) * 128], 3,
                              [reg("cqb")], [areg["cqnT"]], scale_fn=lambda c: cols[:, 21 * l + c:21 * l + 1 + c])
            s = WS.next("WUQ")
            wq = wbuf[:, s, 0:2304].rearrange("p (k h c) -> p k h c", k=3, h=4)
            for h in range(4):
                for T in range(2):
                    bk = nxt("pp", 2)
                    MM([mm(P[:, bk, :], wq[:, k, h, 0:128], cqnT[:, k, T * 512:(T + 1) * 512], k == 0, k == 2) for k in range(3)],
                       rd=[wreg[s], areg["cqnT"]], wr=[pb[bk]])
                    OP("scalar", lambda e, bk=bk, h=h, T=T: e.activation(out=qT[:, h, T * 512:(T + 1) * 512], in_=P[:, bk, :], func=AF.Copy),
                       rd=[pb[bk]], wr=[areg["qT"]])
                    bk = nxt("pp", 2)
                    MM([mm(P[0:64, bk, :], wq[:, k, h, 128:192], cqnT[:, k, T * 512:(T + 1) * 512], k == 0, k == 2) for k in range(3)],
                       rd=[wreg[s], areg["cqnT"]], wr=[pb[bk]])
                    rope(bk, 64, qrT[0:64, h, T * 512:(T + 1) * 512], [areg["qrT"]], 32, T)
            zero_ssq()
            dense_stream(1, False, True, 192 ** -0.5)

            tokB = []
            for rg in areg.values():
                tokB.extend(rg.wdeps())
            dreg = {n: Reg() for n in ("kT", "V", "qT")}
            for rg in dreg.values():
                for t in tokB:
                    rg.read(t)
            kTD = av(24512, 2048)
            VD = av(26560, 2080).rearrange("p (k h c) -> p k h c", k=16, h=2)
            qTD = av(28640, 4096).rearrange("p (h t) -> p h t", h=4)
            items = [(kTD[:, rr * 1024:(rr + 1) * 1024], ob[3][rr * 320 + 64:rr * 320 + 192, :]) for rr in range(2)]
            DMA("sync", "ctxk", items, wr=[dreg["kT"]], deps=[ccoll[3]])
            items = []
            for rr in range(2):
                src = ob[3][rr * 320 + 192:rr * 320 + 320, :].rearrange("r (e c) -> (r e) c", e=8)
                for hh in range(2):
                    items.append((VD[:, rr * 8:(rr + 1) * 8, hh, 0:64], src[:, hh * 64:(hh + 1) * 64].rearrange("(jj p) c -> p jj c", p=128)))
            DMA("sync", "ctxv", items, wr=[dreg["V"]], deps=[ccoll[3]])
            OP("vector", lambda e: e.memset(VD[:, :, :, 64:65], 1.0), wr=[dreg["V"]])
            for g in range(2):
                s = WS.next(f"Dq{g}")
                for cc in range(2):
                    c = 2 * g + cc
                    for T in range(2):
                        bk = proj_fm(s, cc * 128, 128, T)
                        rope(bk, 128, qTD[:, c, T * 512:(T + 1) * 512], [dreg["qT"]], 32, T)
            WS.prefetch()
            DMA("gpsimd", "wo", [(wo[:, 0:11, q * 512:(q + 1) * 512],
                                  w_out[l, 0:11 * 128, q * 512:(q + 1) * 512].rearrange("(k p) c -> p k c", p=128)) for q in range(4)],
                wr=[reg("wo_lo")], deps=tokB)
            zero_ssq()
            r = smr["D"]
            items = []
            for T in range(2):
                for b in range(4):
                    for c in range(4):
                        for sl in range(2):
                            d = {}
                            j = 4 * T + b
                            Gs = [G for G in (2 * j - 1, 2 * j, 2 * j + 1) if G >= 0]

                            def s1(j=j, c=c, sl=sl, d=d, Gs=Gs):
                                w0 = 3 - len(Gs)
                                prt = slice(64 * sl, 64 * sl + 64)
                                sbk = SB[nxt("ps", 4)]
                                MM([mm(P[:, sbk, (w0 + i) * 128:(w0 + i + 1) * 128], kTD[prt, kap(G) * 128:(kap(G) + 1) * 128],
                                       qTD[prt, c, j * 128:(j + 1) * 128], True, True) for i, G in enumerate(Gs)],
                                   rd=[dreg["kT"], dreg["qT"]], wr=[pb[sbk]])
                                ps_ = nxt("pt", 4)
                                d["ps"] = ps_
                                OP("scalar", lambda e: e.activation(out=pt[:, ps_, w0 * 128:384], in_=P[:, sbk, w0 * 128:384],
                                                                    func=AF.Exp, scale=0.125), rd=[pb[sbk]], wr=[ptreg[ps_]])
                                OP("vector", lambda e: e.tensor_tensor(
                                    out=pt[:, ps_, w0 * 128:384], in0=pt[:, ps_, w0 * 128:384],
                                    in1=masks[:, MD0 + (j % 2) * 3 + w0:MD0 + (j % 2) * 3 + 3, :].rearrange("p a b -> p (a b)"), op=ALU.mult),
                                   rd=[ptreg[ps_], creg], wr=[ptreg[ps_]])

                            def s2(T=T, b=b, c=c, sl=sl, d=d, Gs=Gs, l=l):
                                w0 = 3 - len(Gs)
                                ps_ = d["ps"]
                                hd = c + 4 * sl
                                slot = nxt("po8", 8)
                                MM([mm(po8[:, slot, 0:65], pt[:, ps_, (w0 + i) * 128:(w0 + i + 1) * 128], VD[:, kap(G), sl, :], i == 0, i == len(Gs) - 1)
                                    for i, G in enumerate(Gs)], rd=[ptreg[ps_], dreg["V"]], wr=[por[slot]])
                                u = 240 + (hd % 8)
                                OP("vector", lambda e: e.tensor_tensor(out=sm[:, u:u + 1], in0=po8[:, slot, 64:65],
                                                                       in1=esink[:, 8 * l + hd:8 * l + hd + 1], op=ALU.add),
                                   rd=[por[slot], reg("esink")], wr=[r])
                                OP("vector", lambda e: e.reciprocal(out=sm[:, u + 8:u + 9], in_=sm[:, u:u + 1]), rd=[r], wr=[r])
                                OP("vector", lambda e: e.tensor_scalar(out=ytile[:, b, hd * 64:(hd + 1) * 64], in0=po8[:, slot, 0:64],
                                                                       scalar1=sm[:, u + 8:u + 9], scalar2=None, op0=ALU.mult),
                                   rd=[por[slot], r], wr=[yreg])
                                sumsq(b, hd, hd * 64, (hd + 1) * 64)
                                if b == 3 and c == 3 and sl == 1:
                                    epilogue(3, T)
                            items.append((s1, s2))
            pipeline(items, 2)

            S.barrier()
            DMA("gpsimd", "wo", [(wo[:, 11:16, q * 512:(q + 1) * 512],
                                  w_out[l, 11 * 128:16 * 128, q * 512:(q + 1) * 512].rearrange("(k p) c -> p k c", p=128)) for q in range(4)],
                wr=[reg("wo_hi")])
            DMA("sync", "gb", [(gam, ln_g[l:l + 1, :].partition_broadcast(128)[:, 0, :]),
                               (bet, ln_b[l:l + 1, :].partition_broadcast(128)[:, 0, :])], wr=[wreg[0], wreg[1]])
            xsrc = x_in if l == 0 else xs[(l - 1) % 2]
            xdst = out_d if l == depth - 1 else xs[l % 2]
            zreg = reg("ytile")
            r = reg("sm")
            for j in range(NB):
                DMA("sync", "xr", [(xr, xsrc[j * 128:(j + 1) * 128, :])], wr=[wreg[2]])
                for hf in range(2):
                    for q in (2 * hf, 2 * hf + 1):
                        MM([mm(P[:, 4 + q, :], yT[:, k, j * 128:(j + 1) * 128], wo[:, k, q * 512:(q + 1) * 512], k == 0, False) for k in range(11)],
                           rd=[reg("wo_lo"), reg("yT")], wr=[por[2 * q], por[2 * q + 1]])
                    for q in (2 * hf, 2 * hf + 1):
                        MM([mm(P[:, 4 + q, :], yT[:, k, j * 128:(j + 1) * 128], wo[:, k, q * 512:(q + 1) * 512], False, k == 15) for k in range(11, 16)],
                           rd=[reg("wo_hi"), reg("yT")], wr=[por[2 * q], por[2 * q + 1]])
                    for q in (2 * hf, 2 * hf + 1):
                        OP("vector", lambda e, q=q: e.scalar_tensor_tensor(out=zt[:, q * 512:(q + 1) * 512], in0=xr[:, q * 512:(q + 1) * 512], scalar=ALPHA,
                                                                          in1=P[:, 4 + q, :], op0=ALU.mult, op1=ALU.add),
                           rd=[wreg[2], por[2 * q], por[2 * q + 1]], wr=[zreg])
                        OP("vector", lambda e, q=q: e.bn_stats(out=sm[:, 176 + 6 * q:182 + 6 * q], in_=zt[:, q * 512:(q + 1) * 512]), rd=[zreg], wr=[r])
                OP("vector", lambda e: e.bn_aggr(out=sm[:, 200:202], in_=sm[:, 176:200]), rd=[r], wr=[r])
                OP("vector", lambda e: e.tensor_scalar(out=sm[:, 202:203], in0=sm[:, 201:202], scalar1=1e-5, scalar2=None, op0=ALU.add), rd=[r], wr=[r])
                OP("scalar", lambda e: e.activation(out=sm[:, 203:204], in_=sm[:, 202:203], func=AF.Ln), rd=[r], wr=[r])
                OP("scalar", lambda e: e.activation(out=sm[:, 204:205], in_=sm[:, 203:204], func=AF.Exp, scale=-0.5), rd=[r], wr=[r])
                OP("vector", lambda e: e.scalar_tensor_tensor(out=zt, in0=zt, scalar=sm[:, 200:201], in1=gam, op0=ALU.subtract, op1=ALU.mult),
                   rd=[r, zreg, wreg[0]], wr=[zreg])
                OP("vector", lambda e: e.scalar_tensor_tensor(out=zt, in0=zt, scalar=sm[:, 204:205], in1=bet, op0=ALU.mult, op1=ALU.add),
                   rd=[r, zreg, wreg[1]], wr=[zreg])
                DMA("sync", "xo", [(xdst[j * 128:(j + 1) * 128, :], zt)], rd=[zreg])
                if l < depth - 1:
                    block_to_xT(zt, zreg, j)
            S.barrier()

        S.emit()
    return nc


def _mult(delta):
    m = np.zeros_like(delta, dtype=np.float32)
    ok = delta >= 0
    m += (ok & (delta <= 128))
    m += (ok & (delta % 4 == 0) & (delta <= 512))
    m += (ok & (delta % 16 == 0) & (delta <= 2048))
    return m


def host_tables(r):
    i = np.arange(128)[None, :]
    k = np.arange(128)[:, None]
    masks = np.zeros((128, 44, 128), np.float32)
    for jpar in range(2):
        e = (jpar + r) % 2
        for t in range(17):
            masks[:, t * 2 + jpar, :] = _mult((t - 1 + e) * 128 + i - k)
        for w in range(2):
            masks[:, 34 + jpar * 2 + w, :] = (((e - w) * 128 + i - k) >= 0)
        for w in range(3):
            dd = (e + 1 - w) * 128 + i - k
            masks[:, 38 + jpar * 3 + w, :] = ((dd >= 0) & (dd <= 127))
    pos = np.concatenate([gblk(r, j) * 128 + np.arange(128) for j in range(NB)]).astype(np.float32)
    tabs = np.zeros((128, 4, 1024), np.float32)
    p = np.arange(128)
    inv128 = (np.float32(10000.0) ** (-np.arange(0, 128, 2, dtype=np.float32) / np.float32(128))).astype(np.float32)
    inv64 = (np.float32(10000.0) ** (-np.arange(0, 64, 2, dtype=np.float32) / np.float32(64))).astype(np.float32)
    a128 = (pos[None, :] * inv128[p % 64][:, None]).astype(np.float32)
    a64 = (pos[None, :] * inv64[(p % 64) % 32][:, None]).astype(np.float32)
    tabs[:, 0] = np.cos(a128)
    tabs[:, 1] = np.sin(a128) * np.where(p < 64, -1.0, 1.0)[:, None]
    tabs[:, 2] = np.cos(a64)
    tabs[:, 3] = np.sin(a64) * np.where((p % 64) < 32, -1.0, 1.0)[:, None]
    return masks.astype(ml_dtypes.bfloat16), tabs


def host_inputs(x, w_in, q_norm, w_uq, kv_norm, w_ukv, sinks, branch_norm, w_out, ln_gamma, ln_beta):
    f = lambda a: np.ascontiguousarray(np.asarray(a, dtype=np.float32))
    x, w_in, w_uq, w_ukv, w_out = f(x), f(w_in), f(w_uq), f(w_ukv), f(w_out)
    q_norm, kv_norm, sinks, branch_norm, ln_gamma, ln_beta = f(q_norm), f(kv_norm), f(sinks), f(branch_norm), f(ln_gamma), f(ln_beta)
    cols = np.zeros((128, 84), np.float32)
    for l in range(DEPTH):
        cols[:, 21 * l:21 * l + 3] = q_norm[l].reshape(3, 128).T
        cols[:, 21 * l + 3:21 * l + 5] = kv_norm[l].reshape(2, 128).T
        cols[:, 21 * l + 5:21 * l + 21] = branch_norm[l].reshape(16, 128).T
    sinks_bc = np.ascontiguousarray(np.broadcast_to(sinks.reshape(1, 32), (128, 32)))
    negm = np.zeros((128, 8, 8), np.float32)
    for j in range(8):
        negm[:, j, j:] = NEG
    negm = negm.reshape(128, 64)
    ident = np.eye(128, dtype=np.float32).astype(ml_dtypes.bfloat16)
    tb = [host_tables(r) for r in range(2)]
    in_maps = []
    for c in range(8):
        b, r = c // 2, c % 2
        xo = np.concatenate([x[b, gblk(r, j) * 128:(gblk(r, j) + 1) * 128, :] for j in range(NB)], 0)
        in_maps.append({"x": np.ascontiguousarray(xo), "w_in": w_in, "w_uq": w_uq, "w_ukv": w_ukv, "w_out": w_out,
                        "ln_gamma": ln_gamma, "ln_beta": ln_beta, "cols": cols, "sinks_bc": sinks_bc, "negm": negm,
                        "tabs": tb[r][1], "masks": tb[r][0], "ident": ident})
    return in_maps


def assemble(results, key="out"):
    out = np.zeros((4, 2048, D), np.float32)
    for c in range(8):
        b, r = c // 2, c % 2
        o = np.asarray(results[c][key])
        for j in range(NB):
            g = gblk(r, j)
            out[b, g * 128:(g + 1) * 128, :] = o[j * 128:(j + 1) * 128, :]
    return out


_NC = {}


def kernel(x, w_in, q_norm, w_uq, kv_norm, w_ukv, sinks, branch_norm, w_out, ln_gamma, ln_beta):
    in_maps = host_inputs(x, w_in, q_norm, w_uq, kv_norm, w_ukv, sinks, branch_norm, w_out, ln_gamma, ln_beta)
    if "nc" not in _NC:
        _NC["nc"] = build(DEPTH, False)
    res = run_bass_kernel_spmd(_NC["nc"], in_maps, core_ids=list(range(8)))
    return assemble(res.results)
```

```python
import contextlib
import numpy as np
import ml_dtypes
import concourse.bass as bass
import concourse.mybir as mybir
from concourse.bass_utils import run_bass_kernel_spmd

F32 = mybir.dt.float32
BF16 = mybir.dt.bfloat16
AF = mybir.ActivationFunctionType
ALU = mybir.AluOpType
AX = mybir.AxisListType

D = 2048
NIN = 6592
NOWN = 1024
NB = 8
DEPTH = 4
ALPHA = (2 * DEPTH) ** 0.25
O_AQ, O_AK, O_AV = 0, 512, 1024
O_BCQ, O_BCKV, O_BKR = 1536, 1920, 2176
O_CQ, O_CK, O_CV = 2240, 2752, 3264
O_DQ, O_DK, O_DV = 3776, 4288, 4416
O_G = 4544
NEG = -1e30
PAIRS = [[0, 1], [2, 3], [4, 5], [6, 7]]


def gblk(r, j):
    return 2 * j + (j + r) % 2


def kap(G):
    jj = G // 2
    rr = (G % 2 - jj) % 2
    return 8 * rr + jj


class Sched:
    ENGS = ("sync", "scalar", "vector", "gpsimd", "tensor")

    def __init__(self, nc):
        self.nc = nc
        self.ops = {e: [] for e in self.ENGS}
        self.sems = {}
        self.cnt = {}
        self.mult = {}
        self.waited = {e: {} for e in self.ENGS}
        self._cms = []

    def sem(self, key, mult=1):
        if key not in self.sems:
            cm = self.nc.semaphore(key)
            self.sems[key] = cm.__enter__()
            self._cms.append(cm)
            self.cnt[key] = 0
            self.mult[key] = mult
        return self.sems[key]

    def wait(self, eng, toks):
        for t in toks:
            if t is None:
                continue
            key, n = t
            w = self.waited[eng]
            if w.get(key, 0) >= n:
                continue
            w[key] = n
            h = self.sems[key]
            self.ops[eng].append(lambda e, h=h, v=n * self.mult[key]: e.wait_ge(h, v))

    def op(self, eng, fn, deps=(), sig=True, key=None):
        self.wait(eng, deps)
        if sig:
            key = key or ("c_" + eng)
            h = self.sem(key)
            self.cnt[key] += 1
            self.ops[eng].append(lambda e, fn=fn, h=h: fn(e).then_inc(h, 1))
            return (key, self.cnt[key])
        self.ops[eng].append(lambda e, fn=fn: fn(e))
        return None

    def dma(self, eng, chan, items, deps=()):
        key = "d_" + chan
        h = self.sem(key, 16)
        if self.cnt[key]:
            self.wait(eng, [(key, self.cnt[key])])
        self.wait(eng, deps)
        for (o, i, kw) in items:
            self.cnt[key] += 1
            self.ops[eng].append(lambda e, o=o, i=i, kw=kw, h=h: e.dma_start(out=o, in_=i, **kw).then_inc(h, 16))
        return (key, self.cnt[key])

    def barrier(self):
        toks = [(k, c) for k, c in self.cnt.items() if c > 0]
        for e in self.ENGS:
            self.wait(e, toks)

    def emit(self):
        self.barrier()
        with self.nc.Block() as block:
            @block.sync
            def _(e):
                for f in self.ops["sync"]:
                    f(e)

            @block.scalar
            def _(e):
                for f in self.ops["scalar"]:
                    f(e)

            @block.vector
            def _(e):
                for f in self.ops["vector"]:
                    f(e)

            @block.gpsimd
            def _(e):
                for f in self.ops["gpsimd"]:
                    f(e)

            @block.tensor
            def _(e):
                for f in self.ops["tensor"]:
                    f(e)
        for cm in reversed(self._cms):
            cm.__exit__(None, None, None)


class Reg:
    def __init__(self):
        self.w = None
        self.r = {}

    def rdeps(self):
        return [self.w] if self.w else []

    def wdeps(self):
        return ([self.w] if self.w else []) + list(self.r.values())

    def read(self, tok):
        if tok is None:
            return
        k = tok[0]
        if k not in self.r or self.r[k][1] < tok[1]:
            self.r[k] = tok

    def write(self, tok):
        self.w = tok
        self.r = {}


def build(depth=DEPTH, debug=False):
    nc = bass.Bass("TRN2", target_bir_lowering=False)
    S = Sched(nc)
    dt_in = lambda name, shape, dt=F32: nc.dram_tensor(name, shape, dt, kind="ExternalInput")
    x_in = dt_in("x", [NOWN, D])
    w_in = dt_in("w_in", [DEPTH, D, NIN])
    w_uq = dt_in("w_uq", [DEPTH, 384, 768])
    w_ukv = dt_in("w_ukv", [DEPTH, 256, 1024])
    w_out = dt_in("w_out", [DEPTH, D, D])
    ln_g = dt_in("ln_gamma", [DEPTH, D])
    ln_b = dt_in("ln_beta", [DEPTH, D])
    cols_d = dt_in("cols", [128, 84])
    sinks_d = dt_in("sinks_bc", [128, 32])
    negm_d = dt_in("negm", [128, 64])
    tabs_d = dt_in("tabs", [128, 4, 1024])
    masks_d = dt_in("masks", [128, 44, 128], BF16)
    ident_d = dt_in("ident", [128, 128], BF16)
    out_d = nc.dram_tensor("out", [NOWN, D], F32, kind="ExternalOutput")
    if debug:
        ydbg = nc.dram_tensor("ydbg", [NOWN, D], F32, kind="ExternalOutput")
    xs = [nc.dram_tensor("xs0", [NOWN, D], F32), nc.dram_tensor("xs1", [NOWN, D], F32)]
    ibs = [[nc.dram_tensor(f"ib{l}_{i}", [320 if i == 3 else 1024, 1024], BF16) for i in range(4)] for l in range(depth)]
    obs = [[nc.dram_tensor(f"ob{l}_{i}", [640 if i == 3 else 2048, 1024], BF16) for i in range(4)] for l in range(depth)]

    es = contextlib.ExitStack()
    with es:
        sb = lambda name, shape, dt: es.enter_context(nc.sbuf_tensor("sb_" + name, shape, dt))
        xT = sb("xT", [128, 16, NOWN], BF16)
        yT = sb("yT", [128, 16, NOWN], BF16)
        arena = sb("arena", [128, 32768], BF16)
        wbuf = sb("wbuf", [128, 3, 4096], BF16)
        stage = sb("stage", [128, 4, 512], BF16)
        tabs = sb("tabs", [128, 4, 1024], F32)
        masks = sb("masks", [128, 44, 128], BF16)
        ident = sb("ident", [128, 128], BF16)
        pt = sb("pt", [128, 4, 512], BF16)
        ytile = sb("ytile", [128, 4, 512], F32)
        rt1 = sb("rt1", [128, 512], F32)
        rt2 = sb("rt2", [128, 512], F32)
        cols = sb("cols", [128, 84], F32)
        esink = sb("esink", [128, 32], F32)
        negm = sb("negm", [128, 8, 8], F32)
        sm = sb("sm", [128, 256], F32)
        accC = sb("accC", [128, 4, 132], F32)
        sg = sb("sg", [128, 2, 256], F32)
        ygb = sb("ygb", [128, 2, 256], BF16)
        cqb = sb("cqb", [128, 2, 384], BF16)
        P = es.enter_context(nc.psum_tensor("P", [128, 8, 512], F32))

        R = {}
        def reg(name):
            if name not in R:
                R[name] = Reg()
            return R[name]
        pb = [reg(f"pb{i}") for i in range(4)]
        por = [reg(f"po{i}") for i in range(8)]
        wreg = [reg(f"w{i}") for i in range(3)]
        streg = [reg(f"st{i}") for i in range(4)]
        ptreg = [reg(f"pt{i}") for i in range(4)]

        po8 = P[:, 4:8, :].rearrange("p k (b c) -> p (k b) c", c=256)

        def av(off, n):
            return arena[:, off:off + n]
        kT = av(0, 8192).rearrange("p (h t) -> p h t", h=4)
        Vaug = av(8192, 8256).rearrange("p (k h c) -> p k h c", k=16, h=4)
        VD = av(8192, 2080).rearrange("p (k h c) -> p k h c", k=16, h=2)
        qT = av(16448, 4096).rearrange("p (h t) -> p h t", h=4)
        qrT = av(20544, 4096).rearrange("p (h t) -> p h t", h=4)
        krT = av(24640, 2048)
        cqnT = av(26688, 3072).rearrange("p (k t) -> p k t", k=3)
        kmean = av(29760, 32).rearrange("p (h n) -> p h n", h=4)
        ckvnT = av(29792, 2048).rearrange("p (k t) -> p k t", k=2)
        wo = arena[:, :].rearrange("p (k c) -> p k c", k=16)
        gam = wbuf[:, 0, :].bitcast(F32)
        bet = wbuf[:, 1, :].bitcast(F32)
        xr = wbuf[:, 2, :].bitcast(F32)
        zt = ytile[:, :, :].rearrange("p a b -> p (a b)")
        xb = stage[:, :, :].rearrange("p a b -> p (a b)")

        def OP(eng, fn, rd=(), wr=(), deps=()):
            d = list(deps)
            for t in rd:
                d.extend(t.rdeps())
            for t in wr:
                d.extend(t.wdeps())
            tok = S.op(eng, fn, d)
            for t in rd:
                t.read(tok)
            for t in wr:
                t.write(tok)
            return tok

        def DMA(eng, chan, items, rd=(), wr=(), deps=()):
            d = list(deps)
            for t in rd:
                d.extend(t.rdeps())
            for t in wr:
                d.extend(t.wdeps())
            tok = S.dma(eng, chan, [(o, i, {}) for (o, i) in items], d)
            for t in rd:
                t.read(tok)
            for t in wr:
                t.write(tok)
            return tok

        def MM(calls, rd=(), wr=()):
            d = []
            for t in rd:
                d.extend(t.rdeps())
            for t in wr:
                d.extend(t.wdeps())
            S.wait("tensor", d)
            for f in calls[:-1]:
                S.op("tensor", f, sig=False)
            tok = S.op("tensor", calls[-1])
            for t in rd:
                t.read(tok)
            for t in wr:
                t.write(tok)
            return tok

        def mm(out, lhsT, rhs, start, stop):
            return lambda e: e.matmul(out, lhsT=lhsT, rhs=rhs, start=start, stop=stop)

        creg = reg("const")
        DMA("sync", "const", [(tabs[:], tabs_d[:, :, :]), (masks[:], masks_d[:, :, :]), (ident[:], ident_d[:, :]),
                              (cols[:], cols_d[:, :]), (esink[:], sinks_d[:, :]),
                              (negm[:].rearrange("p a b -> p (a b)"), negm_d[:, :])], wr=[creg])
        OP("scalar", lambda e: e.activation(out=esink[:], in_=esink[:], func=AF.Exp), rd=[creg], wr=[reg("esink")])
        S.barrier()
        cos128, ssin128, cos64, ssin64 = (tabs[:, i, :] for i in range(4))
        TA0 = 0
        MB0 = 34
        MD0 = 38

        wstate = {"i": 0}

        def wload(items, name):
            s = wstate["i"] % 3
            wstate["i"] += 1
            DMA("gpsimd", f"w{s}", items(s), wr=[wreg[s]])
            return s

        def wv(s):
            return wbuf[:, s, :].rearrange("p (k c) -> p k c", c=256)

        def std_items(l, off, n):
            return lambda s: [(wv(s)[:, :, 0:n], w_in[l, :, off:off + n].rearrange("(k p) c -> p k c", p=128))]

        class WQ:
            def __init__(self, specs):
                self.specs = specs
                self.issued = 0
                self.slots = []
                self.pos = 0

            def _issue(self):
                name, items = self.specs[self.issued]
                self.slots.append(wload(items, name))
                self.issued += 1

            def next(self, name, la=3):
                assert self.specs[self.pos][0] == name, (self.specs[self.pos][0], name)
                while self.issued < min(len(self.specs), self.pos + la):
                    self._issue()
                s = self.slots[self.pos]
                self.pos += 1
                return s

            def prefetch(self):
                while self.issued < min(len(self.specs), self.pos + 3):
                    self._issue()

        def layer_specs(l):
            sp = []
            for nm, off in (("Ak", O_AK), ("Av", O_AV), ("Ck", O_CK), ("Cv", O_CV)):
                for g in range(2):
                    sp.append((f"{nm}{g}", std_items(l, off + 256 * g, 256)))
            sp.append(("Bkv", std_items(l, O_BCKV, 256)))
            sp.append(("Bkr", std_items(l, O_BKR, 64)))
            sp.append(("WUKV", lambda s: [(wbuf[:, s, 0:2048].rearrange("p (k c) -> p k c", k=2),
                                           w_ukv[l, :, :].rearrange("(k p) c -> p k c", p=128))]))
            sp.append(("Dkv", std_items(l, O_DK, 256)))

            def gates(m):
                r = []
                for T in range(2):
                    for g in range(2):
                        r.append((f"G{m}{g}", std_items(l, O_G + 512 * m + 256 * g, 256)))
                return r
            sp += [("Aq0", std_items(l, O_AQ, 256)), ("Aq1", std_items(l, O_AQ + 256, 256))] + gates(0)
            sp += [("Cq0", std_items(l, O_CQ, 256)), ("Cq1", std_items(l, O_CQ + 256, 256))] + gates(2)
            sp += [("Bcq0", std_items(l, O_BCQ, 256)), ("Bcq1", std_items(l, O_BCQ + 256, 128)),
                   ("WUQ", lambda s: [(wbuf[:, s, 0:2304].rearrange("p (k c) -> p k c", k=3),
                                       w_uq[l, :, :].rearrange("(k p) c -> p k c", p=128))])] + gates(1)

            def dq_items(cp):
                def f(s):
                    it = []
                    for i in range(2):
                        c = 2 * cp + i
                        it.append((wv(s)[:, :, 128 * i:128 * i + 64],
                                   w_in[l, :, O_DQ + 64 * c:O_DQ + 64 * c + 64].rearrange("(k p) c -> p k c", p=128)))
                        it.append((wv(s)[:, :, 128 * i + 64:128 * i + 128],
                                   w_in[l, :, O_DQ + 64 * (4 + c):O_DQ + 64 * (4 + c) + 64].rearrange("(k p) c -> p k c", p=128)))
                    return it
                return f
            sp += [("Dq0", dq_items(0)), ("Dq1", dq_items(1))] + gates(3)
            return sp

        ctr = {"pp": 0, "ps": 0, "pt": 0, "st": 0, "po": 0, "sgq": 0, "po8": 0}

        def nxt(k, n):
            v = ctr[k] % n
            ctr[k] += 1
            return v

        def pipeline(items, LA):
            n = len(items)
            for i in range(n + LA):
                if i < n:
                    items[i][0]()
                if i >= LA:
                    items[i - LA][1]()

        xTreg = reg("xT")

        def proj_fm(s, c0, ncol, T):
            b = nxt("pp", 2)
            calls = [mm(P[0:ncol, b, :], wv(s)[:, k, c0:c0 + ncol], xT[:, k, T * 512:(T + 1) * 512], k == 0, k == 15)
                     for k in range(16)]
            MM(calls, rd=[wreg[s], xTreg], wr=[pb[b]])
            return b

        def proj_tm(s, j, ncol, b=None):
            if b is None:
                b = nxt("pp", 2)
            calls = [mm(P[:, b, 0:ncol], xT[:, k, j * 128:(j + 1) * 128], wv(s)[:, k, 0:ncol], k == 0, k == 15)
                     for k in range(16)]
            MM(calls, rd=[wreg[s], xTreg], wr=[pb[b]])
            return b

        rtreg = reg("rt")

        def rope(src_bank, np_, dst, dstregs, half, T, n=512):
            ct, stb = (cos128, ssin128) if half == 64 else (cos64, ssin64)
            src = P[:, src_bank, 0:n]
            tsl = slice(T * 512, T * 512 + n)
            OP("vector", lambda e: e.tensor_tensor(out=rt1[0:np_, 0:n], in0=src[0:np_, :], in1=ct[0:np_, tsl], op=ALU.mult),
               rd=[pb[src_bank]], wr=[rtreg])
            for base in range(0, np_, 2 * half):
                lo, mid, hi = base, base + half, base + 2 * half
                OP("vector", lambda e, lo=lo, mid=mid, hi=hi: e.tensor_tensor(
                    out=rt2[lo:mid, 0:n], in0=src[mid:hi, :], in1=stb[lo:mid, tsl], op=ALU.mult), rd=[pb[src_bank]], wr=[rtreg])
                OP("vector", lambda e, lo=lo, mid=mid, hi=hi: e.tensor_tensor(
                    out=rt2[mid:hi, 0:n], in0=src[lo:mid, :], in1=stb[mid:hi, tsl], op=ALU.mult), rd=[pb[src_bank]], wr=[rtreg])
            OP("vector", lambda e: e.tensor_tensor(out=dst, in0=rt1[0:np_, 0:n], in1=rt2[0:np_, 0:n], op=ALU.add),
               rd=[rtreg], wr=dstregs)

        def transposes_to(dst_fn, src_fn, nchunk, srcregs, dstregs, scale_fn=None, np_out=128):
            c = 0
            while c < nchunk:
                g = min(8, nchunk - c)
                b = nxt("pp", 2)
                pbv = P[:, b, :].bitcast(BF16)
                calls = []
                for i in range(g):
                    calls.append((lambda e, o=pbv[:, i * 128:(i + 1) * 128], s_=src_fn(c + i): e.transpose(o, s_, ident[:])))
                MM(calls, rd=srcregs + [reg("const")], wr=[pb[b]])
                for i in range(g):
                    d_ = dst_fn(c + i)
                    i_ = pbv[0:np_out, i * 128:(i + 1) * 128]
                    if scale_fn is None:
                        OP("scalar", lambda e, d_=d_, i_=i_: e.activation(out=d_, in_=i_, func=AF.Copy), rd=[pb[b]], wr=dstregs)
                    else:
                        OP("scalar", lambda e, d_=d_, i_=i_, sc=scale_fn(c + i): e.activation(out=d_, in_=i_, func=AF.Copy, scale=sc),
                           rd=[pb[b]], wr=dstregs)
                c += g

        def block_to_xT(src_f32, srcreg, j):
            if src_f32 is not None:
                OP("scalar", lambda e: e.activation(out=xb, in_=src_f32, func=AF.Copy), rd=[srcreg], wr=streg)
            for g in range(2):
                bk = nxt("pp", 2)
                pbv = P[:, bk, :].bitcast(BF16)
                MM([(lambda e, o=pbv[:, i * 128:(i + 1) * 128], s_=xb[:, (g * 8 + i) * 128:(g * 8 + i + 1) * 128]: e.transpose(o, s_, ident[:]))
                    for i in range(8)], rd=list(streg) + [reg("const")], wr=[pb[bk]])
                dst = xT[:, g * 8:(g + 1) * 8, j * 128:(j + 1) * 128]
                src = pbv[:, :].rearrange("p (c i) -> p c i", c=8)
                if g == 0:
                    OP("scalar", lambda e, dst=dst, src=src: e.activation(out=dst, in_=src, func=AF.Copy), rd=[pb[bk]], wr=[xTreg])
                else:
                    OP("vector", lambda e, dst=dst, src=src: e.tensor_copy(out=dst, in_=src), rd=[pb[bk]], wr=[xTreg])

        def stage_out(src_ap_fn, srcregs, dst_dram, eng="scalar"):
            s = nxt("st", 4)
            src, shape_p, shape_n = src_ap_fn
            OP(eng, lambda e: e.activation(out=stage[0:shape_p, s, 0:shape_n], in_=src, func=AF.Copy) if eng == "scalar"
               else e.tensor_copy(out=stage[0:shape_p, s, 0:shape_n], in_=src), rd=srcregs, wr=[streg[s]])
            DMA("sync", f"st{s}", [(dst_dram, stage[0:shape_p, s, 0:shape_n])], rd=[streg[s]])

        def rmsnorm_rows(bank, ncol, dst_bf, dstregs, eps=1e-6):
            r = reg("sm")
            OP("vector", lambda e: e.memset(sm[:, 0:1], 0.0), wr=[r])
            OP("scalar", lambda e: e.activation(out=rt1[:, 0:ncol], in_=P[:, bank, 0:ncol], func=AF.Square, accum_out=sm[:, 0:1]),
               rd=[pb[bank]], wr=[r, rtreg])
            OP("vector", lambda e: e.tensor_scalar(out=sm[:, 1:2], in0=sm[:, 0:1], scalar1=1.0 / ncol, scalar2=eps, op0=ALU.mult, op1=ALU.add),
               rd=[r], wr=[r])
            OP("scalar", lambda e: e.activation(out=sm[:, 2:3], in_=sm[:, 1:2], func=AF.Ln), rd=[r], wr=[r])
            OP("scalar", lambda e: e.activation(out=sm[:, 3:4], in_=sm[:, 2:3], func=AF.Exp, scale=-0.5), rd=[r], wr=[r])
            OP("scalar", lambda e: e.activation(out=dst_bf, in_=P[:, bank, 0:ncol], func=AF.Copy, scale=sm[:, 3:4]),
               rd=[r, pb[bank]], wr=dstregs)

        xrreg = wreg[2]
        for j in range(NB):
            DMA("sync", "xr", [(xr, x_in[j * 128:(j + 1) * 128, :])], wr=[xrreg])
            block_to_xT(xr, xrreg, j)
        S.barrier()

        for l in range(depth):
            WS = WQ(layer_specs(l))
            ib, ob = ibs[l], obs[l]
            ccoll = [None] * 4
            areg = {n: reg(f"a_{n}") for n in ("kT", "V", "qT", "qrT", "krT", "cqnT", "kmean", "ckvnT")}

            def collective(i, deps):
                S.wait("gpsimd", deps)
                key = f"cc{i}"
                S.sem(key)
                tok = S.op("gpsimd", lambda e, ii=ib[i], oo=ob[i]: e.collective_compute(
                    "AllGather", ALU.bypass, replica_groups=PAIRS, ins=[ii.ap().opt()], outs=[oo.ap().opt()]), key=key)
                ccoll[i] = tok

            def kside_AC(nm, ci):
                toks = []
                for g in range(2):
                    s = WS.next(f"{nm}k{g}")
                    for hh in range(2):
                        h = 2 * g + hh
                        for T in range(2):
                            b = proj_fm(s, hh * 128, 128, T)
                            st = nxt("st", 4)
                            rope(b, 128, stage[:, st, :], [streg[st]], 64, T)
                            toks.append(DMA("sync", f"st{st}", [(ib[ci][h * 128:(h + 1) * 128, T * 512:(T + 1) * 512], stage[:, st, :])],
                                            rd=[streg[st]]))
                vview = ib[ci][512:1024, :].rearrange("r (two c) -> (r two) c", two=2)
                for g in range(2):
                    s = WS.next(f"{nm}v{g}")
                    for j in range(NB):
                        b = proj_tm(s, j, 256)
                        st = nxt("st", 4)
                        OP("scalar", lambda e, b=b, st=st: e.activation(out=stage[:, st, 0:256], in_=P[:, b, 0:256], func=AF.Copy),
                           rd=[pb[b]], wr=[streg[st]])
                        toks.append(DMA("sync", f"st{st}", [(vview[j * 128:(j + 1) * 128, g * 256:(g + 1) * 256], stage[:, st, 0:256])],
                                        rd=[streg[st]]))
                collective(ci, toks)

            kside_AC("A", 0)
            kside_AC("C", 1)

            toksB, toksD = [], []
            s = WS.next("Bkv")
            its = []
            for j in range(NB):
                def s1(j=j, s=s):
                    b = proj_tm(s, j, 256, b=2 + j % 2)
                    rmsnorm_rows(b, 256, cqb[:, j % 2, 0:256], [reg(f"cqb{j % 2}")])

                def s2(j=j):
                    transposes_to(lambda c: ckvnT[:, c, j * 128:(j + 1) * 128], lambda c: cqb[:, j % 2, c * 128:(c + 1) * 128], 2,
                                  [reg(f"cqb{j % 2}")], [areg["ckvnT"]], scale_fn=lambda c: cols[:, 21 * l + 3 + c:21 * l + 4 + c])
                its.append((s1, s2))
            pipeline(its, 1)
            s = WS.next("Bkr")
            for T in range(2):
                b = proj_fm(s, 0, 64, T)
                st = nxt("st", 4)
                rope(b, 64, stage[0:64, st, :], [streg[st]], 32, T)
                toksD.append(DMA("sync", f"st{st}", [(ib[3][0:64, T * 512:(T + 1) * 512], stage[0:64, st, :])], rd=[streg[st]]))
            s = WS.next("WUKV")
            wkv = wbuf[:, s, 0:2048].rearrange("p (k h c) -> p k h c", k=2, h=4)
            for h in range(4):
                for T in range(2):
                    b = nxt("pp", 2)
                    MM([mm(P[:, b, :], wkv[:, k, h, 0:128], ckvnT[:, k, T * 512:(T + 1) * 512], k == 0, k == 1) for k in range(2)],
                       rd=[wreg[s], areg["ckvnT"]], wr=[pb[b]])
                    st = nxt("st", 4)
                    OP("scalar", lambda e, b=b, st=st: e.activation(out=stage[:, st, :], in_=P[:, b, :], func=AF.Copy),
                       rd=[pb[b]], wr=[streg[st]])
                    toksB.append(DMA("sync", f"st{st}", [(ib[2][h * 128:(h + 1) * 128, T * 512:(T + 1) * 512], stage[:, st, :])],
                                     rd=[streg[st]]))
            vviewB = ib[2][512:1024, :].rearrange("r (two c) -> (r two) c", two=2)
            for j in range(NB):
                b = nxt("pp", 2)
                MM([mm(P[:, b, :].rearrange("p (h c) -> p h c", h=4), ckvnT[:, k, j * 128:(j + 1) * 128], wkv[:, k, :, 128:256], k == 0, k == 1)
                    for k in range(2)], rd=[wreg[s], areg["ckvnT"]], wr=[pb[b]])
                st = nxt("st", 4)
                OP("scalar", lambda e, b=b, st=st: e.activation(out=stage[:, st, :], in_=P[:, b, :], func=AF.Copy),
                   rd=[pb[b]], wr=[streg[st]])
                toksB.append(DMA("sync", f"st{st}", [(vviewB[j * 128:(j + 1) * 128, :], stage[:, st, :])], rd=[streg[st]]))
            collective(2, toksB)

            s = WS.next("Dkv")
            for T in range(2):
                b = proj_fm(s, 0, 128, T)
                st = nxt("st", 4)
                rope(b, 128, stage[:, st, :], [streg[st]], 32, T)
                toksD.append(DMA("sync", f"st{st}", [(ib[3][64:192, T * 512:(T + 1) * 512], stage[:, st, :])], rd=[streg[st]]))
            vviewD = ib[3][192:320, :].rearrange("r (e c) -> (r e) c", e=8)
            for j in range(NB):
                b = nxt("pp", 2)
                MM([mm(P[:, b, 0:128], xT[:, k, j * 128:(j + 1) * 128], wv(s)[:, k, 128:256], k == 0, k == 15) for k in range(16)],
                   rd=[wreg[s], xTreg], wr=[pb[b]])
                st = nxt("st", 4)
                OP("scalar", lambda e, b=b, st=st: e.activation(out=stage[:, st, 0:128], in_=P[:, b, 0:128], func=AF.Copy),
                   rd=[pb[b]], wr=[streg[st]])
                toksD.append(DMA("sync", f"st{st}", [(vviewD[j * 128:(j + 1) * 128, :], stage[:, st, 0:128])], rd=[streg[st]]))
            collective(3, toksD)

            def load_ctx_AC(ci, wait_ones=True):
                items = []
                for rr in range(2):
                    items.append((kT[:, :, rr * 1024:(rr + 1) * 1024],
                                  ob[ci][rr * 1024:rr * 1024 + 512, :].rearrange("(h d) t -> d h t", d=128)))
                DMA("sync", "ctxk", items, wr=[areg["kT"]], deps=[ccoll[ci]])
                items = []
                for rr in range(2):
                    src = ob[ci][rr * 1024 + 512:rr * 1024 + 1024, :].rearrange("r (two c) -> (r two) c", two=2)
                    for h in range(4):
                        items.append((Vaug[:, rr * 8:(rr + 1) * 8, h, 0:128],
                                      src[:, h * 128:(h + 1) * 128].rearrange("(jj p) c -> p jj c", p=128)))
                DMA("sync", "ctxv", items, wr=[areg["V"]], deps=[ccoll[ci]])
                OP("vector", lambda e: e.memset(Vaug[:, :, :, 128:129], 1.0), wr=[areg["V"]])

            def qproj_AC(nm):
                for g in range(2):
                    s = WS.next(f"{nm}q{g}")
                    for hh in range(2):
                        h = 2 * g + hh
                        for T in range(2):
                            b = proj_fm(s, hh * 128, 128, T)
                            rope(b, 128, qT[:, h, T * 512:(T + 1) * 512], [areg["qT"]], 64, T)

            smr = {k: reg("sm_" + k) for k in ("norm", "epi", "sel0", "sel1", "D", "km", "ssq")}
            ssq = sm[:, 208:240].rearrange("p (b h) -> p b h", b=4)
            yreg = reg("ytile")
            SB = (2, 3, 0, 1)

            def sumsq(b, col, lo, hi):
                OP("vector", lambda e: e.scalar_tensor_tensor(out=rt1[:, 0:hi - lo], in0=ytile[:, b, lo:hi], scalar=1.0, in1=ytile[:, b, lo:hi],
                                                              op0=ALU.mult, op1=ALU.mult, accum_out=ssq[:, b, col:col + 1]),
                   rd=[yreg], wr=[rtreg, smr["ssq"]])

            def normalize_po(slots, h, src4=None, srcregs=None):
                r = smr["norm"]
                if src4 is None:
                    den = po8[:, slots[0]:slots[0] + 4, 128:129]
                    rg = [por[s_] for s_ in slots]
                else:
                    den = src4[:, :, 128:129]
                    rg = srcregs
                OP("vector", lambda e: e.reciprocal(out=sm[:, 8:12].rearrange("p (b o) -> p b o", o=1), in_=den), rd=rg, wr=[r])
                for b in range(4):
                    src = po8[:, slots[b], :] if src4 is None else src4[:, b, :]
                    OP("vector", lambda e, src=src, b=b: e.tensor_scalar(out=ytile[:, b, h * 128:(h + 1) * 128], in0=src[:, 0:128],
                                                                       scalar1=sm[:, 8 + b:9 + b], scalar2=None, op0=ALU.mult),
                       rd=rg + [r], wr=[yreg])
                    sumsq(b, h, h * 128, (h + 1) * 128)

            def scores(T, h, G, isB, scale):
                kp = kap(G)
                sbk = SB[nxt("ps", 4)]
                calls = [mm(P[:, sbk, :], kT[:, h, kp * 128:(kp + 1) * 128], qT[:, h, T * 512:(T + 1) * 512], True, not isB)]
                rd = [areg["kT"], areg["qT"]]
                if isB:
                    calls.append(mm(P[:, sbk, :], krT[:, kp * 128:(kp + 1) * 128], qrT[:, h, T * 512:(T + 1) * 512], False, True))
                    rd += [areg["krT"], areg["qrT"]]
                MM(calls, rd=rd, wr=[pb[sbk]])
                ps_ = nxt("pt", 4)
                OP("scalar", lambda e: e.activation(out=pt[:, ps_, :], in_=P[:, sbk, :], func=AF.Exp, scale=scale),
                   rd=[pb[sbk]], wr=[ptreg[ps_]])
                return ps_, kp

            def mask128(ps_, b, slot):
                OP("vector", lambda e: e.tensor_tensor(out=pt[:, ps_, b * 128:(b + 1) * 128], in0=pt[:, ps_, b * 128:(b + 1) * 128],
                                                        in1=masks[:, slot, :], op=ALU.mult), rd=[ptreg[ps_], creg], wr=[ptreg[ps_]])

            def maskA(ps_, T, G):
                j0 = 4 * T
                t0 = 2 * j0 + 1 - G
                bmin = 0
                while t0 + 2 * bmin < 0:
                    bmin += 1
                slot = lambda b: TA0 + (t0 + 2 * b) * 2 + (b % 2)
                if bmin == 0:
                    mk = bass.AP(masks, slot(0) * 128, [[44 * 128, 128], [8 * 128, 2], [5 * 128, 2], [1, 128]])
                    ptv = pt[:, ps_, :].rearrange("p (u v i) -> p u v i", u=2, v=2)
                    OP("vector", lambda e: e.tensor_tensor(out=ptv, in0=ptv, in1=mk, op=ALU.mult), rd=[ptreg[ps_], creg], wr=[ptreg[ps_]])
                    return
                if bmin == 1:
                    mask128(ps_, 1, slot(1))
                if bmin <= 2:
                    mk = bass.AP(masks, slot(2) * 128, [[44 * 128, 128], [5 * 128, 2], [1, 128]])
                    ptv = pt[:, ps_, 256:512].rearrange("p (v i) -> p v i", v=2)
                    OP("vector", lambda e: e.tensor_tensor(out=ptv, in0=ptv, in1=mk, op=ALU.mult), rd=[ptreg[ps_], creg], wr=[ptreg[ps_]])
                else:
                    mask128(ps_, 3, slot(3))

            def epilogue(m, T):
                r = smr["epi"]
                if debug:
                    for b in range(4):
                        DMA("sync", "dbg", [(ydbg[(4 * T + b) * 128:(4 * T + b + 1) * 128, m * 512:(m + 1) * 512], ytile[:, b, :])], rd=[yreg])
                nh = 8 if m == 3 else 4
                OP("vector", lambda e: e.tensor_reduce(out=sm[:, 16:20], in_=ssq[:, :, 0:nh], op=ALU.add, axis=AX.X), rd=[smr["ssq"]], wr=[r])
                OP("vector", lambda e: e.tensor_scalar(out=sm[:, 20:24], in0=sm[:, 16:20], scalar1=1.0 / 512, scalar2=1e-6, op0=ALU.mult, op1=ALU.add),
                   rd=[r], wr=[r])
                OP("scalar", lambda e: e.activation(out=sm[:, 24:28], in_=sm[:, 20:24], func=AF.Ln), rd=[r], wr=[r])
                OP("scalar", lambda e: e.activation(out=sm[:, 28:32], in_=sm[:, 24:28], func=AF.Exp, scale=-0.5), rd=[r], wr=[r])
                its = []
                slots_g = [WS.next(f"G{m}{g}", la=3 - g) for g in range(2)]
                for g in range(2):
                    s = slots_g[g]
                    for b in range(4):
                        q = nxt("sgq", 2)
                        j = 4 * T + b

                        def sA(s=s, b=b, g=g, q=q, j=j):
                            bk = proj_tm(s, j, 256)
                            OP("scalar", lambda e: e.activation(out=sg[:, q, :], in_=P[:, bk, 0:256], func=AF.Silu),
                               rd=[pb[bk]], wr=[reg(f"sg{q}")])
                            OP("vector", lambda e: e.scalar_tensor_tensor(
                                out=ygb[:, q, :], in0=ytile[:, b, g * 256:(g + 1) * 256], scalar=sm[:, 28 + b:29 + b], in1=sg[:, q, :],
                                op0=ALU.mult, op1=ALU.mult), rd=[yreg, r, reg(f"sg{q}")], wr=[reg(f"yg{q}")])

                        def sB(g=g, q=q, j=j):
                            c0 = m * 4 + g * 2
                            transposes_to(lambda c: yT[:, c0 + c, j * 128:(j + 1) * 128],
                                          lambda c: ygb[:, q, c * 128:(c + 1) * 128], 2, [reg(f"yg{q}")], [reg("yT")],
                                          scale_fn=lambda c: cols[:, 21 * l + 5 + c0 + c:21 * l + 6 + c0 + c])
                        its.append((sA, sB))
                pipeline(its, 1)

            def dense_stream(m, isA, isB, scale):
                items = []
                for T in range(2):
                    j0 = 4 * T
                    nG = 8 * T + 8
                    for h in range(4):
                        st = {}
                        for G in range(nG):
                            d = {}

                            def s1(T=T, h=h, G=G, d=d, st=st, j0=j0):
                                if G == 0:
                                    ob_ = nxt("po", 2)
                                    st["slots"] = [ob_ * 4 + b for b in range(4)]
                                d["ps"], d["kp"] = scores(T, h, G, isB, scale)
                                if isA:
                                    maskA(d["ps"], T, G)
                                else:
                                    for b in range(4):
                                        j = j0 + b
                                        if 2 * j <= G <= 2 * j + 1:
                                            mask128(d["ps"], b, MB0 + (j % 2) * 2 + (G - 2 * j))

                            def s2(T=T, h=h, G=G, d=d, st=st, j0=j0, nG=nG):
                                slots = st["slots"]
                                calls, wr = [], []
                                for b in range(4):
                                    j = j0 + b
                                    if G <= 2 * j + 1:
                                        calls.append(lambda e, b=b, j=j: e.matmul(
                                            po8[:, slots[b], 0:129], lhsT=pt[:, d["ps"], b * 128:(b + 1) * 128], rhs=Vaug[:, d["kp"], h, :],
                                            start=(G == 0 and b % 2 == 0), stop=(G == 2 * j + 1), skip_group_check=True))
                                        wr.append(por[slots[b]])
                                MM(calls, rd=[ptreg[d["ps"]], areg["V"]], wr=wr)
                                if G == nG - 1:
                                    normalize_po(slots, h)
                                    if h == 3:
                                        epilogue(m, T)
                            items.append((s1, s2))
                pipeline(items, 2)

            def zero_ssq():
                OP("vector", lambda e: e.memset(sm[:, 208:240], 0.0), wr=[smr["ssq"]])

            load_ctx_AC(0)
            qproj_AC("A")
            zero_ssq()
            dense_stream(0, True, False, 128 ** -0.5)

            load_ctx_AC(1)
            zero_ssq()
            r = smr["km"]
            for h in range(4):
                OP("vector", lambda e, h=h: e.tensor_reduce(out=sm[:, 32:48], in_=kT[:, h, :].rearrange("p (k t) -> p k t", k=16),
                                                            op=ALU.add, axis=AX.X), rd=[areg["kT"]], wr=[r])
                OP("vector", lambda e: e.tensor_tensor(out=sm[:, 48:56], in0=sm[:, 32:40], in1=sm[:, 40:48], op=ALU.add), rd=[r], wr=[r])
                OP("vector", lambda e, h=h: e.tensor_scalar(out=kmean[:, h, :], in0=sm[:, 48:56], scalar1=1.0 / 256, scalar2=None, op0=ALU.mult),
                   rd=[r], wr=[areg["kmean"]])
            qproj_AC("C")
            selbuf = [sm[:, 64:96].rearrange("p (b n) -> p b n", b=4), sm[:, 144:176].rearrange("p (b n) -> p b n", b=4)]
            gmv = sm[:, 96:128].rearrange("p (b n) -> p b n", b=4)
            top = sm[:, 176:208].rearrange("p (b n) -> p b n", b=4)
            gmr = reg("sm_gm")
            accr = reg("accC")
            items = []
            grp = 0
            for T in range(2):
                j0 = 4 * T
                for h in range(4):
                    st = {"first": [True] * 4}
                    sel = selbuf[grp % 2]
                    selr = smr[f"sel{grp % 2}"]
                    grp += 1
                    nN = j0 + 4
                    for n in range(nN):
                        d = {}

                        def s1(T=T, h=h, n=n, d=d, j0=j0, sel=sel, selr=selr):
                            if n == 0:
                                bk = nxt("pp", 2)
                                MM([mm(P[:, bk, b * 8:(b + 1) * 8], qT[:, h, (j0 + b) * 128:(j0 + b + 1) * 128], kmean[:, h, :], True, True)
                                    for b in range(4)], rd=[areg["qT"], areg["kmean"]], wr=[pb[bk]])
                                OP("vector", lambda e: e.tensor_tensor(out=gmv, in0=P[:, bk, 0:32].rearrange("p (b n) -> p b n", b=4),
                                                                       in1=negm[:, j0:j0 + 4, :], op=ALU.add), rd=[pb[bk], creg], wr=[gmr])
                                for b in range(4):
                                    OP("vector", lambda e, b=b: e.max(out=top[:, b, :], in_=gmv[:, b, :]), rd=[gmr], wr=[gmr])
                                for b in range(4):
                                    OP("vector", lambda e, b=b: e.tensor_scalar(out=sel[:, b, :], in0=gmv[:, b, :], scalar1=top[:, b, 2:3], scalar2=None,
                                                                                op0=ALU.is_ge), rd=[gmr], wr=[selr])
                            d["pss"] = []
                            for w in range(2):
                                ps_, kp = scores(T, h, 2 * n + w, False, 128 ** -0.5)
                                for b in range(4):
                                    if j0 + b == n:
                                        mask128(ps_, b, MB0 + (n % 2) * 2 + w)
                                d["pss"].append((ps_, kp))

                        def s2(T=T, h=h, n=n, d=d, j0=j0, sel=sel, selr=selr, st=st, nN=nN):
                            ob_ = nxt("po", 2)
                            slots = [ob_ * 4 + b for b in range(4)]
                            pss = d["pss"]
                            calls, wr = [], []
                            for b in range(4):
                                if j0 + b >= n:
                                    for w in range(2):
                                        ps_, kp = pss[w]
                                        calls.append(mm(po8[:, slots[b], 0:129], pt[:, ps_, b * 128:(b + 1) * 128], Vaug[:, kp, h, :], w == 0, w == 1))
                                    wr.append(por[slots[b]])
                            MM(calls, rd=[ptreg[pss[0][0]], ptreg[pss[1][0]], areg["V"]], wr=wr)
                            first = st["first"]
                            for b in range(4):
                                j = j0 + b
                                if j < n:
                                    continue
                                src = po8[:, slots[b], 0:129]
                                dst = accC[:, b, 0:129]
                                if j == n:
                                    if first[b]:
                                        OP("vector", lambda e, src=src, dst=dst: e.tensor_copy(out=dst, in_=src), rd=[por[slots[b]]], wr=[accr])
                                    else:
                                        OP("vector", lambda e, src=src, dst=dst: e.tensor_tensor(out=dst, in0=src, in1=dst, op=ALU.add),
                                           rd=[por[slots[b]]], wr=[accr])
                                else:
                                    if first[b]:
                                        OP("vector", lambda e, src=src, dst=dst, b=b: e.tensor_scalar(
                                            out=dst, in0=src, scalar1=sel[:, b, n:n + 1], scalar2=None, op0=ALU.mult),
                                           rd=[por[slots[b]], selr], wr=[accr])
                                    else:
                                        OP("vector", lambda e, src=src, dst=dst, b=b: e.scalar_tensor_tensor(
                                            out=dst, in0=src, scalar=sel[:, b, n:n + 1], in1=dst, op0=ALU.mult, op1=ALU.add),
                                           rd=[por[slots[b]], selr], wr=[accr])
                                first[b] = False
                            if n == nN - 1:
                                normalize_po(None, h, src4=accC, srcregs=[accr])
                                if h == 3:
                                    epilogue(2, T)
                        items.append((s1, s2))
            pipeline(items, 1)

            load_ctx_AC(2)
            DMA("sync", "ctxr", [(krT[0:64, rr * 1024:(rr + 1) * 1024], ob[3][rr * 320:rr * 320 + 64, :]) for rr in range(2)],
                wr=[areg["krT"]], deps=[ccoll[3]])
            OP("gpsimd", lambda e: e.memset(krT[64:128, :], 0.0), wr=[areg["krT"]])
            OP("gpsimd", lambda e: e.memset(qrT[64:128, :, :], 0.0), wr=[areg["qrT"]])
            s0 = WS.next("Bcq0")
            s1_ = WS.next("Bcq1", la=2)
            its = []
            for j in range(NB):
                def s1(j=j):
                    bk = 2 + j % 2
                    calls = [mm(P[:, bk, 0:256], xT[:, k, j * 128:(j + 1) * 128], wv(s0)[:, k, 0:256], k == 0, k == 15) for k in range(16)]
                    calls += [mm(P[:, bk, 256:384], xT[:, k, j * 128:(j + 1) * 128], wv(s1_)[:, k, 0:128], k == 0, k == 15) for k in range(16)]
                    MM(calls, rd=[wreg[s0], wreg[s1_], xTreg], wr=[pb[bk]])
                    rmsnorm_rows(bk, 384, cqb[:, j % 2, 0:384], [reg(f"cqb{j % 2}")])

                def s2(j=j):
                    transposes_to(lambda c: cqnT[:, c, j * 128:(j + 1) * 128], lambda c: cqb[:, j % 2, c * 128:(c + 1) * 128], 3,
                                  [reg(f"cqb{j % 2}")], [areg["cqnT"]], scale_fn=lambda c: cols[:, 21 * l + c:21 * l + 1 + c])
                its.append((s1, s2))
            pipeline(its, 1)
            s = WS.next("WUQ")
            wq = wbuf[:, s, 0:2304].rearrange("p (k h c) -> p k h c", k=3, h=4)
            for h in range(4):
                for T in range(2):
                    bk = nxt("pp", 2)
                    MM([mm(P[:, bk, :], wq[:, k, h, 0:128], cqnT[:, k, T * 512:(T + 1) * 512], k == 0, k == 2) for k in range(3)],
                       rd=[wreg[s], areg["cqnT"]], wr=[pb[bk]])
                    OP("scalar", lambda e, bk=bk, h=h, T=T: e.activation(out=qT[:, h, T * 512:(T + 1) * 512], in_=P[:, bk, :], func=AF.Copy),
                       rd=[pb[bk]], wr=[areg["qT"]])
                    bk = nxt("pp", 2)
                    MM([mm(P[0:64, bk, :], wq[:, k, h, 128:192], cqnT[:, k, T * 512:(T + 1) * 512], k == 0, k == 2) for k in range(3)],
                       rd=[wreg[s], areg["cqnT"]], wr=[pb[bk]])
                    rope(bk, 64, qrT[0:64, h, T * 512:(T + 1) * 512], [areg["qrT"]], 32, T)
            zero_ssq()
            dense_stream(1, False, True, 192 ** -0.5)

            tokB = []
            for rg in areg.values():
                tokB.extend(rg.wdeps())
            dreg = {n: Reg() for n in ("kT", "V", "qT")}
            for rg in dreg.values():
                for t in tokB:
                    rg.read(t)
            kTD = av(24512, 2048)
            VD = av(26560, 2080).rearrange("p (k h c) -> p k h c", k=16, h=2)
            qTD = av(28640, 4096).rearrange("p (h t) -> p h t", h=4)
            items = [(kTD[:, rr * 1024:(rr + 1) * 1024], ob[3][rr * 320 + 64:rr * 320 + 192, :]) for rr in range(2)]
            DMA("sync", "ctxk", items, wr=[dreg["kT"]], deps=[ccoll[3]])
            items = []
            for rr in range(2):
                src = ob[3][rr * 320 + 192:rr * 320 + 320, :].rearrange("r (e c) -> (r e) c", e=8)
                for hh in range(2):
                    items.append((VD[:, rr * 8:(rr + 1) * 8, hh, 0:64], src[:, hh * 64:(hh + 1) * 64].rearrange("(jj p) c -> p jj c", p=128)))
            DMA("sync", "ctxv", items, wr=[dreg["V"]], deps=[ccoll[3]])
            OP("vector", lambda e: e.memset(VD[:, :, :, 64:65], 1.0), wr=[dreg["V"]])
            for g in range(2):
                s = WS.next(f"Dq{g}")
                for cc in range(2):
                    c = 2 * g + cc
                    for T in range(2):
                        bk = proj_fm(s, cc * 128, 128, T)
                        rope(bk, 128, qTD[:, c, T * 512:(T + 1) * 512], [dreg["qT"]], 32, T)
            WS.prefetch()
            DMA("gpsimd", "wo", [(wo[:, 0:11, q * 512:(q + 1) * 512],
                                  w_out[l, 0:11 * 128, q * 512:(q + 1) * 512].rearrange("(k p) c -> p k c", p=128)) for q in range(4)],
                wr=[reg("wo_lo")], deps=tokB)
            zero_ssq()
            r = smr["D"]
            items = []
            for T in range(2):
                for b in range(4):
                    for c in range(4):
                        for sl in range(2):
                            d = {}
                            j = 4 * T + b
                            Gs = [G for G in (2 * j - 1, 2 * j, 2 * j + 1) if G >= 0]

                            def s1(j=j, c=c, sl=sl, d=d, Gs=Gs):
                                w0 = 3 - len(Gs)
                                prt = slice(64 * sl, 64 * sl + 64)
                                sbk = SB[nxt("ps", 4)]
                                MM([mm(P[:, sbk, (w0 + i) * 128:(w0 + i + 1) * 128], kTD[prt, kap(G) * 128:(kap(G) + 1) * 128],
                                       qTD[prt, c, j * 128:(j + 1) * 128], True, True) for i, G in enumerate(Gs)],
                                   rd=[dreg["kT"], dreg["qT"]], wr=[pb[sbk]])
                                ps_ = nxt("pt", 4)
                                d["ps"] = ps_
                                OP("scalar", lambda e: e.activation(out=pt[:, ps_, w0 * 128:384], in_=P[:, sbk, w0 * 128:384],
                                                                    func=AF.Exp, scale=0.125), rd=[pb[sbk]], wr=[ptreg[ps_]])
                                OP("vector", lambda e: e.tensor_tensor(
                                    out=pt[:, ps_, w0 * 128:384], in0=pt[:, ps_, w0 * 128:384],
                                    in1=masks[:, MD0 + (j % 2) * 3 + w0:MD0 + (j % 2) * 3 + 3, :].rearrange("p a b -> p (a b)"), op=ALU.mult),
                                   rd=[ptreg[ps_], creg], wr=[ptreg[ps_]])

                            def s2(T=T, b=b, c=c, sl=sl, d=d, Gs=Gs, l=l):
                                w0 = 3 - len(Gs)
                                ps_ = d["ps"]
                                hd = c + 4 * sl
                                slot = (0, 2, 4, 6, 1, 3, 5, 7)[nxt("po8", 8)]
                                MM([mm(po8[:, slot, 0:65], pt[:, ps_, (w0 + i) * 128:(w0 + i + 1) * 128], VD[:, kap(G), sl, :], i == 0, i == len(Gs) - 1)
                                    for i, G in enumerate(Gs)], rd=[ptreg[ps_], dreg["V"]], wr=[por[slot]])
                                u = 240 + (hd % 8)
                                OP("vector", lambda e: e.tensor_tensor(out=sm[:, u:u + 1], in0=po8[:, slot, 64:65],
                                                                       in1=esink[:, 8 * l + hd:8 * l + hd + 1], op=ALU.add),
                                   rd=[por[slot], reg("esink")], wr=[r])
                                OP("vector", lambda e: e.reciprocal(out=sm[:, u + 8:u + 9], in_=sm[:, u:u + 1]), rd=[r], wr=[r])
                                OP("vector", lambda e: e.tensor_scalar(out=ytile[:, b, hd * 64:(hd + 1) * 64], in0=po8[:, slot, 0:64],
                                                                       scalar1=sm[:, u + 8:u + 9], scalar2=None, op0=ALU.mult),
                                   rd=[por[slot], r], wr=[yreg])
                                sumsq(b, hd, hd * 64, (hd + 1) * 64)
                                if b == 3 and c == 3 and sl == 1:
                                    epilogue(3, T)
                            items.append((s1, s2))
            pipeline(items, 2)

            S.barrier()
            DMA("gpsimd", "wo", [(wo[:, 11:16, q * 512:(q + 1) * 512],
                                  w_out[l, 11 * 128:16 * 128, q * 512:(q + 1) * 512].rearrange("(k p) c -> p k c", p=128)) for q in range(4)],
                wr=[reg("wo_hi")])
            DMA("sync", "gb", [(gam, ln_g[l:l + 1, :].partition_broadcast(128)[:, 0, :]),
                               (bet, ln_b[l:l + 1, :].partition_broadcast(128)[:, 0, :])], wr=[wreg[0], wreg[1]])
            xsrc = x_in if l == 0 else xs[(l - 1) % 2]
            xdst = out_d if l == depth - 1 else xs[l % 2]
            zreg = reg("ytile")
            r = reg("sm")
            def outproj(j):
                DMA("sync", "xr", [(xr, xsrc[j * 128:(j + 1) * 128, :])], wr=[wreg[2]])
                for hf in range(2):
                    for q in (2 * hf, 2 * hf + 1):
                        MM([mm(P[:, 4 + q, :], yT[:, k, j * 128:(j + 1) * 128], wo[:, k, q * 512:(q + 1) * 512], k == 0, False) for k in range(11)],
                           rd=[reg("wo_lo"), reg("yT")], wr=[por[2 * q], por[2 * q + 1]])
                    for q in (2 * hf, 2 * hf + 1):
                        MM([mm(P[:, 4 + q, :], yT[:, k, j * 128:(j + 1) * 128], wo[:, k, q * 512:(q + 1) * 512], False, k == 15) for k in range(11, 16)],
                           rd=[reg("wo_hi"), reg("yT")], wr=[por[2 * q], por[2 * q + 1]])

            outproj(0)
            for j in range(NB):
                for q in range(4):
                    OP("vector", lambda e, q=q: e.scalar_tensor_tensor(out=zt[:, q * 512:(q + 1) * 512], in0=xr[:, q * 512:(q + 1) * 512], scalar=ALPHA,
                                                                      in1=P[:, 4 + q, :], op0=ALU.mult, op1=ALU.add),
                       rd=[wreg[2], por[2 * q], por[2 * q + 1]], wr=[zreg])
                    OP("vector", lambda e, q=q: e.bn_stats(out=sm[:, 176 + 6 * q:182 + 6 * q], in_=zt[:, q * 512:(q + 1) * 512]), rd=[zreg], wr=[r])
                OP("vector", lambda e: e.bn_aggr(out=sm[:, 200:202], in_=sm[:, 176:200]), rd=[r], wr=[r])
                OP("vector", lambda e: e.tensor_scalar(out=sm[:, 202:203], in0=sm[:, 201:202], scalar1=1e-5, scalar2=None, op0=ALU.add), rd=[r], wr=[r])
                OP("scalar", lambda e: e.activation(out=sm[:, 203:204], in_=sm[:, 202:203], func=AF.Ln), rd=[r], wr=[r])
                OP("scalar", lambda e: e.activation(out=sm[:, 204:205], in_=sm[:, 203:204], func=AF.Exp, scale=-0.5), rd=[r], wr=[r])
                OP("vector", lambda e: e.scalar_tensor_tensor(out=zt, in0=zt, scalar=sm[:, 200:201], in1=gam, op0=ALU.subtract, op1=ALU.mult),
                   rd=[r, zreg, wreg[0]], wr=[zreg])
                OP("vector", lambda e: e.scalar_tensor_tensor(out=zt, in0=zt, scalar=sm[:, 204:205], in1=bet, op0=ALU.mult, op1=ALU.add),
                   rd=[r, zreg, wreg[1]], wr=[zreg])
                DMA("sync", "xo", [(xdst[j * 128:(j + 1) * 128, :], zt)], rd=[zreg])
                if l < depth - 1:
                    OP("scalar", lambda e: e.activation(out=xb, in_=zt, func=AF.Copy), rd=[zreg], wr=streg)
                if j + 1 < NB:
                    outproj(j + 1)
                if l < depth - 1:
                    block_to_xT(None, None, j)
            S.barrier()

        S.emit()
    return nc


def _mult(delta):
    m = np.zeros_like(delta, dtype=np.float32)
    ok = delta >= 0
    m += (ok & (delta <= 128))
    m += (ok & (delta % 4 == 0) & (delta <= 512))
    m += (ok & (delta % 16 == 0) & (delta <= 2048))
    return m


def host_tables(r):
    i = np.arange(128)[None, :]
    k = np.arange(128)[:, None]
    masks = np.zeros((128, 44, 128), np.float32)
    for jpar in range(2):
        e = (jpar + r) % 2
        for t in range(17):
            masks[:, t * 2 + jpar, :] = _mult((t - 1 + e) * 128 + i - k)
        for w in range(2):
            masks[:, 34 + jpar * 2 + w, :] = (((e - w) * 128 + i - k) >= 0)
        for w in range(3):
            dd = (e + 1 - w) * 128 + i - k
            masks[:, 38 + jpar * 3 + w, :] = ((dd >= 0) & (dd <= 127))
    pos = np.concatenate([gblk(r, j) * 128 + np.arange(128) for j in range(NB)]).astype(np.float32)
    tabs = np.zeros((128, 4, 1024), np.float32)
    p = np.arange(128)
    inv128 = (np.float32(10000.0) ** (-np.arange(0, 128, 2, dtype=np.float32) / np.float32(128))).astype(np.float32)
    inv64 = (np.float32(10000.0) ** (-np.arange(0, 64, 2, dtype=np.float32) / np.float32(64))).astype(np.float32)
    a128 = (pos[None, :] * inv128[p % 64][:, None]).astype(np.float32)
    a64 = (pos[None, :] * inv64[(p % 64) % 32][:, None]).astype(np.float32)
    tabs[:, 0] = np.cos(a128)
    tabs[:, 1] = np.sin(a128) * np.where(p < 64, -1.0, 1.0)[:, None]
    tabs[:, 2] = np.cos(a64)
    tabs[:, 3] = np.sin(a64) * np.where((p % 64) < 32, -1.0, 1.0)[:, None]
    return masks.astype(ml_dtypes.bfloat16), tabs


def host_inputs(x, w_in, q_norm, w_uq, kv_norm, w_ukv, sinks, branch_norm, w_out, ln_gamma, ln_beta):
    f = lambda a: np.ascontiguousarray(np.asarray(a, dtype=np.float32))
    x, w_in, w_uq, w_ukv, w_out = f(x), f(w_in), f(w_uq), f(w_ukv), f(w_out)
    q_norm, kv_norm, sinks, branch_norm, ln_gamma, ln_beta = f(q_norm), f(kv_norm), f(sinks), f(branch_norm), f(ln_gamma), f(ln_beta)
    cols = np.zeros((128, 84), np.float32)
    for l in range(DEPTH):
        cols[:, 21 * l:21 * l + 3] = q_norm[l].reshape(3, 128).T
        cols[:, 21 * l + 3:21 * l + 5] = kv_norm[l].reshape(2, 128).T
        cols[:, 21 * l + 5:21 * l + 21] = branch_norm[l].reshape(16, 128).T
    sinks_bc = np.ascontiguousarray(np.broadcast_to(sinks.reshape(1, 32), (128, 32)))
    negm = np.zeros((128, 8, 8), np.float32)
    for j in range(8):
        negm[:, j, j:] = NEG
    negm = negm.reshape(128, 64)
    ident = np.eye(128, dtype=np.float32).astype(ml_dtypes.bfloat16)
    tb = [host_tables(r) for r in range(2)]
    in_maps = []
    for c in range(8):
        b, r = c // 2, c % 2
        xo = np.concatenate([x[b, gblk(r, j) * 128:(gblk(r, j) + 1) * 128, :] for j in range(NB)], 0)
        in_maps.append({"x": np.ascontiguousarray(xo), "w_in": w_in, "w_uq": w_uq, "w_ukv": w_ukv, "w_out": w_out,
                        "ln_gamma": ln_gamma, "ln_beta": ln_beta, "cols": cols, "sinks_bc": sinks_bc, "negm": negm,
                        "tabs": tb[r][1], "masks": tb[r][0], "ident": ident})
    return in_maps


def assemble(results, key="out"):
    out = np.zeros((4, 2048, D), np.float32)
    for c in range(8):
        b, r = c // 2, c % 2
        o = np.asarray(results[c][key])
        for j in range(NB):
            g = gblk(r, j)
            out[b, g * 128:(g + 1) * 128, :] = o[j * 128:(j + 1) * 128, :]
    return out


_NC = {}


def kernel(x, w_in, q_norm, w_uq, kv_norm, w_ukv, sinks, branch_norm, w_out, ln_gamma, ln_beta):
    in_maps = host_inputs(x, w_in, q_norm, w_uq, kv_norm, w_ukv, sinks, branch_norm, w_out, ln_gamma, ln_beta)
    if "nc" not in _NC:
        _NC["nc"] = build(DEPTH, False)
    res = run_bass_kernel_spmd(_NC["nc"], in_maps, core_ids=list(range(8)))
    return assemble(res.results)
```

```python
import contextlib
import numpy as np
import ml_dtypes
import concourse.bass as bass
import concourse.mybir as mybir
from concourse.bass_utils import run_bass_kernel_spmd

F32 = mybir.dt.float32
BF16 = mybir.dt.bfloat16
AF = mybir.ActivationFunctionType
ALU = mybir.AluOpType
AX = mybir.AxisListType

D = 2048
NIN = 6592
NOWN = 1024
NB = 8
DEPTH = 4
ALPHA = (2 * DEPTH) ** 0.25
O_AQ, O_AK, O_AV = 0, 512, 1024
O_BCQ, O_BCKV, O_BKR = 1536, 1920, 2176
O_CQ, O_CK, O_CV = 2240, 2752, 3264
O_DQ, O_DK, O_DV = 3776, 4288, 4416
O_G = 4544
NEG = -1e30
PAIRS = [[0, 1], [2, 3], [4, 5], [6, 7]]


def gblk(r, j):
    return 2 * j + (j + r) % 2


def kap(G):
    jj = G // 2
    rr = (G % 2 - jj) % 2
    return 8 * rr + jj


class Sched:
    ENGS = ("sync", "scalar", "vector", "gpsimd", "tensor")

    def __init__(self, nc):
        self.nc = nc
        self.ops = {e: [] for e in self.ENGS}
        self.sems = {}
        self.cnt = {}
        self.mult = {}
        self.waited = {e: {} for e in self.ENGS}
        self._cms = []

    def sem(self, key, mult=1):
        if key not in self.sems:
            cm = self.nc.semaphore(key)
            self.sems[key] = cm.__enter__()
            self._cms.append(cm)
            self.cnt[key] = 0
            self.mult[key] = mult
        return self.sems[key]

    def wait(self, eng, toks):
        for t in toks:
            if t is None:
                continue
            key, n = t
            w = self.waited[eng]
            if w.get(key, 0) >= n:
                continue
            w[key] = n
            h = self.sems[key]
            self.ops[eng].append(lambda e, h=h, v=n * self.mult[key]: e.wait_ge(h, v))

    def op(self, eng, fn, deps=(), sig=True, key=None):
        self.wait(eng, deps)
        if sig:
            key = key or ("c_" + eng)
            h = self.sem(key)
            self.cnt[key] += 1
            self.ops[eng].append(lambda e, fn=fn, h=h: fn(e).then_inc(h, 1))
            return (key, self.cnt[key])
        self.ops[eng].append(lambda e, fn=fn: fn(e))
        return None

    def dma(self, eng, chan, items, deps=()):
        key = "d_" + chan
        h = self.sem(key, 16)
        if self.cnt[key]:
            self.wait(eng, [(key, self.cnt[key])])
        self.wait(eng, deps)
        for (o, i, kw) in items:
            self.cnt[key] += 1
            self.ops[eng].append(lambda e, o=o, i=i, kw=kw, h=h: e.dma_start(out=o, in_=i, **kw).then_inc(h, 16))
        return (key, self.cnt[key])

    def barrier(self):
        toks = [(k, c) for k, c in self.cnt.items() if c > 0]
        for e in self.ENGS:
            self.wait(e, toks)

    def emit(self):
        self.barrier()
        with self.nc.Block() as block:
            @block.sync
            def _(e):
                for f in self.ops["sync"]:
                    f(e)

            @block.scalar
            def _(e):
                for f in self.ops["scalar"]:
                    f(e)

            @block.vector
            def _(e):
                for f in self.ops["vector"]:
                    f(e)

            @block.gpsimd
            def _(e):
                for f in self.ops["gpsimd"]:
                    f(e)

            @block.tensor
            def _(e):
                for f in self.ops["tensor"]:
                    f(e)
        for cm in reversed(self._cms):
            cm.__exit__(None, None, None)


class Reg:
    def __init__(self):
        self.w = None
        self.r = {}

    def rdeps(self):
        return [self.w] if self.w else []

    def wdeps(self):
        return ([self.w] if self.w else []) + list(self.r.values())

    def read(self, tok):
        if tok is None:
            return
        k = tok[0]
        if k not in self.r or self.r[k][1] < tok[1]:
            self.r[k] = tok

    def write(self, tok):
        self.w = tok
        self.r = {}


def build(depth=DEPTH, debug=False):
    nc = bass.Bass("TRN2", target_bir_lowering=False)
    S = Sched(nc)
    dt_in = lambda name, shape, dt=F32: nc.dram_tensor(name, shape, dt, kind="ExternalInput")
    x_in = dt_in("x", [NOWN, D])
    w_in = dt_in("w_in", [DEPTH, D, NIN])
    w_uq = dt_in("w_uq", [DEPTH, 384, 768])
    w_ukv = dt_in("w_ukv", [DEPTH, 256, 1024])
    w_out = dt_in("w_out", [DEPTH, D, D])
    ln_g = dt_in("ln_gamma", [DEPTH, D])
    ln_b = dt_in("ln_beta", [DEPTH, D])
    cols_d = dt_in("cols", [128, 84])
    sinks_d = dt_in("sinks_bc", [128, 32])
    negm_d = dt_in("negm", [128, 64])
    tabs_d = dt_in("tabs", [128, 4, 1024])
    masks_d = dt_in("masks", [128, 44, 128], BF16)
    ident_d = dt_in("ident", [128, 128], BF16)
    out_d = nc.dram_tensor("out", [NOWN, D], F32, kind="ExternalOutput")
    if debug:
        ydbg = nc.dram_tensor("ydbg", [NOWN, D], F32, kind="ExternalOutput")
    xs = [nc.dram_tensor("xs0", [NOWN, D], F32), nc.dram_tensor("xs1", [NOWN, D], F32)]
    ibs = [[nc.dram_tensor(f"ib{l}_{i}", [320 if i == 3 else 1024, 1024], BF16) for i in range(4)] for l in range(depth)]
    obs = [[nc.dram_tensor(f"ob{l}_{i}", [640 if i == 3 else 2048, 1024], BF16) for i in range(4)] for l in range(depth)]

    es = contextlib.ExitStack()
    with es:
        sb = lambda name, shape, dt: es.enter_context(nc.sbuf_tensor("sb_" + name, shape, dt))
        xT = sb("xT", [128, 16, NOWN], BF16)
        yT = sb("yT", [128, 16, NOWN], BF16)
        arena = sb("arena", [128, 32768], BF16)
        wbuf = sb("wbuf", [128, 3, 4096], BF16)
        stage = sb("stage", [128, 4, 512], BF16)
        tabs = sb("tabs", [128, 4, 1024], F32)
        masks = sb("masks", [128, 44, 128], BF16)
        ident = sb("ident", [128, 128], BF16)
        pt = sb("pt", [128, 4, 512], BF16)
        ytile = sb("ytile", [128, 4, 512], F32)
        rt1 = sb("rt1", [128, 512], F32)
        rt2 = sb("rt2", [128, 512], F32)
        cols = sb("cols", [128, 84], F32)
        esink = sb("esink", [128, 32], F32)
        negm = sb("negm", [128, 8, 8], F32)
        sm = sb("sm", [128, 256], F32)
        accC = sb("accC", [128, 4, 132], F32)
        sg = sb("sg", [128, 2, 256], F32)
        ygb = sb("ygb", [128, 2, 256], BF16)
        cqb = sb("cqb", [128, 2, 384], BF16)
        P = es.enter_context(nc.psum_tensor("P", [128, 8, 512], F32))

        R = {}
        def reg(name):
            if name not in R:
                R[name] = Reg()
            return R[name]
        pb = [reg(f"pb{i}") for i in range(4)]
        por = [reg(f"po{i}") for i in range(8)]
        wreg = [reg(f"w{i}") for i in range(3)]
        streg = [reg(f"st{i}") for i in range(4)]
        ptreg = [reg(f"pt{i}") for i in range(4)]

        po8 = P[:, 4:8, :].rearrange("p k (b c) -> p (k b) c", c=256)

        def av(off, n):
            return arena[:, off:off + n]
        kT = av(0, 8192).rearrange("p (h t) -> p h t", h=4)
        Vaug = av(8192, 8256).rearrange("p (k h c) -> p k h c", k=16, h=4)
        VD = av(8192, 2080).rearrange("p (k h c) -> p k h c", k=16, h=2)
        qT = av(16448, 4096).rearrange("p (h t) -> p h t", h=4)
        qrT = av(20544, 4096).rearrange("p (h t) -> p h t", h=4)
        krT = av(24640, 2048)
        cqnT = av(26688, 3072).rearrange("p (k t) -> p k t", k=3)
        kmean = av(29760, 32).rearrange("p (h n) -> p h n", h=4)
        ckvnT = av(29792, 2048).rearrange("p (k t) -> p k t", k=2)
        wo = arena[:, :].rearrange("p (k c) -> p k c", k=16)
        gam = wbuf[:, 0, :].bitcast(F32)
        bet = wbuf[:, 1, :].bitcast(F32)
        xr = wbuf[:, 2, :].bitcast(F32)
        zt = ytile[:, :, :].rearrange("p a b -> p (a b)")
        xb = stage[:, :, :].rearrange("p a b -> p (a b)")

        def OP(eng, fn, rd=(), wr=(), deps=()):
            d = list(deps)
            for t in rd:
                d.extend(t.rdeps())
            for t in wr:
                d.extend(t.wdeps())
            tok = S.op(eng, fn, d)
            for t in rd:
                t.read(tok)
            for t in wr:
                t.write(tok)
            return tok

        def DMA(eng, chan, items, rd=(), wr=(), deps=()):
            d = list(deps)
            for t in rd:
                d.extend(t.rdeps())
            for t in wr:
                d.extend(t.wdeps())
            tok = S.dma(eng, chan, [(o, i, {}) for (o, i) in items], d)
            for t in rd:
                t.read(tok)
            for t in wr:
                t.write(tok)
            return tok

        def MM(calls, rd=(), wr=()):
            d = []
            for t in rd:
                d.extend(t.rdeps())
            for t in wr:
                d.extend(t.wdeps())
            S.wait("tensor", d)
            for f in calls[:-1]:
                S.op("tensor", f, sig=False)
            tok = S.op("tensor", calls[-1])
            for t in rd:
                t.read(tok)
            for t in wr:
                t.write(tok)
            return tok

        def mm(out, lhsT, rhs, start, stop):
            return lambda e: e.matmul(out, lhsT=lhsT, rhs=rhs, start=start, stop=stop)

        creg = reg("const")
        DMA("sync", "const", [(tabs[:], tabs_d[:, :, :]), (masks[:], masks_d[:, :, :]), (ident[:], ident_d[:, :]),
                              (cols[:], cols_d[:, :]), (esink[:], sinks_d[:, :]),
                              (negm[:].rearrange("p a b -> p (a b)"), negm_d[:, :])], wr=[creg])
        OP("scalar", lambda e: e.activation(out=esink[:], in_=esink[:], func=AF.Exp), rd=[creg], wr=[reg("esink")])
        S.barrier()
        cos128, ssin128, cos64, ssin64 = (tabs[:, i, :] for i in range(4))
        TA0 = 0
        MB0 = 34
        MD0 = 38

        wstate = {"i": 0}

        def wload(items, name):
            s = wstate["i"] % 3
            wstate["i"] += 1
            DMA("gpsimd", f"w{s}", items(s), wr=[wreg[s]])
            return s

        def wv(s):
            return wbuf[:, s, :].rearrange("p (k c) -> p k c", c=256)

        def std_items(l, off, n):
            return lambda s: [(wv(s)[:, :, 0:n], w_in[l, :, off:off + n].rearrange("(k p) c -> p k c", p=128))]

        class WQ:
            def __init__(self, specs):
                self.specs = specs
                self.issued = 0
                self.slots = []
                self.pos = 0

            def _issue(self):
                name, items = self.specs[self.issued]
                self.slots.append(wload(items, name))
                self.issued += 1

            def next(self, name, la=3):
                assert self.specs[self.pos][0] == name, (self.specs[self.pos][0], name)
                while self.issued < min(len(self.specs), self.pos + la):
                    self._issue()
                s = self.slots[self.pos]
                self.pos += 1
                return s

            def prefetch(self):
                while self.issued < min(len(self.specs), self.pos + 3):
                    self._issue()

        def layer_specs(l):
            sp = []
            for nm, off in (("Ak", O_AK), ("Av", O_AV), ("Ck", O_CK), ("Cv", O_CV)):
                for g in range(2):
                    sp.append((f"{nm}{g}", std_items(l, off + 256 * g, 256)))
            sp.append(("Bkv", std_items(l, O_BCKV, 256)))
            sp.append(("Bkr", std_items(l, O_BKR, 64)))
            sp.append(("WUKV", lambda s: [(wbuf[:, s, 0:2048].rearrange("p (k c) -> p k c", k=2),
                                           w_ukv[l, :, :].rearrange("(k p) c -> p k c", p=128))]))
            sp.append(("Dkv", std_items(l, O_DK, 256)))

            def gates(m):
                r = []
                for T in range(2):
                    for g in range(2):
                        r.append((f"G{m}{g}", std_items(l, O_G + 512 * m + 256 * g, 256)))
                return r
            sp += [("Aq0", std_items(l, O_AQ, 256)), ("Aq1", std_items(l, O_AQ + 256, 256))] + gates(0)
            sp += [("Cq0", std_items(l, O_CQ, 256)), ("Cq1", std_items(l, O_CQ + 256, 256))] + gates(2)
            sp += [("Bcq0", std_items(l, O_BCQ, 256)), ("Bcq1", std_items(l, O_BCQ + 256, 128)),
                   ("WUQ", lambda s: [(wbuf[:, s, 0:2304].rearrange("p (k c) -> p k c", k=3),
                                       w_uq[l, :, :].rearrange("(k p) c -> p k c", p=128))])] + gates(1)

            def dq_items(cp):
                def f(s):
                    it = []
                    for i in range(2):
                        c = 2 * cp + i
                        it.append((wv(s)[:, :, 128 * i:128 * i + 64],
                                   w_in[l, :, O_DQ + 64 * c:O_DQ + 64 * c + 64].rearrange("(k p) c -> p k c", p=128)))
                        it.append((wv(s)[:, :, 128 * i + 64:128 * i + 128],
                                   w_in[l, :, O_DQ + 64 * (4 + c):O_DQ + 64 * (4 + c) + 64].rearrange("(k p) c -> p k c", p=128)))
                    return it
                return f
            sp += [("Dq0", dq_items(0)), ("Dq1", dq_items(1))] + gates(3)
            return sp

        ctr = {"pp": 0, "ps": 0, "pt": 0, "st": 0, "po": 0, "sgq": 0, "po8": 0}

        def nxt(k, n):
            v = ctr[k] % n
            ctr[k] += 1
            return v

        def pipeline(items, LA):
            n = len(items)
            for i in range(n + LA):
                if i < n:
                    items[i][0]()
                if i >= LA:
                    items[i - LA][1]()

        xTreg = reg("xT")

        def proj_fm(s, c0, ncol, T):
            b = nxt("pp", 2)
            calls = [mm(P[0:ncol, b, :], wv(s)[:, k, c0:c0 + ncol], xT[:, k, T * 512:(T + 1) * 512], k == 0, k == 15)
                     for k in range(16)]
            MM(calls, rd=[wreg[s], xTreg], wr=[pb[b]])
            return b

        def proj_tm(s, j, ncol, b=None):
            if b is None:
                b = nxt("pp", 2)
            calls = [mm(P[:, b, 0:ncol], xT[:, k, j * 128:(j + 1) * 128], wv(s)[:, k, 0:ncol], k == 0, k == 15)
                     for k in range(16)]
            MM(calls, rd=[wreg[s], xTreg], wr=[pb[b]])
            return b

        rtreg = reg("rt")

        def rope(src_bank, np_, dst, dstregs, half, T, n=512):
            ct, stb = (cos128, ssin128) if half == 64 else (cos64, ssin64)
            src = P[:, src_bank, 0:n]
            tsl = slice(T * 512, T * 512 + n)
            OP("vector", lambda e: e.tensor_tensor(out=rt1[0:np_, 0:n], in0=src[0:np_, :], in1=ct[0:np_, tsl], op=ALU.mult),
               rd=[pb[src_bank]], wr=[rtreg])
            for base in range(0, np_, 2 * half):
                lo, mid, hi = base, base + half, base + 2 * half
                OP("vector", lambda e, lo=lo, mid=mid, hi=hi: e.tensor_tensor(
                    out=rt2[lo:mid, 0:n], in0=src[mid:hi, :], in1=stb[lo:mid, tsl], op=ALU.mult), rd=[pb[src_bank]], wr=[rtreg])
                OP("vector", lambda e, lo=lo, mid=mid, hi=hi: e.tensor_tensor(
                    out=rt2[mid:hi, 0:n], in0=src[lo:mid, :], in1=stb[mid:hi, tsl], op=ALU.mult), rd=[pb[src_bank]], wr=[rtreg])
            OP("vector", lambda e: e.tensor_tensor(out=dst, in0=rt1[0:np_, 0:n], in1=rt2[0:np_, 0:n], op=ALU.add),
               rd=[rtreg], wr=dstregs)

        def transposes_to(dst_fn, src_fn, nchunk, srcregs, dstregs, scale_fn=None, np_out=128):
            c = 0
            while c < nchunk:
                g = min(8, nchunk - c)
                b = nxt("pp", 2)
                pbv = P[:, b, :].bitcast(BF16)
                calls = []
                for i in range(g):
                    calls.append((lambda e, o=pbv[:, i * 128:(i + 1) * 128], s_=src_fn(c + i): e.transpose(o, s_, ident[:])))
                MM(calls, rd=srcregs + [reg("const")], wr=[pb[b]])
                for i in range(g):
                    d_ = dst_fn(c + i)
                    i_ = pbv[0:np_out, i * 128:(i + 1) * 128]
                    if scale_fn is None:
                        OP("scalar", lambda e, d_=d_, i_=i_: e.activation(out=d_, in_=i_, func=AF.Copy), rd=[pb[b]], wr=dstregs)
                    else:
                        OP("scalar", lambda e, d_=d_, i_=i_, sc=scale_fn(c + i): e.activation(out=d_, in_=i_, func=AF.Copy, scale=sc),
                           rd=[pb[b]], wr=dstregs)
                c += g

        def block_to_xT(src_f32, srcreg, j):
            if src_f32 is not None:
                OP("scalar", lambda e: e.activation(out=xb, in_=src_f32, func=AF.Copy), rd=[srcreg], wr=streg)
            for g in range(2):
                bk = nxt("pp", 2)
                pbv = P[:, bk, :].bitcast(BF16)
                MM([(lambda e, o=pbv[:, i * 128:(i + 1) * 128], s_=xb[:, (g * 8 + i) * 128:(g * 8 + i + 1) * 128]: e.transpose(o, s_, ident[:]))
                    for i in range(8)], rd=list(streg) + [reg("const")], wr=[pb[bk]])
                dst = xT[:, g * 8:(g + 1) * 8, j * 128:(j + 1) * 128]
                src = pbv[:, :].rearrange("p (c i) -> p c i", c=8)
                if g == 0:
                    OP("scalar", lambda e, dst=dst, src=src: e.activation(out=dst, in_=src, func=AF.Copy), rd=[pb[bk]], wr=[xTreg])
                else:
                    OP("vector", lambda e, dst=dst, src=src: e.tensor_copy(out=dst, in_=src), rd=[pb[bk]], wr=[xTreg])

        def stage_out(src_ap_fn, srcregs, dst_dram, eng="scalar"):
            s = nxt("st", 4)
            src, shape_p, shape_n = src_ap_fn
            OP(eng, lambda e: e.activation(out=stage[0:shape_p, s, 0:shape_n], in_=src, func=AF.Copy) if eng == "scalar"
               else e.tensor_copy(out=stage[0:shape_p, s, 0:shape_n], in_=src), rd=srcregs, wr=[streg[s]])
            DMA("sync", f"st{s}", [(dst_dram, stage[0:shape_p, s, 0:shape_n])], rd=[streg[s]])

        def rmsnorm_rows(bank, ncol, dst_bf, dstregs, eps=1e-6):
            r = reg("sm")
            OP("vector", lambda e: e.memset(sm[:, 0:1], 0.0), wr=[r])
            OP("scalar", lambda e: e.activation(out=rt1[:, 0:ncol], in_=P[:, bank, 0:ncol], func=AF.Square, accum_out=sm[:, 0:1]),
               rd=[pb[bank]], wr=[r, rtreg])
            OP("vector", lambda e: e.tensor_scalar(out=sm[:, 1:2], in0=sm[:, 0:1], scalar1=1.0 / ncol, scalar2=eps, op0=ALU.mult, op1=ALU.add),
               rd=[r], wr=[r])
            OP("scalar", lambda e: e.activation(out=sm[:, 2:3], in_=sm[:, 1:2], func=AF.Ln), rd=[r], wr=[r])
            OP("scalar", lambda e: e.activation(out=sm[:, 3:4], in_=sm[:, 2:3], func=AF.Exp, scale=-0.5), rd=[r], wr=[r])
            OP("scalar", lambda e: e.activation(out=dst_bf, in_=P[:, bank, 0:ncol], func=AF.Copy, scale=sm[:, 3:4]),
               rd=[r, pb[bank]], wr=dstregs)

        xrreg = wreg[2]
        for j in range(NB):
            DMA("sync", "xr", [(xr, x_in[j * 128:(j + 1) * 128, :])], wr=[xrreg])
            block_to_xT(xr, xrreg, j)
        S.barrier()

        for l in range(depth):
            WS = WQ(layer_specs(l))
            ib, ob = ibs[l], obs[l]
            ccoll = [None] * 4
            areg = {n: reg(f"a_{n}") for n in ("kT", "V", "qT", "qrT", "krT", "cqnT", "kmean", "ckvnT")}

            def collective(i, deps):
                S.wait("gpsimd", deps)
                key = f"cc{i}"
                S.sem(key)
                tok = S.op("gpsimd", lambda e, ii=ib[i], oo=ob[i]: e.collective_compute(
                    "AllGather", ALU.bypass, replica_groups=PAIRS, ins=[ii.ap().opt()], outs=[oo.ap().opt()]), key=key)
                ccoll[i] = tok

            def load_ctx_AC(ci, wait_ones=True):
                items = []
                for rr in range(2):
                    items.append((kT[:, :, rr * 1024:(rr + 1) * 1024],
                                  ob[ci][rr * 1024:rr * 1024 + 512, :].rearrange("(h d) t -> d h t", d=128)))
                DMA("sync", "ctxk", items, wr=[areg["kT"]], deps=[ccoll[ci]])
                items = []
                for rr in range(2):
                    src = ob[ci][rr * 1024 + 512:rr * 1024 + 1024, :].rearrange("r (two c) -> (r two) c", two=2)
                    for h in range(4):
                        items.append((Vaug[:, rr * 8:(rr + 1) * 8, h, 0:128],
                                      src[:, h * 128:(h + 1) * 128].rearrange("(jj p) c -> p jj c", p=128)))
                DMA("sync", "ctxv", items, wr=[areg["V"]], deps=[ccoll[ci]])
                OP("vector", lambda e: e.memset(Vaug[:, :, :, 128:129], 1.0), wr=[areg["V"]])

            def kside_AC(nm, ci):
                toks = []
                for g in range(2):
                    s = WS.next(f"{nm}k{g}")
                    for hh in range(2):
                        h = 2 * g + hh
                        for T in range(2):
                            b = proj_fm(s, hh * 128, 128, T)
                            st = nxt("st", 4)
                            rope(b, 128, stage[:, st, :], [streg[st]], 64, T)
                            toks.append(DMA("sync", f"st{st}", [(ib[ci][h * 128:(h + 1) * 128, T * 512:(T + 1) * 512], stage[:, st, :])],
                                            rd=[streg[st]]))
                vview = ib[ci][512:1024, :].rearrange("r (two c) -> (r two) c", two=2)
                for g in range(2):
                    s = WS.next(f"{nm}v{g}")
                    for j in range(NB):
                        b = proj_tm(s, j, 256)
                        st = nxt("st", 4)
                        OP("scalar", lambda e, b=b, st=st: e.activation(out=stage[:, st, 0:256], in_=P[:, b, 0:256], func=AF.Copy),
                           rd=[pb[b]], wr=[streg[st]])
                        toks.append(DMA("sync", f"st{st}", [(vview[j * 128:(j + 1) * 128, g * 256:(g + 1) * 256], stage[:, st, 0:256])],
                                        rd=[streg[st]]))
                collective(ci, toks)

            kside_AC("A", 0)
            kside_AC("C", 1)
            load_ctx_AC(0)

            toksB, toksD = [], []
            s = WS.next("Bkv")
            its = []
            for j in range(NB):
                def s1(j=j, s=s):
                    b = proj_tm(s, j, 256, b=2 + j % 2)
                    rmsnorm_rows(b, 256, cqb[:, j % 2, 0:256], [reg(f"cqb{j % 2}")])

                def s2(j=j):
                    transposes_to(lambda c: ckvnT[:, c, j * 128:(j + 1) * 128], lambda c: cqb[:, j % 2, c * 128:(c + 1) * 128], 2,
                                  [reg(f"cqb{j % 2}")], [areg["ckvnT"]], scale_fn=lambda c: cols[:, 21 * l + 3 + c:21 * l + 4 + c])
                its.append((s1, s2))
            pipeline(its, 1)
            s = WS.next("Bkr")
            for T in range(2):
                b = proj_fm(s, 0, 64, T)
                st = nxt("st", 4)
                rope(b, 64, stage[0:64, st, :], [streg[st]], 32, T)
                toksD.append(DMA("sync", f"st{st}", [(ib[3][0:64, T * 512:(T + 1) * 512], stage[0:64, st, :])], rd=[streg[st]]))
            s = WS.next("WUKV")
            wkv = wbuf[:, s, 0:2048].rearrange("p (k h c) -> p k h c", k=2, h=4)
            for h in range(4):
                for T in range(2):
                    b = nxt("pp", 2)
                    MM([mm(P[:, b, :], wkv[:, k, h, 0:128], ckvnT[:, k, T * 512:(T + 1) * 512], k == 0, k == 1) for k in range(2)],
                       rd=[wreg[s], areg["ckvnT"]], wr=[pb[b]])
                    st = nxt("st", 4)
                    OP("scalar", lambda e, b=b, st=st: e.activation(out=stage[:, st, :], in_=P[:, b, :], func=AF.Copy),
                       rd=[pb[b]], wr=[streg[st]])
                    toksB.append(DMA("sync", f"st{st}", [(ib[2][h * 128:(h + 1) * 128, T * 512:(T + 1) * 512], stage[:, st, :])],
                                     rd=[streg[st]]))
            vviewB = ib[2][512:1024, :].rearrange("r (two c) -> (r two) c", two=2)
            for j in range(NB):
                b = nxt("pp", 2)
                MM([mm(P[:, b, :].rearrange("p (h c) -> p h c", h=4), ckvnT[:, k, j * 128:(j + 1) * 128], wkv[:, k, :, 128:256], k == 0, k == 1)
                    for k in range(2)], rd=[wreg[s], areg["ckvnT"]], wr=[pb[b]])
                st = nxt("st", 4)
                OP("scalar", lambda e, b=b, st=st: e.activation(out=stage[:, st, :], in_=P[:, b, :], func=AF.Copy),
                   rd=[pb[b]], wr=[streg[st]])
                toksB.append(DMA("sync", f"st{st}", [(vviewB[j * 128:(j + 1) * 128, :], stage[:, st, :])], rd=[streg[st]]))
            collective(2, toksB)

            s = WS.next("Dkv")
            for T in range(2):
                b = proj_fm(s, 0, 128, T)
                st = nxt("st", 4)
                rope(b, 128, stage[:, st, :], [streg[st]], 32, T)
                toksD.append(DMA("sync", f"st{st}", [(ib[3][64:192, T * 512:(T + 1) * 512], stage[:, st, :])], rd=[streg[st]]))
            vviewD = ib[3][192:320, :].rearrange("r (e c) -> (r e) c", e=8)
            for j in range(NB):
                b = nxt("pp", 2)
                MM([mm(P[:, b, 0:128], xT[:, k, j * 128:(j + 1) * 128], wv(s)[:, k, 128:256], k == 0, k == 15) for k in range(16)],
                   rd=[wreg[s], xTreg], wr=[pb[b]])
                st = nxt("st", 4)
                OP("scalar", lambda e, b=b, st=st: e.activation(out=stage[:, st, 0:128], in_=P[:, b, 0:128], func=AF.Copy),
                   rd=[pb[b]], wr=[streg[st]])
                toksD.append(DMA("sync", f"st{st}", [(vviewD[j * 128:(j + 1) * 128, :], stage[:, st, 0:128])], rd=[streg[st]]))
            collective(3, toksD)

            def qproj_AC(nm):
                for g in range(2):
                    s = WS.next(f"{nm}q{g}")
                    for hh in range(2):
                        h = 2 * g + hh
                        for T in range(2):
                            b = proj_fm(s, hh * 128, 128, T)
                            rope(b, 128, qT[:, h, T * 512:(T + 1) * 512], [areg["qT"]], 64, T)

            smr = {k: reg("sm_" + k) for k in ("norm", "epi", "sel0", "sel1", "D", "km", "ssq")}
            ssq = sm[:, 208:240].rearrange("p (b h) -> p b h", b=4)
            yreg = reg("ytile")
            SB = (2, 3, 0, 1)

            def sumsq(b, col, lo, hi):
                OP("vector", lambda e: e.scalar_tensor_tensor(out=rt1[:, 0:hi - lo], in0=ytile[:, b, lo:hi], scalar=1.0, in1=ytile[:, b, lo:hi],
                                                              op0=ALU.mult, op1=ALU.mult, accum_out=ssq[:, b, col:col + 1]),
                   rd=[yreg], wr=[rtreg, smr["ssq"]])

            def normalize_po(slots, h, src4=None, srcregs=None):
                r = smr["norm"]
                if src4 is None:
                    den = po8[:, slots[0]:slots[0] + 4, 128:129]
                    rg = [por[s_] for s_ in slots]
                else:
                    den = src4[:, :, 128:129]
                    rg = srcregs
                OP("vector", lambda e: e.reciprocal(out=sm[:, 8:12].rearrange("p (b o) -> p b o", o=1), in_=den), rd=rg, wr=[r])
                for b in range(4):
                    src = po8[:, slots[b], :] if src4 is None else src4[:, b, :]
                    OP("vector", lambda e, src=src, b=b: e.tensor_scalar(out=ytile[:, b, h * 128:(h + 1) * 128], in0=src[:, 0:128],
                                                                       scalar1=sm[:, 8 + b:9 + b], scalar2=None, op0=ALU.mult),
                       rd=rg + [r], wr=[yreg])
                    sumsq(b, h, h * 128, (h + 1) * 128)

            def scores(T, h, G, isB, scale):
                kp = kap(G)
                sbk = SB[nxt("ps", 4)]
                calls = [mm(P[:, sbk, :], kT[:, h, kp * 128:(kp + 1) * 128], qT[:, h, T * 512:(T + 1) * 512], True, not isB)]
                rd = [areg["kT"], areg["qT"]]
                if isB:
                    calls.append(mm(P[:, sbk, :], krT[:, kp * 128:(kp + 1) * 128], qrT[:, h, T * 512:(T + 1) * 512], False, True))
                    rd += [areg["krT"], areg["qrT"]]
                MM(calls, rd=rd, wr=[pb[sbk]])
                ps_ = nxt("pt", 4)
                OP("scalar", lambda e: e.activation(out=pt[:, ps_, :], in_=P[:, sbk, :], func=AF.Exp, scale=scale),
                   rd=[pb[sbk]], wr=[ptreg[ps_]])
                return ps_, kp

            def mask128(ps_, b, slot):
                OP("vector", lambda e: e.tensor_tensor(out=pt[:, ps_, b * 128:(b + 1) * 128], in0=pt[:, ps_, b * 128:(b + 1) * 128],
                                                        in1=masks[:, slot, :], op=ALU.mult), rd=[ptreg[ps_], creg], wr=[ptreg[ps_]])

            def maskA(ps_, T, G):
                j0 = 4 * T
                t0 = 2 * j0 + 1 - G
                bmin = 0
                while t0 + 2 * bmin < 0:
                    bmin += 1
                slot = lambda b: TA0 + (t0 + 2 * b) * 2 + (b % 2)
                if bmin == 0:
                    mk = bass.AP(masks, slot(0) * 128, [[44 * 128, 128], [8 * 128, 2], [5 * 128, 2], [1, 128]])
                    ptv = pt[:, ps_, :].rearrange("p (u v i) -> p u v i", u=2, v=2)
                    OP("vector", lambda e: e.tensor_tensor(out=ptv, in0=ptv, in1=mk, op=ALU.mult), rd=[ptreg[ps_], creg], wr=[ptreg[ps_]])
                    return
                if bmin == 1:
                    mask128(ps_, 1, slot(1))
                if bmin <= 2:
                    mk = bass.AP(masks, slot(2) * 128, [[44 * 128, 128], [5 * 128, 2], [1, 128]])
                    ptv = pt[:, ps_, 256:512].rearrange("p (v i) -> p v i", v=2)
                    OP("vector", lambda e: e.tensor_tensor(out=ptv, in0=ptv, in1=mk, op=ALU.mult), rd=[ptreg[ps_], creg], wr=[ptreg[ps_]])
                else:
                    mask128(ps_, 3, slot(3))

            def epilogue(m, T):
                r = smr["epi"]
                if debug:
                    for b in range(4):
                        DMA("sync", "dbg", [(ydbg[(4 * T + b) * 128:(4 * T + b + 1) * 128, m * 512:(m + 1) * 512], ytile[:, b, :])], rd=[yreg])
                nh = 8 if m == 3 else 4
                OP("vector", lambda e: e.tensor_reduce(out=sm[:, 16:20], in_=ssq[:, :, 0:nh], op=ALU.add, axis=AX.X), rd=[smr["ssq"]], wr=[r])
                OP("vector", lambda e: e.tensor_scalar(out=sm[:, 20:24], in0=sm[:, 16:20], scalar1=1.0 / 512, scalar2=1e-6, op0=ALU.mult, op1=ALU.add),
                   rd=[r], wr=[r])
                OP("scalar", lambda e: e.activation(out=sm[:, 24:28], in_=sm[:, 20:24], func=AF.Ln), rd=[r], wr=[r])
                OP("scalar", lambda e: e.activation(out=sm[:, 28:32], in_=sm[:, 24:28], func=AF.Exp, scale=-0.5), rd=[r], wr=[r])
                its = []
                slots_g = [WS.next(f"G{m}{g}", la=3 - g) for g in range(2)]
                for g in range(2):
                    s = slots_g[g]
                    for b in range(4):
                        q = nxt("sgq", 2)
                        j = 4 * T + b

                        def sA(s=s, b=b, g=g, q=q, j=j):
                            bk = proj_tm(s, j, 256)
                            OP("scalar", lambda e: e.activation(out=sg[:, q, :], in_=P[:, bk, 0:256], func=AF.Silu),
                               rd=[pb[bk]], wr=[reg(f"sg{q}")])
                            OP("vector", lambda e: e.scalar_tensor_tensor(
                                out=ygb[:, q, :], in0=ytile[:, b, g * 256:(g + 1) * 256], scalar=sm[:, 28 + b:29 + b], in1=sg[:, q, :],
                                op0=ALU.mult, op1=ALU.mult), rd=[yreg, r, reg(f"sg{q}")], wr=[reg(f"yg{q}")])

                        def sB(g=g, q=q, j=j):
                            c0 = m * 4 + g * 2
                            transposes_to(lambda c: yT[:, c0 + c, j * 128:(j + 1) * 128],
                                          lambda c: ygb[:, q, c * 128:(c + 1) * 128], 2, [reg(f"yg{q}")], [reg("yT")],
                                          scale_fn=lambda c: cols[:, 21 * l + 5 + c0 + c:21 * l + 6 + c0 + c])
                        its.append((sA, sB))
                pipeline(its, 1)

            def dense_stream(m, isA, isB, scale):
                items = []
                for T in range(2):
                    j0 = 4 * T
                    nG = 8 * T + 8
                    for h in range(4):
                        st = {}
                        for G in range(nG):
                            d = {}

                            def s1(T=T, h=h, G=G, d=d, st=st, j0=j0):
                                if G == 0:
                                    ob_ = nxt("po", 2)
                                    st["slots"] = [ob_ * 4 + b for b in range(4)]
                                d["ps"], d["kp"] = scores(T, h, G, isB, scale)
                                if isA:
                                    maskA(d["ps"], T, G)
                                else:
                                    for b in range(4):
                                        j = j0 + b
                                        if 2 * j <= G <= 2 * j + 1:
                                            mask128(d["ps"], b, MB0 + (j % 2) * 2 + (G - 2 * j))

                            def s2(T=T, h=h, G=G, d=d, st=st, j0=j0, nG=nG):
                                slots = st["slots"]
                                calls, wr = [], []
                                for b in range(4):
                                    j = j0 + b
                                    if G <= 2 * j + 1:
                                        calls.append(lambda e, b=b, j=j: e.matmul(
                                            po8[:, slots[b], 0:129], lhsT=pt[:, d["ps"], b * 128:(b + 1) * 128], rhs=Vaug[:, d["kp"], h, :],
                                            start=(G == 0 and b % 2 == 0), stop=(G == 2 * j + 1), skip_group_check=True))
                                        wr.append(por[slots[b]])
                                MM(calls, rd=[ptreg[d["ps"]], areg["V"]], wr=wr)
                                if G == nG - 1:
                                    normalize_po(slots, h)
                                    if h == 3:
                                        epilogue(m, T)
                            items.append((s1, s2))
                pipeline(items, 2)

            def zero_ssq():
                OP("vector", lambda e: e.memset(sm[:, 208:240], 0.0), wr=[smr["ssq"]])

            qproj_AC("A")
            zero_ssq()
            dense_stream(0, True, False, 128 ** -0.5)

            load_ctx_AC(1)
            zero_ssq()
            r = smr["km"]
            for h in range(4):
                OP("vector", lambda e, h=h: e.tensor_reduce(out=sm[:, 32:48], in_=kT[:, h, :].rearrange("p (k t) -> p k t", k=16),
                                                            op=ALU.add, axis=AX.X), rd=[areg["kT"]], wr=[r])
                OP("vector", lambda e: e.tensor_tensor(out=sm[:, 48:56], in0=sm[:, 32:40], in1=sm[:, 40:48], op=ALU.add), rd=[r], wr=[r])
                OP("vector", lambda e, h=h: e.tensor_scalar(out=kmean[:, h, :], in0=sm[:, 48:56], scalar1=1.0 / 256, scalar2=None, op0=ALU.mult),
                   rd=[r], wr=[areg["kmean"]])
            qproj_AC("C")
            selbuf = [sm[:, 64:96].rearrange("p (b n) -> p b n", b=4), sm[:, 144:176].rearrange("p (b n) -> p b n", b=4)]
            gmv = sm[:, 96:128].rearrange("p (b n) -> p b n", b=4)
            top = sm[:, 176:208].rearrange("p (b n) -> p b n", b=4)
            gmr = reg("sm_gm")
            accr = reg("accC")
            items = []
            grp = 0
            for T in range(2):
                j0 = 4 * T
                for h in range(4):
                    st = {"first": [True] * 4}
                    sel = selbuf[grp % 2]
                    selr = smr[f"sel{grp % 2}"]
                    grp += 1
                    nN = j0 + 4
                    for n in range(nN):
                        d = {}

                        def s1(T=T, h=h, n=n, d=d, j0=j0, sel=sel, selr=selr):
                            if n == 0:
                                bk = nxt("pp", 2)
                                MM([mm(P[:, bk, b * 8:(b + 1) * 8], qT[:, h, (j0 + b) * 128:(j0 + b + 1) * 128], kmean[:, h, :], True, True)
                                    for b in range(4)], rd=[areg["qT"], areg["kmean"]], wr=[pb[bk]])
                                OP("vector", lambda e: e.tensor_tensor(out=gmv, in0=P[:, bk, 0:32].rearrange("p (b n) -> p b n", b=4),
                                                                       in1=negm[:, j0:j0 + 4, :], op=ALU.add), rd=[pb[bk], creg], wr=[gmr])
                                for b in range(4):
                                    OP("vector", lambda e, b=b: e.max(out=top[:, b, :], in_=gmv[:, b, :]), rd=[gmr], wr=[gmr])
                                for b in range(4):
                                    OP("vector", lambda e, b=b: e.tensor_scalar(out=sel[:, b, :], in0=gmv[:, b, :], scalar1=top[:, b, 2:3], scalar2=None,
                                                                                op0=ALU.is_ge), rd=[gmr], wr=[selr])
                            d["pss"] = []
                            for w in range(2):
                                ps_, kp = scores(T, h, 2 * n + w, False, 128 ** -0.5)
                                for b in range(4):
                                    if j0 + b == n:
                                        mask128(ps_, b, MB0 + (n % 2) * 2 + w)
                                d["pss"].append((ps_, kp))

                        def s2(T=T, h=h, n=n, d=d, j0=j0, sel=sel, selr=selr, st=st, nN=nN):
                            ob_ = nxt("po", 2)
                            slots = [ob_ * 4 + b for b in range(4)]
                            pss = d["pss"]
                            calls, wr = [], []
                            for b in range(4):
                                if j0 + b >= n:
                                    for w in range(2):
                                        ps_, kp = pss[w]
                                        calls.append(mm(po8[:, slots[b], 0:129], pt[:, ps_, b * 128:(b + 1) * 128], Vaug[:, kp, h, :], w == 0, w == 1))
                                    wr.append(por[slots[b]])
                            MM(calls, rd=[ptreg[pss[0][0]], ptreg[pss[1][0]], areg["V"]], wr=wr)
                            first = st["first"]
                            for b in range(4):
                                j = j0 + b
                                if j < n:
                                    continue
                                src = po8[:, slots[b], 0:129]
                                dst = accC[:, b, 0:129]
                                if j == n:
                                    if first[b]:
                                        OP("vector", lambda e, src=src, dst=dst: e.tensor_copy(out=dst, in_=src), rd=[por[slots[b]]], wr=[accr])
                                    else:
                                        OP("vector", lambda e, src=src, dst=dst: e.tensor_tensor(out=dst, in0=src, in1=dst, op=ALU.add),
                                           rd=[por[slots[b]]], wr=[accr])
                                else:
                                    if first[b]:
                                        OP("vector", lambda e, src=src, dst=dst, b=b: e.tensor_scalar(
                                            out=dst, in0=src, scalar1=sel[:, b, n:n + 1], scalar2=None, op0=ALU.mult),
                                           rd=[por[slots[b]], selr], wr=[accr])
                                    else:
                                        OP("vector", lambda e, src=src, dst=dst, b=b: e.scalar_tensor_tensor(
                                            out=dst, in0=src, scalar=sel[:, b, n:n + 1], in1=dst, op0=ALU.mult, op1=ALU.add),
                                           rd=[por[slots[b]], selr], wr=[accr])
                                first[b] = False
                            if n == nN - 1:
                                normalize_po(None, h, src4=accC, srcregs=[accr])
                                if h == 3:
                                    epilogue(2, T)
                        items.append((s1, s2))
            pipeline(items, 1)

            load_ctx_AC(2)
            DMA("sync", "ctxr", [(krT[0:64, rr * 1024:(rr + 1) * 1024], ob[3][rr * 320:rr * 320 + 64, :]) for rr in range(2)],
                wr=[areg["krT"]], deps=[ccoll[3]])
            OP("gpsimd", lambda e: e.memset(krT[64:128, :], 0.0), wr=[areg["krT"]])
            OP("gpsimd", lambda e: e.memset(qrT[64:128, :, :], 0.0), wr=[areg["qrT"]])
            s0 = WS.next("Bcq0")
            s1_ = WS.next("Bcq1", la=2)
            its = []
            for j in range(NB):
                def s1(j=j):
                    bk = 2 + j % 2
                    calls = [mm(P[:, bk, 0:256], xT[:, k, j * 128:(j + 1) * 128], wv(s0)[:, k, 0:256], k == 0, k == 15) for k in range(16)]
                    calls += [mm(P[:, bk, 256:384], xT[:, k, j * 128:(j + 1) * 128], wv(s1_)[:, k, 0:128], k == 0, k == 15) for k in range(16)]
                    MM(calls, rd=[wreg[s0], wreg[s1_], xTreg], wr=[pb[bk]])
                    rmsnorm_rows(bk, 384, cqb[:, j % 2, 0:384], [reg(f"cqb{j % 2}")])

                def s2(j=j):
                    transposes_to(lambda c: cqnT[:, c, j * 128:(j + 1) * 128], lambda c: cqb[:, j % 2, c * 128:(c + 1) * 128], 3,
                                  [reg(f"cqb{j % 2}")], [areg["cqnT"]], scale_fn=lambda c: cols[:, 21 * l + c:21 * l + 1 + c])
                its.append((s1, s2))
            pipeline(its, 1)
            s = WS.next("WUQ")
            wq = wbuf[:, s, 0:2304].rearrange("p (k h c) -> p k h c", k=3, h=4)
            for h in range(4):
                for T in range(2):
                    bk = nxt("pp", 2)
                    MM([mm(P[:, bk, :], wq[:, k, h, 0:128], cqnT[:, k, T * 512:(T + 1) * 512], k == 0, k == 2) for k in range(3)],
                       rd=[wreg[s], areg["cqnT"]], wr=[pb[bk]])
                    OP("scalar", lambda e, bk=bk, h=h, T=T: e.activation(out=qT[:, h, T * 512:(T + 1) * 512], in_=P[:, bk, :], func=AF.Copy),
                       rd=[pb[bk]], wr=[areg["qT"]])
                    bk = nxt("pp", 2)
                    MM([mm(P[0:64, bk, :], wq[:, k, h, 128:192], cqnT[:, k, T * 512:(T + 1) * 512], k == 0, k == 2) for k in range(3)],
                       rd=[wreg[s], areg["cqnT"]], wr=[pb[bk]])
                    rope(bk, 64, qrT[0:64, h, T * 512:(T + 1) * 512], [areg["qrT"]], 32, T)
            zero_ssq()
            dense_stream(1, False, True, 192 ** -0.5)

            tokB = []
            for rg in areg.values():
                tokB.extend(rg.wdeps())
            dreg = {n: Reg() for n in ("kT", "V", "qT")}
            for rg in dreg.values():
                for t in tokB:
                    rg.read(t)
            kTD = av(24512, 2048)
            VD = av(26560, 2080).rearrange("p (k h c) -> p k h c", k=16, h=2)
            qTD = av(28640, 4096).rearrange("p (h t) -> p h t", h=4)
            items = [(kTD[:, rr * 1024:(rr + 1) * 1024], ob[3][rr * 320 + 64:rr * 320 + 192, :]) for rr in range(2)]
            DMA("sync", "ctxk", items, wr=[dreg["kT"]], deps=[ccoll[3]])
            items = []
            for rr in range(2):
                src = ob[3][rr * 320 + 192:rr * 320 + 320, :].rearrange("r (e c) -> (r e) c", e=8)
                for hh in range(2):
                    items.append((VD[:, rr * 8:(rr + 1) * 8, hh, 0:64], src[:, hh * 64:(hh + 1) * 64].rearrange("(jj p) c -> p jj c", p=128)))
            DMA("sync", "ctxv", items, wr=[dreg["V"]], deps=[ccoll[3]])
            OP("vector", lambda e: e.memset(VD[:, :, :, 64:65], 1.0), wr=[dreg["V"]])
            for g in range(2):
                s = WS.next(f"Dq{g}")
                for cc in range(2):
                    c = 2 * g + cc
                    for T in range(2):
                        bk = proj_fm(s, cc * 128, 128, T)
                        rope(bk, 128, qTD[:, c, T * 512:(T + 1) * 512], [dreg["qT"]], 32, T)
            WS.prefetch()
            DMA("gpsimd", "wo", [(wo[:, 0:11, q * 512:(q + 1) * 512],
                                  w_out[l, 0:11 * 128, q * 512:(q + 1) * 512].rearrange("(k p) c -> p k c", p=128)) for q in range(4)],
                wr=[reg("wo_lo")], deps=tokB)
            zero_ssq()
            r = smr["D"]
            items = []
            for T in range(2):
                for b in range(4):
                    for c in range(4):
                        for sl in range(2):
                            d = {}
                            j = 4 * T + b
                            Gs = [G for G in (2 * j - 1, 2 * j, 2 * j + 1) if G >= 0]

                            def s1(j=j, c=c, sl=sl, d=d, Gs=Gs):
                                w0 = 3 - len(Gs)
                                prt = slice(64 * sl, 64 * sl + 64)
                                sbk = SB[nxt("ps", 4)]
                                MM([mm(P[:, sbk, (w0 + i) * 128:(w0 + i + 1) * 128], kTD[prt, kap(G) * 128:(kap(G) + 1) * 128],
                                       qTD[prt, c, j * 128:(j + 1) * 128], True, True) for i, G in enumerate(Gs)],
                                   rd=[dreg["kT"], dreg["qT"]], wr=[pb[sbk]])
                                ps_ = nxt("pt", 4)
                                d["ps"] = ps_
                                OP("scalar", lambda e: e.activation(out=pt[:, ps_, w0 * 128:384], in_=P[:, sbk, w0 * 128:384],
                                                                    func=AF.Exp, scale=0.125), rd=[pb[sbk]], wr=[ptreg[ps_]])
                                OP("vector", lambda e: e.tensor_tensor(
                                    out=pt[:, ps_, w0 * 128:384], in0=pt[:, ps_, w0 * 128:384],
                                    in1=masks[:, MD0 + (j % 2) * 3 + w0:MD0 + (j % 2) * 3 + 3, :].rearrange("p a b -> p (a b)"), op=ALU.mult),
                                   rd=[ptreg[ps_], creg], wr=[ptreg[ps_]])

                            def s2(T=T, b=b, c=c, sl=sl, d=d, Gs=Gs, l=l):
                                w0 = 3 - len(Gs)
                                ps_ = d["ps"]
                                hd = c + 4 * sl
                                slot = (0, 2, 4, 6, 1, 3, 5, 7)[nxt("po8", 8)]
                                MM([mm(po8[:, slot, 0:65], pt[:, ps_, (w0 + i) * 128:(w0 + i + 1) * 128], VD[:, kap(G), sl, :], i == 0, i == len(Gs) - 1)
                                    for i, G in enumerate(Gs)], rd=[ptreg[ps_], dreg["V"]], wr=[por[slot]])
                                u = 240 + (hd % 8)
                                OP("vector", lambda e: e.tensor_tensor(out=sm[:, u:u + 1], in0=po8[:, slot, 64:65],
                                                                       in1=esink[:, 8 * l + hd:8 * l + hd + 1], op=ALU.add),
                                   rd=[por[slot], reg("esink")], wr=[r])
                                OP("vector", lambda e: e.reciprocal(out=sm[:, u + 8:u + 9], in_=sm[:, u:u + 1]), rd=[r], wr=[r])
                                OP("vector", lambda e: e.tensor_scalar(out=ytile[:, b, hd * 64:(hd + 1) * 64], in0=po8[:, slot, 0:64],
                                                                       scalar1=sm[:, u + 8:u + 9], scalar2=None, op0=ALU.mult),
                                   rd=[por[slot], r], wr=[yreg])
                                sumsq(b, hd, hd * 64, (hd + 1) * 64)
                                if b == 3 and c == 3 and sl == 1:
                                    epilogue(3, T)
                            items.append((s1, s2))
            pipeline(items, 2)

            tokD = []
            for rg in dreg.values():
                tokD.extend(rg.wdeps())
            DMA("gpsimd", "wo", [(wo[:, 11:16, q * 512:(q + 1) * 512],
                                  w_out[l, 11 * 128:16 * 128, q * 512:(q + 1) * 512].rearrange("(k p) c -> p k c", p=128)) for q in range(4)],
                wr=[reg("wo_hi")], deps=tokD)
            xsrc = x_in if l == 0 else xs[(l - 1) % 2]
            xdst = out_d if l == depth - 1 else xs[l % 2]
            zreg = reg("ytile")
            r = reg("sm")
            def outproj(j):
                DMA("sync", "xr", [(xr, xsrc[j * 128:(j + 1) * 128, :])], wr=[wreg[2]])
                if j == 0:
                    DMA("sync", "gb", [(gam, ln_g[l:l + 1, :].partition_broadcast(128)[:, 0, :]),
                                       (bet, ln_b[l:l + 1, :].partition_broadcast(128)[:, 0, :])], wr=[wreg[0], wreg[1]])
                for hf in range(2):
                    for q in (2 * hf, 2 * hf + 1):
                        MM([mm(P[:, 4 + q, :], yT[:, k, j * 128:(j + 1) * 128], wo[:, k, q * 512:(q + 1) * 512], k == 0, False) for k in range(11)],
                           rd=[reg("wo_lo"), reg("yT")], wr=[por[2 * q], por[2 * q + 1]])
                    for q in (2 * hf, 2 * hf + 1):
                        MM([mm(P[:, 4 + q, :], yT[:, k, j * 128:(j + 1) * 128], wo[:, k, q * 512:(q + 1) * 512], False, k == 15) for k in range(11, 16)],
                           rd=[reg("wo_hi"), reg("yT")], wr=[por[2 * q], por[2 * q + 1]])

            outproj(0)
            for j in range(NB):
                for q in range(4):
                    OP("vector", lambda e, q=q: e.scalar_tensor_tensor(out=zt[:, q * 512:(q + 1) * 512], in0=xr[:, q * 512:(q + 1) * 512], scalar=ALPHA,
                                                                      in1=P[:, 4 + q, :], op0=ALU.mult, op1=ALU.add),
                       rd=[wreg[2], por[2 * q], por[2 * q + 1]], wr=[zreg])
                    OP("vector", lambda e, q=q: e.bn_stats(out=sm[:, 176 + 6 * q:182 + 6 * q], in_=zt[:, q * 512:(q + 1) * 512]), rd=[zreg], wr=[r])
                OP("vector", lambda e: e.bn_aggr(out=sm[:, 200:202], in_=sm[:, 176:200]), rd=[r], wr=[r])
                OP("vector", lambda e: e.tensor_scalar(out=sm[:, 202:203], in0=sm[:, 201:202], scalar1=1e-5, scalar2=None, op0=ALU.add), rd=[r], wr=[r])
                OP("scalar", lambda e: e.activation(out=sm[:, 203:204], in_=sm[:, 202:203], func=AF.Ln), rd=[r], wr=[r])
                OP("scalar", lambda e: e.activation(out=sm[:, 204:205], in_=sm[:, 203:204], func=AF.Exp, scale=-0.5), rd=[r], wr=[r])
                OP("vector", lambda e: e.scalar_tensor_tensor(out=zt, in0=zt, scalar=sm[:, 200:201], in1=gam, op0=ALU.subtract, op1=ALU.mult),
                   rd=[r, zreg, wreg[0]], wr=[zreg])
                OP("vector", lambda e: e.scalar_tensor_tensor(out=zt, in0=zt, scalar=sm[:, 204:205], in1=bet, op0=ALU.mult, op1=ALU.add),
                   rd=[r, zreg, wreg[1]], wr=[zreg])
                DMA("sync", "xo", [(xdst[j * 128:(j + 1) * 128, :], zt)], rd=[zreg])
                if l < depth - 1:
                    OP("scalar", lambda e: e.activation(out=xb, in_=zt, func=AF.Copy), rd=[zreg], wr=streg)
                if j + 1 < NB:
                    outproj(j + 1)
                if l < depth - 1:
                    block_to_xT(None, None, j)
            S.barrier()

        S.emit()
    return nc


def _mult(delta):
    m = np.zeros_like(delta, dtype=np.float32)
    ok = delta >= 0
    m += (ok & (delta <= 128))
    m += (ok & (delta % 4 == 0) & (delta <= 512))
    m += (ok & (delta % 16 == 0) & (delta <= 2048))
    return m


def host_tables(r):
    i = np.arange(128)[None, :]
    k = np.arange(128)[:, None]
    masks = np.zeros((128, 44, 128), np.float32)
    for jpar in range(2):
        e = (jpar + r) % 2
        for t in range(17):
            masks[:, t * 2 + jpar, :] = _mult((t - 1 + e) * 128 + i - k)
        for w in range(2):
            masks[:, 34 + jpar * 2 + w, :] = (((e - w) * 128 + i - k) >= 0)
        for w in range(3):
            dd = (e + 1 - w) * 128 + i - k
            masks[:, 38 + jpar * 3 + w, :] = ((dd >= 0) & (dd <= 127))
    pos = np.concatenate([gblk(r, j) * 128 + np.arange(128) for j in range(NB)]).astype(np.float32)
    tabs = np.zeros((128, 4, 1024), np.float32)
    p = np.arange(128)
    inv128 = (np.float32(10000.0) ** (-np.arange(0, 128, 2, dtype=np.float32) / np.float32(128))).astype(np.float32)
    inv64 = (np.float32(10000.0) ** (-np.arange(0, 64, 2, dtype=np.float32) / np.float32(64))).astype(np.float32)
    a128 = (pos[None, :] * inv128[p % 64][:, None]).astype(np.float32)
    a64 = (pos[None, :] * inv64[(p % 64) % 32][:, None]).astype(np.float32)
    tabs[:, 0] = np.cos(a128)
    tabs[:, 1] = np.sin(a128) * np.where(p < 64, -1.0, 1.0)[:, None]
    tabs[:, 2] = np.cos(a64)
    tabs[:, 3] = np.sin(a64) * np.where((p % 64) < 32, -1.0, 1.0)[:, None]
    return masks.astype(ml_dtypes.bfloat16), tabs


def host_inputs(x, w_in, q_norm, w_uq, kv_norm, w_ukv, sinks, branch_norm, w_out, ln_gamma, ln_beta):
    f = lambda a: np.ascontiguousarray(np.asarray(a, dtype=np.float32))
    x, w_in, w_uq, w_ukv, w_out = f(x), f(w_in), f(w_uq), f(w_ukv), f(w_out)
    q_norm, kv_norm, sinks, branch_norm, ln_gamma, ln_beta = f(q_norm), f(kv_norm), f(sinks), f(branch_norm), f(ln_gamma), f(ln_beta)
    cols = np.zeros((128, 84), np.float32)
    for l in range(DEPTH):
        cols[:, 21 * l:21 * l + 3] = q_norm[l].reshape(3, 128).T
        cols[:, 21 * l + 3:21 * l + 5] = kv_norm[l].reshape(2, 128).T
        cols[:, 21 * l + 5:21 * l + 21] = branch_norm[l].reshape(16, 128).T
    sinks_bc = np.ascontiguousarray(np.broadcast_to(sinks.reshape(1, 32), (128, 32)))
    negm = np.zeros((128, 8, 8), np.float32)
    for j in range(8):
        negm[:, j, j:] = NEG
    negm = negm.reshape(128, 64)
    ident = np.eye(128, dtype=np.float32).astype(ml_dtypes.bfloat16)
    tb = [host_tables(r) for r in range(2)]
    in_maps = []
    for c in range(8):
        b, r = c // 2, c % 2
        xo = np.concatenate([x[b, gblk(r, j) * 128:(gblk(r, j) + 1) * 128, :] for j in range(NB)], 0)
        in_maps.append({"x": np.ascontiguousarray(xo), "w_in": w_in, "w_uq": w_uq, "w_ukv": w_ukv, "w_out": w_out,
                        "ln_gamma": ln_gamma, "ln_beta": ln_beta, "cols": cols, "sinks_bc": sinks_bc, "negm": negm,
                        "tabs": tb[r][1], "masks": tb[r][0], "ident": ident})
    return in_maps


def assemble(results, key="out"):
    out = np.zeros((4, 2048, D), np.float32)
    for c in range(8):
        b, r = c // 2, c % 2
        o = np.asarray(results[c][key])
        for j in range(NB):
            g = gblk(r, j)
            out[b, g * 128:(g + 1) * 128, :] = o[j * 128:(j + 1) * 128, :]
    return out


_NC = {}


def kernel(x, w_in, q_norm, w_uq, kv_norm, w_ukv, sinks, branch_norm, w_out, ln_gamma, ln_beta):
    in_maps = host_inputs(x, w_in, q_norm, w_uq, kv_norm, w_ukv, sinks, branch_norm, w_out, ln_gamma, ln_beta)
    if "nc" not in _NC:
        _NC["nc"] = build(DEPTH, False)
    res = run_bass_kernel_spmd(_NC["nc"], in_maps, core_ids=list(range(8)))
    return assemble(res.results)
```

```python
import contextlib
import numpy as np
import ml_dtypes
import concourse.bass as bass
import concourse.mybir as mybir
from concourse.bass_utils import run_bass_kernel_spmd

F32 = mybir.dt.float32
BF16 = mybir.dt.bfloat16
AF = mybir.ActivationFunctionType
ALU = mybir.AluOpType
AX = mybir.AxisListType

D = 2048
NIN = 6592
NOWN = 1024
NB = 8
DEPTH = 4
ALPHA = (2 * DEPTH) ** 0.25
O_AQ, O_AK, O_AV = 0, 512, 1024
O_BCQ, O_BCKV, O_BKR = 1536, 1920, 2176
O_CQ, O_CK, O_CV = 2240, 2752, 3264
O_DQ, O_DK, O_DV = 3776, 4288, 4416
O_G = 4544
NEG = -1e30
PAIRS = [[0, 1], [2, 3], [4, 5], [6, 7]]


def gblk(r, j):
    return 2 * j + (j + r) % 2


def kap(G):
    jj = G // 2
    rr = (G % 2 - jj) % 2
    return 8 * rr + jj


class Sched:
    ENGS = ("sync", "scalar", "vector", "gpsimd", "tensor")

    def __init__(self, nc):
        self.nc = nc
        self.ops = {e: [] for e in self.ENGS}
        self.sems = {}
        self.cnt = {}
        self.mult = {}
        self.waited = {e: {} for e in self.ENGS}
        self._cms = []

    def sem(self, key, mult=1):
        if key not in self.sems:
            cm = self.nc.semaphore(key)
            self.sems[key] = cm.__enter__()
            self._cms.append(cm)
            self.cnt[key] = 0
            self.mult[key] = mult
        return self.sems[key]

    def wait(self, eng, toks):
        for t in toks:
            if t is None:
                continue
            key, n = t
            w = self.waited[eng]
            if w.get(key, 0) >= n:
                continue
            w[key] = n
            h = self.sems[key]
            self.ops[eng].append(lambda e, h=h, v=n * self.mult[key]: e.wait_ge(h, v))

    def op(self, eng, fn, deps=(), sig=True, key=None):
        self.wait(eng, deps)
        if sig:
            key = key or ("c_" + eng)
            h = self.sem(key)
            self.cnt[key] += 1
            self.ops[eng].append(lambda e, fn=fn, h=h: fn(e).then_inc(h, 1))
            return (key, self.cnt[key])
        self.ops[eng].append(lambda e, fn=fn: fn(e))
        return None

    def dma(self, eng, chan, items, deps=()):
        key = "d_" + chan
        h = self.sem(key, 16)
        if self.cnt[key]:
            self.wait(eng, [(key, self.cnt[key])])
        self.wait(eng, deps)
        for (o, i, kw) in items:
            self.cnt[key] += 1
            self.ops[eng].append(lambda e, o=o, i=i, kw=kw, h=h: e.dma_start(out=o, in_=i, **kw).then_inc(h, 16))
        return (key, self.cnt[key])

    def barrier(self):
        toks = [(k, c) for k, c in self.cnt.items() if c > 0]
        for e in self.ENGS:
            self.wait(e, toks)

    def emit(self):
        self.barrier()
        with self.nc.Block() as block:
            @block.sync
            def _(e):
                for f in self.ops["sync"]:
                    f(e)

            @block.scalar
            def _(e):
                for f in self.ops["scalar"]:
                    f(e)

            @block.vector
            def _(e):
                for f in self.ops["vector"]:
                    f(e)

            @block.gpsimd
            def _(e):
                for f in self.ops["gpsimd"]:
                    f(e)

            @block.tensor
            def _(e):
                for f in self.ops["tensor"]:
                    f(e)
        for cm in reversed(self._cms):
            cm.__exit__(None, None, None)


class Reg:
    def __init__(self):
        self.w = None
        self.r = {}

    def rdeps(self):
        return [self.w] if self.w else []

    def wdeps(self):
        return ([self.w] if self.w else []) + list(self.r.values())

    def read(self, tok):
        if tok is None:
            return
        k = tok[0]
        if k not in self.r or self.r[k][1] < tok[1]:
            self.r[k] = tok

    def write(self, tok):
        self.w = tok
        self.r = {}


def build(depth=DEPTH, debug=False):
    nc = bass.Bass("TRN2", target_bir_lowering=False)
    S = Sched(nc)
    dt_in = lambda name, shape, dt=F32: nc.dram_tensor(name, shape, dt, kind="ExternalInput")
    x_in = dt_in("x", [NOWN, D])
    w_in = dt_in("w_in", [DEPTH, D, NIN])
    w_uq = dt_in("w_uq", [DEPTH, 384, 768])
    w_ukv = dt_in("w_ukv", [DEPTH, 256, 1024])
    w_out = dt_in("w_out", [DEPTH, D, D])
    ln_g = dt_in("ln_gamma", [DEPTH, D])
    ln_b = dt_in("ln_beta", [DEPTH, D])
    cols_d = dt_in("cols", [128, 84])
    sinks_d = dt_in("sinks_bc", [128, 32])
    negm_d = dt_in("negm", [128, 64])
    tabs_d = dt_in("tabs", [128, 4, 1024])
    masks_d = dt_in("masks", [128, 44, 128], BF16)
    ident_d = dt_in("ident", [128, 128], BF16)
    out_d = nc.dram_tensor("out", [NOWN, D], F32, kind="ExternalOutput")
    if debug:
        ydbg = nc.dram_tensor("ydbg", [NOWN, D], F32, kind="ExternalOutput")
    xs = [nc.dram_tensor("xs0", [NOWN, D], F32), nc.dram_tensor("xs1", [NOWN, D], F32)]
    ibs = [[nc.dram_tensor(f"ib{l}_{i}", [320 if i == 3 else 1024, 1024], BF16) for i in range(4)] for l in range(depth)]
    obs = [[nc.dram_tensor(f"ob{l}_{i}", [640 if i == 3 else 2048, 1024], BF16) for i in range(4)] for l in range(depth)]

    es = contextlib.ExitStack()
    with es:
        sb = lambda name, shape, dt: es.enter_context(nc.sbuf_tensor("sb_" + name, shape, dt))
        xT = sb("xT", [128, 16, NOWN], BF16)
        yT = sb("yT", [128, 16, NOWN], BF16)
        arena = sb("arena", [128, 32768], BF16)
        wbuf = sb("wbuf", [128, 3, 4096], BF16)
        stage = sb("stage", [128, 4, 512], BF16)
        tabs = sb("tabs", [128, 4, 1024], F32)
        masks = sb("masks", [128, 44, 128], BF16)
        ident = sb("ident", [128, 128], BF16)
        pt = sb("pt", [128, 4, 512], BF16)
        ytile = sb("ytile", [128, 4, 512], F32)
        rt1 = sb("rt1", [128, 512], F32)
        rt2 = sb("rt2", [128, 512], F32)
        cols = sb("cols", [128, 84], F32)
        esink = sb("esink", [128, 32], F32)
        negm = sb("negm", [128, 8, 8], F32)
        sm = sb("sm", [128, 256], F32)
        accC = sb("accC", [128, 4, 132], F32)
        sg = sb("sg", [128, 2, 256], F32)
        ygb = sb("ygb", [128, 2, 256], BF16)
        cqb = sb("cqb", [128, 2, 384], BF16)
        P = es.enter_context(nc.psum_tensor("P", [128, 8, 512], F32))

        R = {}
        def reg(name):
            if name not in R:
                R[name] = Reg()
            return R[name]
        pb = [reg(f"pb{i}") for i in range(4)]
        por = [reg(f"po{i}") for i in range(8)]
        wreg = [reg(f"w{i}") for i in range(3)]
        streg = [reg(f"st{i}") for i in range(4)]
        ptreg = [reg(f"pt{i}") for i in range(4)]

        po8 = P[:, 4:8, :].rearrange("p k (b c) -> p (k b) c", c=256)

        def av(off, n):
            return arena[:, off:off + n]
        kT = av(0, 8192).rearrange("p (h t) -> p h t", h=4)
        Vaug = av(8192, 8256).rearrange("p (k h c) -> p k h c", k=16, h=4)
        VD = av(8192, 2080).rearrange("p (k h c) -> p k h c", k=16, h=2)
        qT = av(16448, 4096).rearrange("p (h t) -> p h t", h=4)
        qrT = av(20544, 4096).rearrange("p (h t) -> p h t", h=4)
        krT = av(24640, 2048)
        cqnT = av(26688, 3072).rearrange("p (k t) -> p k t", k=3)
        kmean = av(29760, 32).rearrange("p (h n) -> p h n", h=4)
        ckvnT = av(29792, 2048).rearrange("p (k t) -> p k t", k=2)
        wo = arena[:, :].rearrange("p (k c) -> p k c", k=16)
        gam = wbuf[:, 0, :].bitcast(F32)
        bet = wbuf[:, 1, :].bitcast(F32)
        xr = wbuf[:, 2, :].bitcast(F32)
        zt = ytile[:, :, :].rearrange("p a b -> p (a b)")
        xb = stage[:, :, :].rearrange("p a b -> p (a b)")

        def OP(eng, fn, rd=(), wr=(), deps=()):
            d = list(deps)
            for t in rd:
                d.extend(t.rdeps())
            for t in wr:
                d.extend(t.wdeps())
            tok = S.op(eng, fn, d)
            for t in rd:
                t.read(tok)
            for t in wr:
                t.write(tok)
            return tok

        def DMA(eng, chan, items, rd=(), wr=(), deps=()):
            d = list(deps)
            for t in rd:
                d.extend(t.rdeps())
            for t in wr:
                d.extend(t.wdeps())
            tok = S.dma(eng, chan, [(o, i, {}) for (o, i) in items], d)
            for t in rd:
                t.read(tok)
            for t in wr:
                t.write(tok)
            return tok

        def MM(calls, rd=(), wr=()):
            d = []
            for t in rd:
                d.extend(t.rdeps())
            for t in wr:
                d.extend(t.wdeps())
            S.wait("tensor", d)
            for f in calls[:-1]:
                S.op("tensor", f, sig=False)
            tok = S.op("tensor", calls[-1])
            for t in rd:
                t.read(tok)
            for t in wr:
                t.write(tok)
            return tok

        def mm(out, lhsT, rhs, start, stop):
            return lambda e: e.matmul(out, lhsT=lhsT, rhs=rhs, start=start, stop=stop)

        creg = reg("const")
        DMA("sync", "const", [(tabs[:], tabs_d[:, :, :]), (masks[:], masks_d[:, :, :]), (ident[:], ident_d[:, :]),
                              (cols[:], cols_d[:, :]), (esink[:], sinks_d[:, :]),
                              (negm[:].rearrange("p a b -> p (a b)"), negm_d[:, :])], wr=[creg])
        OP("scalar", lambda e: e.activation(out=esink[:], in_=esink[:], func=AF.Exp), rd=[creg], wr=[reg("esink")])
        S.barrier()
        cos128, ssin128, cos64, ssin64 = (tabs[:, i, :] for i in range(4))
        TA0 = 0
        MB0 = 34
        MD0 = 38

        wstate = {"i": 0}

        def wload(items, name):
            s = wstate["i"] % 3
            wstate["i"] += 1
            DMA("gpsimd", f"w{s}", items(s), wr=[wreg[s]])
            return s

        def wv(s):
            return wbuf[:, s, :].rearrange("p (k c) -> p k c", c=256)

        def std_items(l, off, n):
            return lambda s: [(wv(s)[:, :, 0:n], w_in[l, :, off:off + n].rearrange("(k p) c -> p k c", p=128))]

        class WQ:
            def __init__(self, specs):
                self.specs = specs
                self.issued = 0
                self.slots = []
                self.pos = 0

            def _issue(self):
                name, items = self.specs[self.issued]
                self.slots.append(wload(items, name))
                self.issued += 1

            def next(self, name, la=3):
                assert self.specs[self.pos][0] == name, (self.specs[self.pos][0], name)
                while self.issued < min(len(self.specs), self.pos + la):
                    self._issue()
                s = self.slots[self.pos]
                self.pos += 1
                return s

            def prefetch(self):
                while self.issued < min(len(self.specs), self.pos + 3):
                    self._issue()

        def layer_specs(l):
            sp = []
            for nm, off in (("Ak", O_AK), ("Av", O_AV), ("Ck", O_CK), ("Cv", O_CV)):
                for g in range(2):
                    sp.append((f"{nm}{g}", std_items(l, off + 256 * g, 256)))
            sp.append(("Bkv", std_items(l, O_BCKV, 256)))
            sp.append(("Bkr", std_items(l, O_BKR, 64)))
            sp.append(("WUKV", lambda s: [(wbuf[:, s, 0:2048].rearrange("p (k c) -> p k c", k=2),
                                           w_ukv[l, :, :].rearrange("(k p) c -> p k c", p=128))]))
            sp.append(("Dkv", std_items(l, O_DK, 256)))

            def gates(m):
                r = []
                for T in range(2):
                    for g in range(2):
                        r.append((f"G{m}{g}", std_items(l, O_G + 512 * m + 256 * g, 256)))
                return r
            sp += [("Aq0", std_items(l, O_AQ, 256)), ("Aq1", std_items(l, O_AQ + 256, 256))] + gates(0)
            sp += [("Cq0", std_items(l, O_CQ, 256)), ("Cq1", std_items(l, O_CQ + 256, 256))] + gates(2)
            sp += [("Bcq0", std_items(l, O_BCQ, 256)), ("Bcq1", std_items(l, O_BCQ + 256, 128)),
                   ("WUQ", lambda s: [(wbuf[:, s, 0:2304].rearrange("p (k c) -> p k c", k=3),
                                       w_uq[l, :, :].rearrange("(k p) c -> p k c", p=128))])] + gates(1)

            def dq_items(cp):
                def f(s):
                    it = []
                    for i in range(2):
                        c = 2 * cp + i
                        it.append((wv(s)[:, :, 128 * i:128 * i + 64],
                                   w_in[l, :, O_DQ + 64 * c:O_DQ + 64 * c + 64].rearrange("(k p) c -> p k c", p=128)))
                        it.append((wv(s)[:, :, 128 * i + 64:128 * i + 128],
                                   w_in[l, :, O_DQ + 64 * (4 + c):O_DQ + 64 * (4 + c) + 64].rearrange("(k p) c -> p k c", p=128)))
                    return it
                return f
            sp += [("Dq0", dq_items(0)), ("Dq1", dq_items(1))] + gates(3)
            return sp

        ctr = {"pp": 0, "ps": 0, "pt": 0, "st": 0, "po": 0, "sgq": 0, "po8": 0}

        def nxt(k, n):
            v = ctr[k] % n
            ctr[k] += 1
            return v

        def pipeline(items, LA):
            n = len(items)
            for i in range(n + LA):
                if i < n:
                    items[i][0]()
                if i >= LA:
                    items[i - LA][1]()

        xTreg = reg("xT")

        def proj_fm(s, c0, ncol, T):
            b = nxt("pp", 2)
            calls = [mm(P[0:ncol, b, :], wv(s)[:, k, c0:c0 + ncol], xT[:, k, T * 512:(T + 1) * 512], k == 0, k == 15)
                     for k in range(16)]
            MM(calls, rd=[wreg[s], xTreg], wr=[pb[b]])
            return b

        def proj_tm(s, j, ncol, b=None):
            if b is None:
                b = nxt("pp", 2)
            calls = [mm(P[:, b, 0:ncol], xT[:, k, j * 128:(j + 1) * 128], wv(s)[:, k, 0:ncol], k == 0, k == 15)
                     for k in range(16)]
            MM(calls, rd=[wreg[s], xTreg], wr=[pb[b]])
            return b

        rtreg = reg("rt")

        def rope(src_bank, np_, dst, dstregs, half, T, n=512):
            ct, stb = (cos128, ssin128) if half == 64 else (cos64, ssin64)
            src = P[:, src_bank, 0:n]
            tsl = slice(T * 512, T * 512 + n)
            OP("vector", lambda e: e.tensor_tensor(out=rt1[0:np_, 0:n], in0=src[0:np_, :], in1=ct[0:np_, tsl], op=ALU.mult),
               rd=[pb[src_bank]], wr=[rtreg])
            for base in range(0, np_, 2 * half):
                lo, mid, hi = base, base + half, base + 2 * half
                OP("vector", lambda e, lo=lo, mid=mid, hi=hi: e.tensor_tensor(
                    out=rt2[lo:mid, 0:n], in0=src[mid:hi, :], in1=stb[lo:mid, tsl], op=ALU.mult), rd=[pb[src_bank]], wr=[rtreg])
                OP("vector", lambda e, lo=lo, mid=mid, hi=hi: e.tensor_tensor(
                    out=rt2[mid:hi, 0:n], in0=src[lo:mid, :], in1=stb[mid:hi, tsl], op=ALU.mult), rd=[pb[src_bank]], wr=[rtreg])
            OP("vector", lambda e: e.tensor_tensor(out=dst, in0=rt1[0:np_, 0:n], in1=rt2[0:np_, 0:n], op=ALU.add),
               rd=[rtreg], wr=dstregs)

        def transposes_to(dst_fn, src_fn, nchunk, srcregs, dstregs, scale_fn=None, np_out=128):
            c = 0
            while c < nchunk:
                g = min(8, nchunk - c)
                b = nxt("pp", 2)
                pbv = P[:, b, :].bitcast(BF16)
                calls = []
                for i in range(g):
                    calls.append((lambda e, o=pbv[:, i * 128:(i + 1) * 128], s_=src_fn(c + i): e.transpose(o, s_, ident[:])))
                MM(calls, rd=srcregs + [reg("const")], wr=[pb[b]])
                for i in range(g):
                    d_ = dst_fn(c + i)
                    i_ = pbv[0:np_out, i * 128:(i + 1) * 128]
                    if scale_fn is None:
                        OP("scalar", lambda e, d_=d_, i_=i_: e.activation(out=d_, in_=i_, func=AF.Copy), rd=[pb[b]], wr=dstregs)
                    else:
                        OP("scalar", lambda e, d_=d_, i_=i_, sc=scale_fn(c + i): e.activation(out=d_, in_=i_, func=AF.Copy, scale=sc),
                           rd=[pb[b]], wr=dstregs)
                c += g

        def block_to_xT(src_f32, srcreg, j):
            if src_f32 is not None:
                OP("scalar", lambda e: e.activation(out=xb, in_=src_f32, func=AF.Copy), rd=[srcreg], wr=streg)
            for g in range(2):
                bk = nxt("pp", 2)
                pbv = P[:, bk, :].bitcast(BF16)
                MM([(lambda e, o=pbv[:, i * 128:(i + 1) * 128], s_=xb[:, (g * 8 + i) * 128:(g * 8 + i + 1) * 128]: e.transpose(o, s_, ident[:]))
                    for i in range(8)], rd=list(streg) + [reg("const")], wr=[pb[bk]])
                dst = xT[:, g * 8:(g + 1) * 8, j * 128:(j + 1) * 128]
                src = pbv[:, :].rearrange("p (c i) -> p c i", c=8)
                if g == 0:
                    OP("scalar", lambda e, dst=dst, src=src: e.activation(out=dst, in_=src, func=AF.Copy), rd=[pb[bk]], wr=[xTreg])
                else:
                    OP("vector", lambda e, dst=dst, src=src: e.tensor_copy(out=dst, in_=src), rd=[pb[bk]], wr=[xTreg])

        def stage_out(src_ap_fn, srcregs, dst_dram, eng="scalar"):
            s = nxt("st", 4)
            src, shape_p, shape_n = src_ap_fn
            OP(eng, lambda e: e.activation(out=stage[0:shape_p, s, 0:shape_n], in_=src, func=AF.Copy) if eng == "scalar"
               else e.tensor_copy(out=stage[0:shape_p, s, 0:shape_n], in_=src), rd=srcregs, wr=[streg[s]])
            DMA("sync", f"st{s}", [(dst_dram, stage[0:shape_p, s, 0:shape_n])], rd=[streg[s]])

        def rmsnorm_rows(bank, ncol, dst_bf, dstregs, eps=1e-6):
            r = reg("sm")
            OP("vector", lambda e: e.memset(sm[:, 0:1], 0.0), wr=[r])
            OP("scalar", lambda e: e.activation(out=rt1[:, 0:ncol], in_=P[:, bank, 0:ncol], func=AF.Square, accum_out=sm[:, 0:1]),
               rd=[pb[bank]], wr=[r, rtreg])
            OP("vector", lambda e: e.tensor_scalar(out=sm[:, 1:2], in0=sm[:, 0:1], scalar1=1.0 / ncol, scalar2=eps, op0=ALU.mult, op1=ALU.add),
               rd=[r], wr=[r])
            OP("scalar", lambda e: e.activation(out=sm[:, 2:3], in_=sm[:, 1:2], func=AF.Ln), rd=[r], wr=[r])
            OP("scalar", lambda e: e.activation(out=sm[:, 3:4], in_=sm[:, 2:3], func=AF.Exp, scale=-0.5), rd=[r], wr=[r])
            OP("scalar", lambda e: e.activation(out=dst_bf, in_=P[:, bank, 0:ncol], func=AF.Copy, scale=sm[:, 3:4]),
               rd=[r, pb[bank]], wr=dstregs)

        xrreg = wreg[2]
        for j in range(NB):
            DMA("sync", "xr", [(xr, x_in[j * 128:(j + 1) * 128, :])], wr=[xrreg])
            block_to_xT(xr, xrreg, j)
        S.barrier()

        for l in range(depth):
            WS = WQ(layer_specs(l))
            ib, ob = ibs[l], obs[l]
            ccoll = [None] * 4
            areg = {n: reg(f"a_{n}") for n in ("kT", "V", "qT", "qrT", "krT", "cqnT", "kmean", "ckvnT")}

            v1reg = Reg()
            OP("vector", lambda e: e.memset(Vaug[:, :, :, 128:129], 1.0), wr=[v1reg])

            def collective(i, deps):
                S.wait("gpsimd", deps)
                key = f"cc{i}"
                S.sem(key)
                tok = S.op("gpsimd", lambda e, ii=ib[i], oo=ob[i]: e.collective_compute(
                    "AllGather", ALU.bypass, replica_groups=PAIRS, ins=[ii.ap().opt()], outs=[oo.ap().opt()]), key=key)
                ccoll[i] = tok

            def load_ctx_AC(ci, wait_ones=True):
                items = []
                for rr in range(2):
                    items.append((kT[:, :, rr * 1024:(rr + 1) * 1024],
                                  ob[ci][rr * 1024:rr * 1024 + 512, :].rearrange("(h d) t -> d h t", d=128)))
                DMA("sync", "ctxk", items, wr=[areg["kT"]], deps=[ccoll[ci]])
                items = []
                for rr in range(2):
                    src = ob[ci][rr * 1024 + 512:rr * 1024 + 1024, :].rearrange("r (two c) -> (r two) c", two=2)
                    for h in range(4):
                        items.append((Vaug[:, rr * 8:(rr + 1) * 8, h, 0:128],
                                      src[:, h * 128:(h + 1) * 128].rearrange("(jj p) c -> p jj c", p=128)))
                DMA("sync", "ctxv", items, wr=[areg["V"]], deps=[ccoll[ci]])

            def kside_AC(nm, ci):
                toks = []
                for g in range(2):
                    s = WS.next(f"{nm}k{g}")
                    for hh in range(2):
                        h = 2 * g + hh
                        for T in range(2):
                            b = proj_fm(s, hh * 128, 128, T)
                            st = nxt("st", 4)
                            rope(b, 128, stage[:, st, :], [streg[st]], 64, T)
                            toks.append(DMA("sync", f"st{st}", [(ib[ci][h * 128:(h + 1) * 128, T * 512:(T + 1) * 512], stage[:, st, :])],
                                            rd=[streg[st]]))
                vview = ib[ci][512:1024, :].rearrange("r (two c) -> (r two) c", two=2)
                for g in range(2):
                    s = WS.next(f"{nm}v{g}")
                    for j in range(NB):
                        b = proj_tm(s, j, 256)
                        st = nxt("st", 4)
                        OP("scalar", lambda e, b=b, st=st: e.activation(out=stage[:, st, 0:256], in_=P[:, b, 0:256], func=AF.Copy),
                           rd=[pb[b]], wr=[streg[st]])
                        toks.append(DMA("sync", f"st{st}", [(vview[j * 128:(j + 1) * 128, g * 256:(g + 1) * 256], stage[:, st, 0:256])],
                                        rd=[streg[st]]))
                collective(ci, toks)

            kside_AC("A", 0)
            kside_AC("C", 1)
            load_ctx_AC(0)

            toksB, toksD = [], []
            s = WS.next("Bkv")
            its = []
            for j in range(NB):
                def s1(j=j, s=s):
                    b = proj_tm(s, j, 256, b=2 + j % 2)
                    rmsnorm_rows(b, 256, cqb[:, j % 2, 0:256], [reg(f"cqb{j % 2}")])

                def s2(j=j):
                    transposes_to(lambda c: ckvnT[:, c, j * 128:(j + 1) * 128], lambda c: cqb[:, j % 2, c * 128:(c + 1) * 128], 2,
                                  [reg(f"cqb{j % 2}")], [areg["ckvnT"]], scale_fn=lambda c: cols[:, 21 * l + 3 + c:21 * l + 4 + c])
                its.append((s1, s2))
            pipeline(its, 1)
            s = WS.next("Bkr")
            for T in range(2):
                b = proj_fm(s, 0, 64, T)
                st = nxt("st", 4)
                rope(b, 64, stage[0:64, st, :], [streg[st]], 32, T)
                toksD.append(DMA("sync", f"st{st}", [(ib[3][0:64, T * 512:(T + 1) * 512], stage[0:64, st, :])], rd=[streg[st]]))
            s = WS.next("WUKV")
            wkv = wbuf[:, s, 0:2048].rearrange("p (k h c) -> p k h c", k=2, h=4)
            for h in range(4):
                for T in range(2):
                    b = nxt("pp", 2)
                    MM([mm(P[:, b, :], wkv[:, k, h, 0:128], ckvnT[:, k, T * 512:(T + 1) * 512], k == 0, k == 1) for k in range(2)],
                       rd=[wreg[s], areg["ckvnT"]], wr=[pb[b]])
                    st = nxt("st", 4)
                    OP("scalar", lambda e, b=b, st=st: e.activation(out=stage[:, st, :], in_=P[:, b, :], func=AF.Copy),
                       rd=[pb[b]], wr=[streg[st]])
                    toksB.append(DMA("sync", f"st{st}", [(ib[2][h * 128:(h + 1) * 128, T * 512:(T + 1) * 512], stage[:, st, :])],
                                     rd=[streg[st]]))
            vviewB = ib[2][512:1024, :].rearrange("r (two c) -> (r two) c", two=2)
            for j in range(NB):
                b = nxt("pp", 2)
                MM([mm(P[:, b, :].rearrange("p (h c) -> p h c", h=4), ckvnT[:, k, j * 128:(j + 1) * 128], wkv[:, k, :, 128:256], k == 0, k == 1)
                    for k in range(2)], rd=[wreg[s], areg["ckvnT"]], wr=[pb[b]])
                st = nxt("st", 4)
                OP("scalar", lambda e, b=b, st=st: e.activation(out=stage[:, st, :], in_=P[:, b, :], func=AF.Copy),
                   rd=[pb[b]], wr=[streg[st]])
                toksB.append(DMA("sync", f"st{st}", [(vviewB[j * 128:(j + 1) * 128, :], stage[:, st, :])], rd=[streg[st]]))
            collective(2, toksB)

            s = WS.next("Dkv")
            for T in range(2):
                b = proj_fm(s, 0, 128, T)
                st = nxt("st", 4)
                rope(b, 128, stage[:, st, :], [streg[st]], 32, T)
                toksD.append(DMA("sync", f"st{st}", [(ib[3][64:192, T * 512:(T + 1) * 512], stage[:, st, :])], rd=[streg[st]]))
            vviewD = ib[3][192:320, :].rearrange("r (e c) -> (r e) c", e=8)
            for j in range(NB):
                b = nxt("pp", 2)
                MM([mm(P[:, b, 0:128], xT[:, k, j * 128:(j + 1) * 128], wv(s)[:, k, 128:256], k == 0, k == 15) for k in range(16)],
                   rd=[wreg[s], xTreg], wr=[pb[b]])
                st = nxt("st", 4)
                OP("scalar", lambda e, b=b, st=st: e.activation(out=stage[:, st, 0:128], in_=P[:, b, 0:128], func=AF.Copy),
                   rd=[pb[b]], wr=[streg[st]])
                toksD.append(DMA("sync", f"st{st}", [(vviewD[j * 128:(j + 1) * 128, :], stage[:, st, 0:128])], rd=[streg[st]]))
            collective(3, toksD)

            def qproj_AC(nm):
                for g in range(2):
                    s = WS.next(f"{nm}q{g}")
                    for hh in range(2):
                        h = 2 * g + hh
                        for T in range(2):
                            b = proj_fm(s, hh * 128, 128, T)
                            rope(b, 128, qT[:, h, T * 512:(T + 1) * 512], [areg["qT"]], 64, T)

            smr = {k: reg("sm_" + k) for k in ("norm", "epi", "sel0", "sel1", "D", "km", "ssq")}
            ssq = sm[:, 208:240].rearrange("p (b h) -> p b h", b=4)
            yreg = reg("ytile")
            SB = (2, 3, 0, 1)

            def sumsq(b, col, lo, hi):
                OP("vector", lambda e: e.scalar_tensor_tensor(out=rt1[:, 0:hi - lo], in0=ytile[:, b, lo:hi], scalar=1.0, in1=ytile[:, b, lo:hi],
                                                              op0=ALU.mult, op1=ALU.mult, accum_out=ssq[:, b, col:col + 1]),
                   rd=[yreg], wr=[rtreg, smr["ssq"]])

            def normalize_po(slots, h, src4=None, srcregs=None):
                r = smr["norm"]
                if src4 is None:
                    den = po8[:, slots[0]:slots[0] + 4, 128:129]
                    rg = [por[s_] for s_ in slots]
                else:
                    den = src4[:, :, 128:129]
                    rg = srcregs
                OP("vector", lambda e: e.reciprocal(out=sm[:, 8:12].rearrange("p (b o) -> p b o", o=1), in_=den), rd=rg, wr=[r])
                for b in range(4):
                    src = po8[:, slots[b], :] if src4 is None else src4[:, b, :]
                    OP("vector", lambda e, src=src, b=b: e.tensor_scalar(out=ytile[:, b, h * 128:(h + 1) * 128], in0=src[:, 0:128],
                                                                       scalar1=sm[:, 8 + b:9 + b], scalar2=None, op0=ALU.mult),
                       rd=rg + [r], wr=[yreg])
                    sumsq(b, h, h * 128, (h + 1) * 128)

            def scores(T, h, G, isB, scale):
                kp = kap(G)
                sbk = SB[nxt("ps", 4)]
                calls = [mm(P[:, sbk, :], kT[:, h, kp * 128:(kp + 1) * 128], qT[:, h, T * 512:(T + 1) * 512], True, not isB)]
                rd = [areg["kT"], areg["qT"]]
                if isB:
                    calls.append(mm(P[:, sbk, :], krT[:, kp * 128:(kp + 1) * 128], qrT[:, h, T * 512:(T + 1) * 512], False, True))
                    rd += [areg["krT"], areg["qrT"]]
                MM(calls, rd=rd, wr=[pb[sbk]])
                ps_ = nxt("pt", 4)
                OP("scalar", lambda e: e.activation(out=pt[:, ps_, :], in_=P[:, sbk, :], func=AF.Exp, scale=scale),
                   rd=[pb[sbk]], wr=[ptreg[ps_]])
                return ps_, kp

            def mask128(ps_, b, slot):
                OP("vector", lambda e: e.tensor_tensor(out=pt[:, ps_, b * 128:(b + 1) * 128], in0=pt[:, ps_, b * 128:(b + 1) * 128],
                                                        in1=masks[:, slot, :], op=ALU.mult), rd=[ptreg[ps_], creg], wr=[ptreg[ps_]])

            def maskA(ps_, T, G):
                j0 = 4 * T
                t0 = 2 * j0 + 1 - G
                bmin = 0
                while t0 + 2 * bmin < 0:
                    bmin += 1
                slot = lambda b: TA0 + (t0 + 2 * b) * 2 + (b % 2)
                if bmin == 0:
                    mk = bass.AP(masks, slot(0) * 128, [[44 * 128, 128], [8 * 128, 2], [5 * 128, 2], [1, 128]])
                    ptv = pt[:, ps_, :].rearrange("p (u v i) -> p u v i", u=2, v=2)
                    OP("vector", lambda e: e.tensor_tensor(out=ptv, in0=ptv, in1=mk, op=ALU.mult), rd=[ptreg[ps_], creg], wr=[ptreg[ps_]])
                    return
                if bmin == 1:
                    mask128(ps_, 1, slot(1))
                if bmin <= 2:
                    mk = bass.AP(masks, slot(2) * 128, [[44 * 128, 128], [5 * 128, 2], [1, 128]])
                    ptv = pt[:, ps_, 256:512].rearrange("p (v i) -> p v i", v=2)
                    OP("vector", lambda e: e.tensor_tensor(out=ptv, in0=ptv, in1=mk, op=ALU.mult), rd=[ptreg[ps_], creg], wr=[ptreg[ps_]])
                else:
                    mask128(ps_, 3, slot(3))

            def epilogue(m, T):
                r = smr["epi"]
                if debug:
                    for b in range(4):
                        DMA("sync", "dbg", [(ydbg[(4 * T + b) * 128:(4 * T + b + 1) * 128, m * 512:(m + 1) * 512], ytile[:, b, :])], rd=[yreg])
                nh = 8 if m == 3 else 4
                OP("vector", lambda e: e.tensor_reduce(out=sm[:, 16:20], in_=ssq[:, :, 0:nh], op=ALU.add, axis=AX.X), rd=[smr["ssq"]], wr=[r])
                OP("vector", lambda e: e.tensor_scalar(out=sm[:, 20:24], in0=sm[:, 16:20], scalar1=1.0 / 512, scalar2=1e-6, op0=ALU.mult, op1=ALU.add),
                   rd=[r], wr=[r])
                OP("scalar", lambda e: e.activation(out=sm[:, 24:28], in_=sm[:, 20:24], func=AF.Ln), rd=[r], wr=[r])
                OP("scalar", lambda e: e.activation(out=sm[:, 28:32], in_=sm[:, 24:28], func=AF.Exp, scale=-0.5), rd=[r], wr=[r])
                its = []
                slots_g = [WS.next(f"G{m}{g}", la=3 - g) for g in range(2)]
                for g in range(2):
                    s = slots_g[g]
                    for b in range(4):
                        q = nxt("sgq", 2)
                        j = 4 * T + b

                        def sA(s=s, b=b, g=g, q=q, j=j):
                            bk = proj_tm(s, j, 256)
                            OP("scalar", lambda e: e.activation(out=sg[:, q, :], in_=P[:, bk, 0:256], func=AF.Silu),
                               rd=[pb[bk]], wr=[reg(f"sg{q}")])
                            OP("vector", lambda e: e.scalar_tensor_tensor(
                                out=ygb[:, q, :], in0=ytile[:, b, g * 256:(g + 1) * 256], scalar=sm[:, 28 + b:29 + b], in1=sg[:, q, :],
                                op0=ALU.mult, op1=ALU.mult), rd=[yreg, r, reg(f"sg{q}")], wr=[reg(f"yg{q}")])

                        def sB(g=g, q=q, j=j):
                            c0 = m * 4 + g * 2
                            transposes_to(lambda c: yT[:, c0 + c, j * 128:(j + 1) * 128],
                                          lambda c: ygb[:, q, c * 128:(c + 1) * 128], 2, [reg(f"yg{q}")], [reg("yT")],
                                          scale_fn=lambda c: cols[:, 21 * l + 5 + c0 + c:21 * l + 6 + c0 + c])
                        its.append((sA, sB))
                pipeline(its, 1)

            def dense_stream(m, isA, isB, scale):
                items = []
                for T in range(2):
                    j0 = 4 * T
                    nG = 8 * T + 8
                    for h in range(4):
                        st = {}
                        for G in range(nG):
                            d = {}

                            def s1(T=T, h=h, G=G, d=d, st=st, j0=j0):
                                if G == 0:
                                    ob_ = nxt("po", 2)
                                    st["slots"] = [ob_ * 4 + b for b in range(4)]
                                d["ps"], d["kp"] = scores(T, h, G, isB, scale)
                                if isA:
                                    maskA(d["ps"], T, G)
                                else:
                                    for b in range(4):
                                        j = j0 + b
                                        if 2 * j <= G <= 2 * j + 1:
                                            mask128(d["ps"], b, MB0 + (j % 2) * 2 + (G - 2 * j))

                            def s2(T=T, h=h, G=G, d=d, st=st, j0=j0, nG=nG):
                                slots = st["slots"]
                                calls, wr = [], []
                                for b in range(4):
                                    j = j0 + b
                                    if G <= 2 * j + 1:
                                        calls.append(lambda e, b=b, j=j: e.matmul(
                                            po8[:, slots[b], 0:129], lhsT=pt[:, d["ps"], b * 128:(b + 1) * 128], rhs=Vaug[:, d["kp"], h, :],
                                            start=(G == 0 and b % 2 == 0), stop=(G == 2 * j + 1), skip_group_check=True))
                                        wr.append(por[slots[b]])
                                MM(calls, rd=[ptreg[d["ps"]], areg["V"], v1reg], wr=wr)
                                if G == nG - 1:
                                    normalize_po(slots, h)
                                    if h == 3:
                                        epilogue(m, T)
                            items.append((s1, s2))
                pipeline(items, 3)

            def zero_ssq():
                OP("vector", lambda e: e.memset(sm[:, 208:240], 0.0), wr=[smr["ssq"]])

            qproj_AC("A")
            zero_ssq()
            dense_stream(0, True, False, 128 ** -0.5)

            load_ctx_AC(1)
            zero_ssq()
            r = smr["km"]
            for h in range(4):
                OP("vector", lambda e, h=h: e.tensor_reduce(out=sm[:, 32:48], in_=kT[:, h, :].rearrange("p (k t) -> p k t", k=16),
                                                            op=ALU.add, axis=AX.X), rd=[areg["kT"]], wr=[r])
                OP("vector", lambda e: e.tensor_tensor(out=sm[:, 48:56], in0=sm[:, 32:40], in1=sm[:, 40:48], op=ALU.add), rd=[r], wr=[r])
                OP("vector", lambda e, h=h: e.tensor_scalar(out=kmean[:, h, :], in0=sm[:, 48:56], scalar1=1.0 / 256, scalar2=None, op0=ALU.mult),
                   rd=[r], wr=[areg["kmean"]])
            qproj_AC("C")
            selbuf = [sm[:, 64:96].rearrange("p (b n) -> p b n", b=4), sm[:, 144:176].rearrange("p (b n) -> p b n", b=4)]
            gmv = sm[:, 96:128].rearrange("p (b n) -> p b n", b=4)
            top = sm[:, 176:208].rearrange("p (b n) -> p b n", b=4)
            gmr = reg("sm_gm")
            accr = reg("accC")
            items = []
            grp = 0
            for T in range(2):
                j0 = 4 * T
                for h in range(4):
                    st = {"first": [True] * 4}
                    sel = selbuf[grp % 2]
                    selr = smr[f"sel{grp % 2}"]
                    grp += 1
                    nN = j0 + 4
                    for n in range(nN):
                        d = {}

                        def s1(T=T, h=h, n=n, d=d, j0=j0, sel=sel, selr=selr):
                            if n == 0:
                                bk = nxt("pp", 2)
                                MM([mm(P[:, bk, b * 8:(b + 1) * 8], qT[:, h, (j0 + b) * 128:(j0 + b + 1) * 128], kmean[:, h, :], True, True)
                                    for b in range(4)], rd=[areg["qT"], areg["kmean"]], wr=[pb[bk]])
                                OP("vector", lambda e: e.tensor_tensor(out=gmv, in0=P[:, bk, 0:32].rearrange("p (b n) -> p b n", b=4),
                                                                       in1=negm[:, j0:j0 + 4, :], op=ALU.add), rd=[pb[bk], creg], wr=[gmr])
                                for b in range(4):
                                    OP("vector", lambda e, b=b: e.max(out=top[:, b, :], in_=gmv[:, b, :]), rd=[gmr], wr=[gmr])
                                for b in range(4):
                                    OP("vector", lambda e, b=b: e.tensor_scalar(out=sel[:, b, :], in0=gmv[:, b, :], scalar1=top[:, b, 2:3], scalar2=None,
                                                                                op0=ALU.is_ge), rd=[gmr], wr=[selr])
                            d["pss"] = []
                            for w in range(2):
                                ps_, kp = scores(T, h, 2 * n + w, False, 128 ** -0.5)
                                for b in range(4):
                                    if j0 + b == n:
                                        mask128(ps_, b, MB0 + (n % 2) * 2 + w)
                                d["pss"].append((ps_, kp))

                        def s2(T=T, h=h, n=n, d=d, j0=j0, sel=sel, selr=selr, st=st, nN=nN):
                            ob_ = nxt("po", 2)
                            slots = [ob_ * 4 + b for b in range(4)]
                            pss = d["pss"]
                            calls, wr = [], []
                            for b in range(4):
                                if j0 + b >= n:
                                    for w in range(2):
                                        ps_, kp = pss[w]
                                        calls.append(mm(po8[:, slots[b], 0:129], pt[:, ps_, b * 128:(b + 1) * 128], Vaug[:, kp, h, :], w == 0, w == 1))
                                    wr.append(por[slots[b]])
                            MM(calls, rd=[ptreg[pss[0][0]], ptreg[pss[1][0]], areg["V"], v1reg], wr=wr)
                            first = st["first"]
                            for b in range(4):
                                j = j0 + b
                                if j < n:
                                    continue
                                src = po8[:, slots[b], 0:129]
                                dst = accC[:, b, 0:129]
                                if j == n:
                                    if first[b]:
                                        OP("vector", lambda e, src=src, dst=dst: e.tensor_copy(out=dst, in_=src), rd=[por[slots[b]]], wr=[accr])
                                    else:
                                        OP("vector", lambda e, src=src, dst=dst: e.tensor_tensor(out=dst, in0=src, in1=dst, op=ALU.add),
                                           rd=[por[slots[b]]], wr=[accr])
                                else:
                                    if first[b]:
                                        OP("vector", lambda e, src=src, dst=dst, b=b: e.tensor_scalar(
                                            out=dst, in0=src, scalar1=sel[:, b, n:n + 1], scalar2=None, op0=ALU.mult),
                                           rd=[por[slots[b]], selr], wr=[accr])
                                    else:
                                        OP("vector", lambda e, src=src, dst=dst, b=b: e.scalar_tensor_tensor(
                                            out=dst, in0=src, scalar=sel[:, b, n:n + 1], in1=dst, op0=ALU.mult, op1=ALU.add),
                                           rd=[por[slots[b]], selr], wr=[accr])
                                first[b] = False
                            if n == nN - 1:
                                normalize_po(None, h, src4=accC, srcregs=[accr])
                                if h == 3:
                                    epilogue(2, T)
                        items.append((s1, s2))
            pipeline(items, 1)

            load_ctx_AC(2)
            DMA("sync", "ctxr", [(krT[0:64, rr * 1024:(rr + 1) * 1024], ob[3][rr * 320:rr * 320 + 64, :]) for rr in range(2)],
                wr=[areg["krT"]], deps=[ccoll[3]])
            OP("gpsimd", lambda e: e.memset(krT[64:128, :], 0.0), wr=[areg["krT"]])
            OP("gpsimd", lambda e: e.memset(qrT[64:128, :, :], 0.0), wr=[areg["qrT"]])
            s0 = WS.next("Bcq0")
            s1_ = WS.next("Bcq1", la=2)
            its = []
            for j in range(NB):
                def s1(j=j):
                    bk = 2 + j % 2
                    calls = [mm(P[:, bk, 0:256], xT[:, k, j * 128:(j + 1) * 128], wv(s0)[:, k, 0:256], k == 0, k == 15) for k in range(16)]
                    calls += [mm(P[:, bk, 256:384], xT[:, k, j * 128:(j + 1) * 128], wv(s1_)[:, k, 0:128], k == 0, k == 15) for k in range(16)]
                    MM(calls, rd=[wreg[s0], wreg[s1_], xTreg], wr=[pb[bk]])
                    rmsnorm_rows(bk, 384, cqb[:, j % 2, 0:384], [reg(f"cqb{j % 2}")])

                def s2(j=j):
                    transposes_to(lambda c: cqnT[:, c, j * 128:(j + 1) * 128], lambda c: cqb[:, j % 2, c * 128:(c + 1) * 128], 3,
                                  [reg(f"cqb{j % 2}")], [areg["cqnT"]], scale_fn=lambda c: cols[:, 21 * l + c:21 * l + 1 + c])
                its.append((s1, s2))
            pipeline(its, 1)
            s = WS.next("WUQ")
            wq = wbuf[:, s, 0:2304].rearrange("p (k h c) -> p k h c", k=3, h=4)
            for h in range(4):
                for T in range(2):
                    bk = nxt("pp", 2)
                    MM([mm(P[:, bk, :], wq[:, k, h, 0:128], cqnT[:, k, T * 512:(T + 1) * 512], k == 0, k == 2) for k in range(3)],
                       rd=[wreg[s], areg["cqnT"]], wr=[pb[bk]])
                    OP("scalar", lambda e, bk=bk, h=h, T=T: e.activation(out=qT[:, h, T * 512:(T + 1) * 512], in_=P[:, bk, :], func=AF.Copy),
                       rd=[pb[bk]], wr=[areg["qT"]])
                    bk = nxt("pp", 2)
                    MM([mm(P[0:64, bk, :], wq[:, k, h, 128:192], cqnT[:, k, T * 512:(T + 1) * 512], k == 0, k == 2) for k in range(3)],
                       rd=[wreg[s], areg["cqnT"]], wr=[pb[bk]])
                    rope(bk, 64, qrT[0:64, h, T * 512:(T + 1) * 512], [areg["qrT"]], 32, T)
            zero_ssq()
            dense_stream(1, False, True, 192 ** -0.5)

            tokB = list(v1reg.wdeps())
            for rg in areg.values():
                tokB.extend(rg.wdeps())
            dreg = {n: Reg() for n in ("kT", "V", "qT")}
            for rg in dreg.values():
                for t in tokB:
                    rg.read(t)
            kTD = av(24512, 2048)
            VD = av(26560, 2080).rearrange("p (k h c) -> p k h c", k=16, h=2)
            qTD = av(28640, 4096).rearrange("p (h t) -> p h t", h=4)
            items = [(kTD[:, rr * 1024:(rr + 1) * 1024], ob[3][rr * 320 + 64:rr * 320 + 192, :]) for rr in range(2)]
            DMA("sync", "ctxk", items, wr=[dreg["kT"]], deps=[ccoll[3]])
            items = []
            for rr in range(2):
                src = ob[3][rr * 320 + 192:rr * 320 + 320, :].rearrange("r (e c) -> (r e) c", e=8)
                for hh in range(2):
                    items.append((VD[:, rr * 8:(rr + 1) * 8, hh, 0:64], src[:, hh * 64:(hh + 1) * 64].rearrange("(jj p) c -> p jj c", p=128)))
            DMA("sync", "ctxv", items, wr=[dreg["V"]], deps=[ccoll[3]])
            d1reg = Reg()
            for t in tokB:
                d1reg.read(t)
            OP("vector", lambda e: e.memset(VD[:, :, :, 64:65], 1.0), wr=[d1reg])
            for g in range(2):
                s = WS.next(f"Dq{g}")
                for cc in range(2):
                    c = 2 * g + cc
                    for T in range(2):
                        bk = proj_fm(s, cc * 128, 128, T)
                        rope(bk, 128, qTD[:, c, T * 512:(T + 1) * 512], [dreg["qT"]], 32, T)
            WS.prefetch()
            DMA("gpsimd", "wo", [(wo[:, 0:11, q * 512:(q + 1) * 512],
                                  w_out[l, 0:11 * 128, q * 512:(q + 1) * 512].rearrange("(k p) c -> p k c", p=128)) for q in range(4)],
                wr=[reg("wo_lo")], deps=tokB)
            zero_ssq()
            r = smr["D"]
            items = []
            for T in range(2):
                for b in range(4):
                    for c in range(4):
                        for sl in range(2):
                            d = {}
                            j = 4 * T + b
                            Gs = [G for G in (2 * j - 1, 2 * j, 2 * j + 1) if G >= 0]

                            def s1(j=j, c=c, sl=sl, d=d, Gs=Gs):
                                w0 = 3 - len(Gs)
                                prt = slice(64 * sl, 64 * sl + 64)
                                sbk = SB[nxt("ps", 4)]
                                MM([mm(P[:, sbk, (w0 + i) * 128:(w0 + i + 1) * 128], kTD[prt, kap(G) * 128:(kap(G) + 1) * 128],
                                       qTD[prt, c, j * 128:(j + 1) * 128], True, True) for i, G in enumerate(Gs)],
                                   rd=[dreg["kT"], dreg["qT"]], wr=[pb[sbk]])
                                ps_ = nxt("pt", 4)
                                d["ps"] = ps_
                                OP("scalar", lambda e: e.activation(out=pt[:, ps_, w0 * 128:384], in_=P[:, sbk, w0 * 128:384],
                                                                    func=AF.Exp, scale=0.125), rd=[pb[sbk]], wr=[ptreg[ps_]])
                                OP("vector", lambda e: e.tensor_tensor(
                                    out=pt[:, ps_, w0 * 128:384], in0=pt[:, ps_, w0 * 128:384],
                                    in1=masks[:, MD0 + (j % 2) * 3 + w0:MD0 + (j % 2) * 3 + 3, :].rearrange("p a b -> p (a b)"), op=ALU.mult),
                                   rd=[ptreg[ps_], creg], wr=[ptreg[ps_]])

                            def s2(T=T, b=b, c=c, sl=sl, d=d, Gs=Gs, l=l):
                                w0 = 3 - len(Gs)
                                ps_ = d["ps"]
                                hd = c + 4 * sl
                                slot = (0, 2, 4, 6, 1, 3, 5, 7)[nxt("po8", 8)]
                                MM([mm(po8[:, slot, 0:65], pt[:, ps_, (w0 + i) * 128:(w0 + i + 1) * 128], VD[:, kap(G), sl, :], i == 0, i == len(Gs) - 1)
                                    for i, G in enumerate(Gs)], rd=[ptreg[ps_], dreg["V"], d1reg], wr=[por[slot]])
                                u = 240 + (hd % 8)
                                OP("vector", lambda e: e.tensor_tensor(out=sm[:, u:u + 1], in0=po8[:, slot, 64:65],
                                                                       in1=esink[:, 8 * l + hd:8 * l + hd + 1], op=ALU.add),
                                   rd=[por[slot], reg("esink")], wr=[r])
                                OP("vector", lambda e: e.reciprocal(out=sm[:, u + 8:u + 9], in_=sm[:, u:u + 1]), rd=[r], wr=[r])
                                OP("vector", lambda e: e.tensor_scalar(out=ytile[:, b, hd * 64:(hd + 1) * 64], in0=po8[:, slot, 0:64],
                                                                       scalar1=sm[:, u + 8:u + 9], scalar2=None, op0=ALU.mult),
                                   rd=[por[slot], r], wr=[yreg])
                                sumsq(b, hd, hd * 64, (hd + 1) * 64)
                                if b == 3 and c == 3 and sl == 1:
                                    epilogue(3, T)
                            items.append((s1, s2))
            pipeline(items, 2)

            tokD = list(d1reg.wdeps())
            for rg in dreg.values():
                tokD.extend(rg.wdeps())
            DMA("gpsimd", "wo", [(wo[:, 11:16, q * 512:(q + 1) * 512],
                                  w_out[l, 11 * 128:16 * 128, q * 512:(q + 1) * 512].rearrange("(k p) c -> p k c", p=128)) for q in range(4)],
                wr=[reg("wo_hi")], deps=tokD)
            xsrc = x_in if l == 0 else xs[(l - 1) % 2]
            xdst = out_d if l == depth - 1 else xs[l % 2]
            zreg = reg("ytile")
            r = reg("sm")
            def outproj(j):
                DMA("sync", "xr", [(xr, xsrc[j * 128:(j + 1) * 128, :])], wr=[wreg[2]])
                if j == 0:
                    DMA("sync", "gb", [(gam, ln_g[l:l + 1, :].partition_broadcast(128)[:, 0, :]),
                                       (bet, ln_b[l:l + 1, :].partition_broadcast(128)[:, 0, :])], wr=[wreg[0], wreg[1]])
                for hf in range(2):
                    for q in (2 * hf, 2 * hf + 1):
                        MM([mm(P[:, 4 + q, :], yT[:, k, j * 128:(j + 1) * 128], wo[:, k, q * 512:(q + 1) * 512], k == 0, False) for k in range(11)],
                           rd=[reg("wo_lo"), reg("yT")], wr=[por[2 * q], por[2 * q + 1]])
                    for q in (2 * hf, 2 * hf + 1):
                        MM([mm(P[:, 4 + q, :], yT[:, k, j * 128:(j + 1) * 128], wo[:, k, q * 512:(q + 1) * 512], False, k == 15) for k in range(11, 16)],
                           rd=[reg("wo_hi"), reg("yT")], wr=[por[2 * q], por[2 * q + 1]])

            outproj(0)
            for j in range(NB):
                for q in range(4):
                    OP("vector", lambda e, q=q: e.scalar_tensor_tensor(out=zt[:, q * 512:(q + 1) * 512], in0=xr[:, q * 512:(q + 1) * 512], scalar=ALPHA,
                                                                      in1=P[:, 4 + q, :], op0=ALU.mult, op1=ALU.add),
                       rd=[wreg[2], por[2 * q], por[2 * q + 1]], wr=[zreg])
                    OP("vector", lambda e, q=q: e.bn_stats(out=sm[:, 176 + 6 * q:182 + 6 * q], in_=zt[:, q * 512:(q + 1) * 512]), rd=[zreg], wr=[r])
                OP("vector", lambda e: e.bn_aggr(out=sm[:, 200:202], in_=sm[:, 176:200]), rd=[r], wr=[r])
                OP("vector", lambda e: e.tensor_scalar(out=sm[:, 202:203], in0=sm[:, 201:202], scalar1=1e-5, scalar2=None, op0=ALU.add), rd=[r], wr=[r])
                OP("scalar", lambda e: e.activation(out=sm[:, 203:204], in_=sm[:, 202:203], func=AF.Ln), rd=[r], wr=[r])
                OP("scalar", lambda e: e.activation(out=sm[:, 204:205], in_=sm[:, 203:204], func=AF.Exp, scale=-0.5), rd=[r], wr=[r])
                OP("vector", lambda e: e.scalar_tensor_tensor(out=zt, in0=zt, scalar=sm[:, 200:201], in1=gam, op0=ALU.subtract, op1=ALU.mult),
                   rd=[r, zreg, wreg[0]], wr=[zreg])
                OP("vector", lambda e: e.scalar_tensor_tensor(out=zt, in0=zt, scalar=sm[:, 204:205], in1=bet, op0=ALU.mult, op1=ALU.add),
                   rd=[r, zreg, wreg[1]], wr=[zreg])
                DMA("sync", "xo", [(xdst[j * 128:(j + 1) * 128, :], zt)], rd=[zreg])
                if l < depth - 1:
                    OP("scalar", lambda e: e.activation(out=xb, in_=zt, func=AF.Copy), rd=[zreg], wr=streg)
                if j + 1 < NB:
                    outproj(j + 1)
                if l < depth - 1:
                    block_to_xT(None, None, j)
            S.barrier()

        S.emit()
    return nc


def _mult(delta):
    m = np.zeros_like(delta, dtype=np.float32)
    ok = delta >= 0
    m += (ok & (delta <= 128))
    m += (ok & (delta % 4 == 0) & (delta <= 512))
    m += (ok & (delta % 16 == 0) & (delta <= 2048))
    return m


def host_tables(r):
    i = np.arange(128)[None, :]
    k = np.arange(128)[:, None]
    masks = np.zeros((128, 44, 128), np.float32)
    for jpar in range(2):
        e = (jpar + r) % 2
        for t in range(17):
            masks[:, t * 2 + jpar, :] = _mult((t - 1 + e) * 128 + i - k)
        for w in range(2):
            masks[:, 34 + jpar * 2 + w, :] = (((e - w) * 128 + i - k) >= 0)
        for w in range(3):
            dd = (e + 1 - w) * 128 + i - k
            masks[:, 38 + jpar * 3 + w, :] = ((dd >= 0) & (dd <= 127))
    pos = np.concatenate([gblk(r, j) * 128 + np.arange(128) for j in range(NB)]).astype(np.float32)
    tabs = np.zeros((128, 4, 1024), np.float32)
    p = np.arange(128)
    inv128 = (np.float32(10000.0) ** (-np.arange(0, 128, 2, dtype=np.float32) / np.float32(128))).astype(np.float32)
    inv64 = (np.float32(10000.0) ** (-np.arange(0, 64, 2, dtype=np.float32) / np.float32(64))).astype(np.float32)
    a128 = (pos[None, :] * inv128[p % 64][:, None]).astype(np.float32)
    a64 = (pos[None, :] * inv64[(p % 64) % 32][:, None]).astype(np.float32)
    tabs[:, 0] = np.cos(a128)
    tabs[:, 1] = np.sin(a128) * np.where(p < 64, -1.0, 1.0)[:, None]
    tabs[:, 2] = np.cos(a64)
    tabs[:, 3] = np.sin(a64) * np.where((p % 64) < 32, -1.0, 1.0)[:, None]
    return masks.astype(ml_dtypes.bfloat16), tabs


def host_inputs(x, w_in, q_norm, w_uq, kv_norm, w_ukv, sinks, branch_norm, w_out, ln_gamma, ln_beta):
    f = lambda a: np.ascontiguousarray(np.asarray(a, dtype=np.float32))
    x, w_in, w_uq, w_ukv, w_out = f(x), f(w_in), f(w_uq), f(w_ukv), f(w_out)
    q_norm, kv_norm, sinks, branch_norm, ln_gamma, ln_beta = f(q_norm), f(kv_norm), f(sinks), f(branch_norm), f(ln_gamma), f(ln_beta)
    cols = np.zeros((128, 84), np.float32)
    for l in range(DEPTH):
        cols[:, 21 * l:21 * l + 3] = q_norm[l].reshape(3, 128).T
        cols[:, 21 * l + 3:21 * l + 5] = kv_norm[l].reshape(2, 128).T
        cols[:, 21 * l + 5:21 * l + 21] = branch_norm[l].reshape(16, 128).T
    sinks_bc = np.ascontiguousarray(np.broadcast_to(sinks.reshape(1, 32), (128, 32)))
    negm = np.zeros((128, 8, 8), np.float32)
    for j in range(8):
        negm[:, j, j:] = NEG
    negm = negm.reshape(128, 64)
    ident = np.eye(128, dtype=np.float32).astype(ml_dtypes.bfloat16)
    tb = [host_tables(r) for r in range(2)]
    in_maps = []
    for c in range(8):
        b, r = c // 2, c % 2
        xo = np.concatenate([x[b, gblk(r, j) * 128:(gblk(r, j) + 1) * 128, :] for j in range(NB)], 0)
        in_maps.append({"x": np.ascontiguousarray(xo), "w_in": w_in, "w_uq": w_uq, "w_ukv": w_ukv, "w_out": w_out,
                        "ln_gamma": ln_gamma, "ln_beta": ln_beta, "cols": cols, "sinks_bc": sinks_bc, "negm": negm,
                        "tabs": tb[r][1], "masks": tb[r][0], "ident": ident})
    return in_maps


def assemble(results, key="out"):
    out = np.zeros((4, 2048, D), np.float32)
    for c in range(8):
        b, r = c // 2, c % 2
        o = np.asarray(results[c][key])
        for j in range(NB):
            g = gblk(r, j)
            out[b, g * 128:(g + 1) * 128, :] = o[j * 128:(j + 1) * 128, :]
    return out


_NC = {}


def kernel(x, w_in, q_norm, w_uq, kv_norm, w_ukv, sinks, branch_norm, w_out, ln_gamma, ln_beta):
    in_maps = host_inputs(x, w_in, q_norm, w_uq, kv_norm, w_ukv, sinks, branch_norm, w_out, ln_gamma, ln_beta)
    if "nc" not in _NC:
        _NC["nc"] = build(DEPTH, False)
    res = run_bass_kernel_spmd(_NC["nc"], in_maps, core_ids=list(range(8)))
    return assemble(res.results)
```
